# Optimizing a Trainium2 kernel written in Bass

```python
import math
import jax
import jax.numpy as jnp
from jax import lax
import numpy as np

D_MODEL = 1024
BATCH = 8
SEQ = 8192
DEPTH = 2

HEAD_DIM = 64
GRID_W = 64
N_MEM = 256
NA_HEADS = 6
NA_WIN_ROWS = 8
NA_WIN_COLS = 16
ML_HEADS = 4
ML_CHUNK = 64
GDN_HEADS = 6
GDN_CHUNK = 64
CONV_K = 5
N_DIR = 2
XA_HEADS = 4
XA_HEAD_DIM = D_MODEL // XA_HEADS
D_FF = 4 * D_MODEL
NA_WIDTH = NA_HEADS * HEAD_DIM
ML_WIDTH = ML_HEADS * HEAD_DIM
GDN_WIDTH = GDN_HEADS * HEAD_DIM
D_MIX = NA_WIDTH + ML_WIDTH + GDN_WIDTH
EPS = 1e-6
IN_SIZES = (NA_WIDTH, NA_WIDTH, NA_WIDTH,
            ML_WIDTH, ML_WIDTH, ML_WIDTH, ML_WIDTH, N_DIR * ML_HEADS, N_DIR * ML_HEADS,
            GDN_WIDTH, GDN_WIDTH, GDN_WIDTH, GDN_WIDTH, N_DIR * GDN_HEADS, N_DIR * GDN_HEADS)
IN_OFFSETS = tuple(int(o) for o in np.cumsum(IN_SIZES)[:-1])
P_IN = int(sum(IN_SIZES))

kernel_name = 'hybrid_na_mlstm_gdn_encoder'


def rmsnorm(x, w):
    xf = x.astype(jnp.float32)
    y = xf * lax.rsqrt(jnp.mean(xf * xf, axis=-1, keepdims=True) + EPS)
    return (y * w.astype(jnp.float32)).astype(x.dtype)


def head_rms(t):
    return t * lax.rsqrt(jnp.mean(t * t, axis=-1, keepdims=True) + EPS)


def l2norm(t):
    return t * lax.rsqrt(jnp.sum(t * t, axis=-1, keepdims=True) + EPS)


def neighbourhood_attention(q, k, v, rel_bias):
    b, s, h, dh = q.shape
    rows = s // GRID_W
    wr = min(NA_WIN_ROWS, rows)
    wc = NA_WIN_COLS
    qg = (q * dh ** -0.5).reshape(b, rows, GRID_W, h, dh)
    kg = k.reshape(b, rows, GRID_W, h, dh)
    vg = v.reshape(b, rows, GRID_W, h, dh)
    cols = jnp.arange(GRID_W)
    col_idx = jnp.clip(cols - wc // 2, 0, GRID_W - wc)[:, None] + jnp.arange(wc)[None, :]
    dc = col_idx - cols[:, None] + (NA_WIN_COLS - 1)

    def row_block(args):
        q_r, r = args
        r0 = jnp.clip(r - wr // 2, 0, rows - wr)
        k_win = lax.dynamic_slice_in_dim(kg, r0, wr, axis=1)[:, :, col_idx]
        v_win = lax.dynamic_slice_in_dim(vg, r0, wr, axis=1)[:, :, col_idx]
        dr = r0 + jnp.arange(wr) - r + (NA_WIN_ROWS - 1)
        bias = rel_bias[:, dr[None, :, None], dc[:, None, :]]
        logits = jnp.einsum('bqhd,brqchd->bhqrc', q_r, k_win).astype(jnp.float32) + bias.astype(jnp.float32)
        p = jax.nn.softmax(logits.reshape(b, h, GRID_W, wr * wc), axis=-1).reshape(logits.shape)
        return jnp.einsum('bhqrc,brqchd->bqhd', p.astype(v.dtype), v_win)

    out = lax.map(row_block, (jnp.moveaxis(qg, 1, 0), jnp.arange(rows)))
    return jnp.moveaxis(out, 0, 1).reshape(b, s, h * dh)


def mlstm_direction(q, k, v, ig, f_pre):
    b, h, s, d = q.shape
    L = ML_CHUNK
    nc = s // L
    q = q.reshape(b, h, nc, L, d)
    k = k.reshape(b, h, nc, L, d) * d ** -0.5
    v = v.reshape(b, h, nc, L, d)
    ig = ig.reshape(b, h, nc, L)
    bcum = jnp.cumsum(jax.nn.log_sigmoid(f_pre).reshape(b, h, nc, L), axis=-1)
    b_last = bcum[..., -1]
    w = b_last[..., None] - bcum + ig
    m_loc = jnp.max(w, axis=-1)
    wexp = jnp.exp(w - m_loc[..., None])
    c_loc = jnp.einsum('bhnl,bhnld,bhnle->nbhde', wexp, k, v)
    n_loc = jnp.einsum('bhnl,bhnld->nbhd', wexp, k)

    def step(carry, xs):
        c, n, m = carry
        c_l, n_l, m_l, bl = xs
        m_new = jnp.maximum(bl + m, m_l)
        a = jnp.exp(bl + m - m_new)
        g = jnp.exp(m_l - m_new)
        c_new = a[..., None, None] * c + g[..., None, None] * c_l
        n_new = a[..., None] * n + g[..., None] * n_l
        return (c_new, n_new, m_new), (c, n, m)

    init = (jnp.zeros((b, h, d, d), jnp.float32), jnp.zeros((b, h, d), jnp.float32), jnp.zeros((b, h), jnp.float32))
    _, (c_prev, n_prev, m_prev) = lax.scan(
        step, init, (c_loc, n_loc, jnp.moveaxis(m_loc, -1, 0), jnp.moveaxis(b_last, -1, 0)))
    c_prev = jnp.moveaxis(c_prev, 0, 2)
    n_prev = jnp.moveaxis(n_prev, 0, 2)
    m_prev = jnp.moveaxis(m_prev, 0, -1)
    causal = jnp.tril(jnp.ones((L, L), dtype=bool))
    dmat = jnp.where(causal, bcum[..., :, None] - bcum[..., None, :] + ig[..., None, :], -jnp.inf)
    inter = bcum + m_prev[..., None]
    m_t = jnp.maximum(jnp.max(dmat, axis=-1), inter)
    s_intra = jnp.einsum('bhnld,bhnsd->bhnls', q, k) * jnp.exp(dmat - m_t[..., None])
    a_inter = jnp.exp(inter - m_t)
    num = a_inter[..., None] * jnp.einsum('bhnld,bhnde->bhnle', q, c_prev) + jnp.einsum('bhnls,bhnse->bhnle', s_intra, v)
    den = a_inter * jnp.einsum('bhnld,bhnd->bhnl', q, n_prev) + jnp.sum(s_intra, axis=-1)
    out = num / jnp.maximum(jnp.abs(den), jnp.exp(-m_t))[..., None]
    return out.reshape(b, h, s, d)


def mlstm_mixer(q, k, v, o_pre, i_pre, f_pre, i_bias, f_bias, norm_w):
    b, s, _ = q.shape

    def heads(t):
        return t.reshape(b, s, ML_HEADS, HEAD_DIM).transpose(0, 2, 1, 3).astype(jnp.float32)

    qh, kh, vh = heads(q), heads(k), heads(v)
    ig = (i_pre.reshape(b, s, N_DIR, ML_HEADS).astype(jnp.float32) + i_bias.astype(jnp.float32)).transpose(2, 0, 3, 1)
    fg = (f_pre.reshape(b, s, N_DIR, ML_HEADS).astype(jnp.float32) + f_bias.astype(jnp.float32)).transpose(2, 0, 3, 1)
    h_fwd = mlstm_direction(qh, kh, vh, ig[0], fg[0])
    h_bwd = jnp.flip(mlstm_direction(jnp.flip(qh, 2), jnp.flip(kh, 2), jnp.flip(vh, 2),
                                     jnp.flip(ig[1], 2), jnp.flip(fg[1], 2)), 2)
    hs = head_rms((h_fwd + h_bwd).transpose(0, 2, 1, 3)).reshape(b, s, ML_WIDTH)
    out = hs * norm_w.astype(jnp.float32) * jax.nn.sigmoid(o_pre.astype(jnp.float32))
    return out.astype(q.dtype)


def centred_depthwise_conv(x, w):
    c = x.shape[-1]
    return lax.conv_general_dilated(
        x, w[:, None, :].astype(x.dtype), window_strides=(1,),
        padding=[(CONV_K // 2, CONV_K // 2)],
        dimension_numbers=('NWC', 'WIO', 'NWC'), feature_group_count=c)


def gdn_direction(q, k, v, g, beta):
    b, h, s, dk = q.shape
    dv = v.shape[-1]
    L = GDN_CHUNK
    nc = s // L
    q = (q * dk ** -0.5).reshape(b, h, nc, L, dk)
    k = k.reshape(b, h, nc, L, dk)
    v = v.reshape(b, h, nc, L, dv)
    beta = beta.reshape(b, h, nc, L)
    gc = jnp.cumsum(g.reshape(b, h, nc, L), axis=-1)
    incl = jnp.tril(jnp.ones((L, L), dtype=bool))
    strict = jnp.tril(jnp.ones((L, L), dtype=bool), -1)
    diff = gc[..., :, None] - gc[..., None, :]
    lmask = jnp.where(incl, jnp.exp(jnp.where(incl, diff, 0.0)), 0.0)
    kb = k * beta[..., None]
    a = jnp.where(strict, jnp.einsum('bhnld,bhnsd->bhnls', kb, k) * lmask, 0.0)
    eye = jnp.eye(L, dtype=jnp.float32)
    t = lax.linalg.triangular_solve(a + eye, jnp.broadcast_to(eye, a.shape),
                                    left_side=True, lower=True, unit_diagonal=True)
    u = jnp.einsum('bhnls,bhnse->bhnle', t, v * beta[..., None])
    wk = jnp.einsum('bhnls,bhnsd->bhnld', t, kb * jnp.exp(gc)[..., None])
    attn = jnp.where(incl, jnp.einsum('bhnld,bhnsd->bhnls', q, k) * lmask, 0.0)
    q_dec = q * jnp.exp(gc)[..., None]
    g_last = gc[..., -1]
    k_state = k * jnp.exp(g_last[..., None] - gc)[..., None]

    def step(state, xs):
        u_c, w_c, a_c, qd_c, ks_c, gl_c = xs
        v_new = u_c - jnp.einsum('bhld,bhde->bhle', w_c, state)
        o = jnp.einsum('bhld,bhde->bhle', qd_c, state) + jnp.einsum('bhls,bhse->bhle', a_c, v_new)
        state = state * jnp.exp(gl_c)[..., None, None] + jnp.einsum('bhld,bhle->bhde', ks_c, v_new)
        return state, o

    xs = tuple(jnp.moveaxis(t_, 2, 0) for t_ in (u, wk, attn, q_dec, k_state, g_last))
    _, o = lax.scan(step, jnp.zeros((b, h, dk, dv), jnp.float32), xs)
    return jnp.moveaxis(o, 0, 2).reshape(b, h, s, dv)


def gated_deltanet_mixer(q, k, v, z, beta_pre, alpha_pre, conv_w, a_log, dt_bias, norm_w):
    b, s, _ = q.shape
    qkv = jax.nn.silu(centred_depthwise_conv(jnp.concatenate([q, k, v], axis=-1), conv_w))
    qc, kc, vc = jnp.split(qkv, 3, axis=-1)

    def heads(t):
        return t.reshape(b, s, GDN_HEADS, HEAD_DIM).transpose(0, 2, 1, 3).astype(jnp.float32)

    qh, kh, vh = l2norm(heads(qc)), l2norm(heads(kc)), heads(vc)
    beta = jax.nn.sigmoid(beta_pre.reshape(b, s, N_DIR, GDN_HEADS).astype(jnp.float32)).transpose(2, 0, 3, 1)
    g = (-jnp.exp(a_log.astype(jnp.float32)) * jax.nn.softplus(
        alpha_pre.reshape(b, s, N_DIR, GDN_HEADS).astype(jnp.float32) + dt_bias.astype(jnp.float32))).transpose(2, 0, 3, 1)
    o_fwd = gdn_direction(qh, kh, vh, g[0], beta[0])
    o_bwd = jnp.flip(gdn_direction(jnp.flip(qh, 2), jnp.flip(kh, 2), jnp.flip(vh, 2),
                                   jnp.flip(g[1], 2), jnp.flip(beta[1], 2)), 2)
    o = head_rms((o_fwd + o_bwd).transpose(0, 2, 1, 3)).reshape(b, s, GDN_WIDTH)
    o = o * norm_w.astype(jnp.float32) * jax.nn.silu(z.astype(jnp.float32))
    return o.astype(z.dtype)


def memory_cross_attention(h, mem_n, w_q, w_kv, w_o):
    b, s, _ = h.shape
    m = mem_n.shape[1]
    q = (h @ w_q).reshape(b, s, XA_HEADS, XA_HEAD_DIM)
    kk, vv = jnp.split(mem_n @ w_kv, 2, axis=-1)
    kk = kk.reshape(b, m, XA_HEADS, XA_HEAD_DIM)
    vv = vv.reshape(b, m, XA_HEADS, XA_HEAD_DIM)
    logits = jnp.einsum('bshd,bmhd->bhsm', q, kk).astype(jnp.float32) * XA_HEAD_DIM ** -0.5
    p = jax.nn.softmax(logits, axis=-1)
    o = jnp.einsum('bhsm,bmhd->bshd', p.astype(vv.dtype), vv).reshape(b, s, XA_HEADS * XA_HEAD_DIM)
    return o @ w_o


def setup_inputs(seed: int = 0) -> dict:
    key = jax.random.key(seed)
    ks = jax.random.split(key, 24)
    f32 = jnp.float32

    def nrm(k, shape, scale):
        return jax.random.normal(k, shape, f32) * scale

    def gain(k, shape):
        return 1.0 + 0.02 * jax.random.normal(k, shape, f32)

    dt = jnp.exp(jax.random.uniform(ks[12], (DEPTH, N_DIR, GDN_HEADS), f32, math.log(1e-3), math.log(1e-1)))
    return {
        'x': nrm(ks[0], (BATCH, SEQ, D_MODEL), 1.0),
        'mem': nrm(ks[1], (BATCH, N_MEM, D_MODEL), 1.0),
        'norm_mix_w': gain(ks[2], (DEPTH, D_MODEL)),
        'w_in': nrm(ks[3], (DEPTH, D_MODEL, P_IN), D_MODEL ** -0.5),
        'na_rel_bias': nrm(ks[4], (DEPTH, NA_HEADS, 2 * NA_WIN_ROWS - 1, 2 * NA_WIN_COLS - 1), 0.02),
        'ml_i_bias': nrm(ks[5], (DEPTH, N_DIR, ML_HEADS), 0.1),
        'ml_f_bias': 3.0 + 3.0 * jax.random.uniform(ks[6], (DEPTH, N_DIR, ML_HEADS), f32),
        'ml_norm_w': gain(ks[7], (DEPTH, ML_WIDTH)),
        'gdn_conv_w': nrm(ks[8], (DEPTH, CONV_K, 3 * GDN_WIDTH), CONV_K ** -0.5),
        'gdn_a_log': jnp.log(jax.random.uniform(ks[9], (DEPTH, N_DIR, GDN_HEADS), f32, 1.0, 16.0)),
        'gdn_dt_bias': dt + jnp.log(-jnp.expm1(-dt)),
        'gdn_norm_w': gain(ks[10], (DEPTH, GDN_WIDTH)),
        'w_out': nrm(ks[11], (DEPTH, D_MIX, D_MODEL), D_MIX ** -0.5),
        'norm_xa_w': gain(ks[13], (DEPTH, D_MODEL)),
        'norm_mem_w': gain(ks[14], (DEPTH, D_MODEL)),
        'w_xq': nrm(ks[15], (DEPTH, D_MODEL, XA_HEADS * XA_HEAD_DIM), D_MODEL ** -0.5),
        'w_xkv': nrm(ks[16], (DEPTH, D_MODEL, 2 * XA_HEADS * XA_HEAD_DIM), D_MODEL ** -0.5),
        'w_xo': nrm(ks[17], (DEPTH, XA_HEADS * XA_HEAD_DIM, D_MODEL), (XA_HEADS * XA_HEAD_DIM) ** -0.5),
        'norm_ffn_w': gain(ks[18], (DEPTH, D_MODEL)),
        'w_ff1': nrm(ks[19], (DEPTH, D_MODEL, D_FF), D_MODEL ** -0.5),
        'w_ff2': nrm(ks[20], (DEPTH, D_FF, D_MODEL), D_FF ** -0.5),
        'norm_out_w': gain(ks[21], (D_MODEL,)),
    }


def reference(x, mem, norm_mix_w, w_in, na_rel_bias, ml_i_bias, ml_f_bias, ml_norm_w,
              gdn_conv_w, gdn_a_log, gdn_dt_bias, gdn_norm_w, w_out, norm_xa_w, norm_mem_w,
              w_xq, w_xkv, w_xo, norm_ffn_w, w_ff1, w_ff2, norm_out_w):
    b, s, _ = x.shape
    for l in range(DEPTH):
        h = rmsnorm(x, norm_mix_w[l])
        (na_q, na_k, na_v, ml_q, ml_k, ml_v, ml_o, ml_i, ml_f,
         gd_q, gd_k, gd_v, gd_z, gd_b, gd_a) = jnp.split(h @ w_in[l], IN_OFFSETS, axis=-1)
        y_na = neighbourhood_attention(na_q.reshape(b, s, NA_HEADS, HEAD_DIM),
                                       na_k.reshape(b, s, NA_HEADS, HEAD_DIM),
                                       na_v.reshape(b, s, NA_HEADS, HEAD_DIM), na_rel_bias[l])
        y_ml = mlstm_mixer(ml_q, ml_k, ml_v, ml_o, ml_i, ml_f, ml_i_bias[l], ml_f_bias[l], ml_norm_w[l])
        y_gd = gated_deltanet_mixer(gd_q, gd_k, gd_v, gd_z, gd_b, gd_a, gdn_conv_w[l],
                                    gdn_a_log[l], gdn_dt_bias[l], gdn_norm_w[l])
        x = x + jnp.concatenate([y_na, y_ml, y_gd], axis=-1) @ w_out[l]
        x = x + memory_cross_attention(rmsnorm(x, norm_xa_w[l]), rmsnorm(mem, norm_mem_w[l]),
                                       w_xq[l], w_xkv[l], w_xo[l])
        h = rmsnorm(x, norm_ffn_w[l])
        x = x + jnp.square(jax.nn.relu(h @ w_ff1[l])) @ w_ff2[l]
    return rmsnorm(x, norm_out_w)
```

```python
import numpy as np
from contextlib import ExitStack

import concourse.bass as bass
import concourse.mybir as mybir
from concourse.bass_utils import run_bass_kernel_spmd

F32 = mybir.dt.float32
BF16 = mybir.dt.bfloat16
AF = mybir.ActivationFunctionType
ALU = mybir.AluOpType
AX = mybir.AxisListType

ENGS = ("pe", "dve", "act", "pool", "sp")
SEM_CAP = 30000
N_DMA_SEM = 12


def _prod(v):
    r = 1
    for a in v:
        r *= int(a)
    return r


def region(ap):
    t = ap.tensor
    shape = [int(s) for s in t.shape]
    rowlen = _prod(shape[1:])
    off = int(ap.offset)
    p0 = off // rowlen
    c0 = off % rowlen
    p1, c1 = p0, c0
    for step, cnt in ap.ap:
        step, cnt = int(step), int(cnt)
        if cnt <= 1 or step == 0:
            continue
        ext = step * (cnt - 1)
        if step % rowlen == 0:
            p1 += ext // rowlen
        else:
            c1 += ext
    if c1 >= rowlen:
        tot = off + (p1 - p0) * rowlen + (c1 - c0)
        p1 = tot // rowlen
        c0, c1 = 0, rowlen - 1
    if type(t).__name__ == "PSumTensorHandle":
        return (t.name, 0, 127, 0, rowlen - 1)
    return (t.name, p0, p1, c0, c1)


def _overlap(a, b):
    return not (a[2] < b[1] or b[2] < a[1] or a[4] < b[3] or b[4] < a[3])


def _contains(a, b):
    return a[1] <= b[1] and a[2] >= b[2] and a[3] <= b[3] and a[4] >= b[4]


class Op:
    __slots__ = ("eng", "fn", "dma", "seq", "deps", "signal", "sig_idx", "dsem", "dval",
                 "clock", "prewait")


class Prog:
    def __init__(self, nc, es):
        self.nc = nc
        self.es = es
        self.ops = []
        self.recs = {}
        self.nseq = {e: 0 for e in ENGS}
        self.clock = {e: {x: 0 for x in ENGS} for e in ENGS}
        self.known_dma = {e: set() for e in ENGS}
        self.last_compute = {e: None for e in ENGS}
        self.pending_dma = []
        self.esems = {e: [] for e in ENGS}
        self.nsig = {e: 0 for e in ENGS}
        self.dsems = {}
        for q in ("sp", "act", "pool"):
            self.dsems[q] = [[es.enter_context(nc.semaphore(f"d_{q}_{i}")), 0, None]
                             for i in range(N_DMA_SEM)]
        self.dma_rr = {q: 0 for q in self.dsems}
        self.emitted = 0

    def _need(self, op, dep):
        if dep is op:
            return
        c = op.eng
        if dep.dma:
            if dep in self.known_dma[c]:
                return
            self.known_dma[c].add(dep)
            op.deps.append(dep)
            clk = dep.clock
        else:
            if self.clock[c][dep.eng] >= dep.seq:
                return
            op.deps.append(dep)
            dep.signal = True
            clk = dict(dep.clock)
            clk[dep.eng] = max(clk[dep.eng], dep.seq)
        mine = self.clock[c]
        for e in ENGS:
            if clk[e] > mine[e]:
                mine[e] = clk[e]

    def add(self, eng, fn, reads=(), writes=(), dma=False):
        op = Op()
        op.eng, op.fn, op.dma = eng, fn, dma
        op.deps, op.signal, op.sig_idx = [], False, None
        op.dsem = op.dval = None
        op.prewait = None
        self.nseq[eng] += 1
        op.seq = self.nseq[eng]
        if dma:
            q = eng
            i = self.dma_rr[q]
            self.dma_rr[q] = (i + 1) % N_DMA_SEM
            slot = self.dsems[q][i]
            if slot[2] is not None:
                self._need(op, slot[2])
            slot[1] += 16
            slot[2] = op
            op.dsem, op.dval = slot[0], slot[1]
        accs = ([(region(a), type(a.tensor).__name__ == "PSumTensorHandle") for a in reads] +
                [(region(a), True) for a in writes])
        for box, is_w in accs:
            for rbox, rop, rw in self.recs.get(box[0], ()):
                if _overlap(box, rbox) and (is_w or rw):
                    same = (rop.eng == eng and not rop.dma and not dma)
                    if same and eng == "pe":
                        pass
                    else:
                        self._need(op, rop)
        for box, is_w in accs:
            keep = []
            for rec in self.recs.get(box[0], ()):
                rbox, rop, rw = rec
                if is_w and _contains(box, rbox) and rop is not op:
                    continue
                if (not is_w) and (not rw) and rbox == box and rop.eng == eng and not rop.dma and not dma:
                    continue
                keep.append(rec)
            keep.append((box, op, is_w))
            self.recs[box[0]] = keep
        op.clock = dict(self.clock[eng])
        self.ops.append(op)
        if dma:
            self.pending_dma.append(op)
        else:
            self.last_compute[eng] = op
        return op

    def barrier(self):
        for e in ENGS:
            op = Op()
            op.eng, op.fn, op.dma = e, None, False
            op.deps, op.signal, op.sig_idx = [], False, None
            op.dsem = op.dval = None
            op.prewait = None
            self.nseq[e] += 1
            op.seq = self.nseq[e]
            for o in ENGS:
                lc = self.last_compute[o]
                if lc is not None:
                    self._need(op, lc)
            for q in self.dsems:
                for slot in self.dsems[q]:
                    if slot[2] is not None:
                        self._need(op, slot[2])
            op.clock = dict(self.clock[e])
            self.ops.append(op)
        self.pending_dma = []
        self.recs = {}

    def _sem_for(self, eng, idx):
        k = (idx - 1) // SEM_CAP
        while len(self.esems[eng]) <= k:
            self.esems[eng].append(self.es.enter_context(
                self.nc.semaphore(f"s_{eng}_{len(self.esems[eng])}")))
        return self.esems[eng][k], idx - k * SEM_CAP

    def emit(self):
        ops = self.ops[self.emitted:]
        self.emitted = len(self.ops)
        for op in ops:
            if op.signal and not op.dma:
                self.nsig[op.eng] += 1
                op.sig_idx = self.nsig[op.eng]
        per = {e: [o for o in ops if o.eng == e] for e in ENGS}
        prog = self

        def run(e, h):
            for op in per[e]:
                for d in op.deps:
                    if d.dma:
                        h.wait_ge(d.dsem, d.dval)
                    else:
                        s, v = prog._sem_for(d.eng, d.sig_idx)
                        h.wait_ge(s, v)
                if op.fn is None:
                    if op.signal:
                        s, v = prog._sem_for(e, op.sig_idx)
                        h.sem_inc(s, 1)
                    continue
                inst = op.fn(h)
                if op.dma:
                    inst.then_inc(op.dsem, 16)
                elif op.signal:
                    s, v = prog._sem_for(e, op.sig_idx)
                    inst.then_inc(s, 1)

        for op in ops:
            if op.signal and not op.dma:
                self._sem_for(op.eng, op.sig_idx)
        with self.nc.Block() as block:
            @block.tensor
            def _(h):
                run("pe", h)

            @block.vector
            def _(h):
                run("dve", h)

            @block.scalar
            def _(h):
                run("act", h)

            @block.gpsimd
            def _(h):
                run("pool", h)

            @block.sync
            def _(h):
                run("sp", h)


class K:
    def __init__(self, nc, es):
        self.nc = nc
        self.P = Prog(nc, es)
        self.uid = 0

    def name(self, base):
        self.uid += 1
        return f"{base}_{self.uid}"

    def mm(self, out, lhsT, rhs, start=True, stop=True):
        self.P.add("pe", lambda h: h.matmul(out, lhsT, rhs, start=start, stop=stop),
                   reads=[lhsT, rhs], writes=[out])

    def tr(self, out, in_, ident):
        self.P.add("pe", lambda h: h.transpose(out, in_, ident), reads=[in_, ident], writes=[out])

    def act(self, out, in_, func, bias=None, scale=None, accum=None):
        kw = {}
        rd = [in_]
        wr = [out]
        if bias is not None:
            kw["bias"] = bias
            if not isinstance(bias, (int, float)):
                rd.append(bias)
        if scale is not None:
            kw["scale"] = scale
            if not isinstance(scale, (int, float)):
                rd.append(scale)
        if accum is not None:
            kw["accum_out"] = accum
            wr.append(accum)
        self.P.add("act", lambda h: h.activation(out, in_, func, **kw), reads=rd, writes=wr)

    def ts(self, eng, out, in0, s1, op0, s2=None, op1=None, accum=None):
        rd = [in0]
        if not isinstance(s1, (int, float)):
            rd.append(s1)
        if s2 is not None and not isinstance(s2, (int, float)):
            rd.append(s2)
        wr = [out]
        kw = {}
        if op1 is not None:
            kw["op1"] = op1
        if accum is not None:
            kw["accum_out"] = accum
            wr.append(accum)
        self.P.add(eng, lambda h: h.tensor_scalar(out, in0, s1, s2, op0, **kw), reads=rd, writes=wr)

    def tt(self, eng, out, in0, in1, op):
        self.P.add(eng, lambda h: h.tensor_tensor(out, in0, in1, op), reads=[in0, in1], writes=[out])

    def stt(self, out, in0, scalar, in1, op0, op1):
        rd = [in0, in1]
        if not isinstance(scalar, (int, float)):
            rd.append(scalar)
        self.P.add("dve", lambda h: h.scalar_tensor_tensor(out, in0, scalar, in1, op0, op1),
                   reads=rd, writes=[out])

    def copy(self, eng, out, in_):
        if eng == "act":
            self.P.add("act", lambda h: h.copy(out, in_), reads=[in_], writes=[out])
        else:
            self.P.add(eng, lambda h: h.tensor_copy(out, in_), reads=[in_], writes=[out])

    def memset(self, eng, out, val):
        self.P.add(eng, lambda h: h.memset(out, val), reads=[], writes=[out])

    def recip(self, out, in_):
        self.P.add("dve", lambda h: h.reciprocal(out, in_), reads=[in_], writes=[out])

    def scan(self, out, d0, d1, init, op0, op1):
        self.P.add("dve", lambda h: h.tensor_tensor_scan(out, d0, d1, init, op0, op1),
                   reads=[d0, d1], writes=[out])

    def dma(self, q, out, in_, slow=False):
        if slow:
            self.P.add(q, lambda h: h.dma_start(out=out, in_=in_, allow_slow_non_contiguous=True),
                       reads=[in_], writes=[out], dma=True)
        else:
            self.P.add(q, lambda h: h.dma_start(out=out, in_=in_), reads=[in_], writes=[out], dma=True)


D = 1024
DEPTH = 2
P_IN = 3752
NMEM = 256
DFF = 4096
EPS = 1e-6
WIN_BLOCKS = [(0, 768, 0), (1152, 1664, 768), (2192, 3344, 1280), (2176, 2192, 2432),
              (3728, 3752, 2448), (768, 1152, 2472), (1664, 2176, 2856), (3344, 3728, 3368)]
FM_ROWS = 2432
TM_COLS = 1536
FM_NAQ, FM_NAK, FM_MLQ, FM_MLK, FM_GDQ, FM_GDK, FM_GDV = 0, 384, 768, 1024, 1280, 1664, 2048
TM_NAV, TM_MLV, TM_MLO, TM_GDZ, TM_MLK = 0, 384, 640, 896, 1280


class Ring:
    def __init__(self, k, es, name, n, shape, dtype, psum=False):
        mk = k.nc.psum_tensor if psum else k.nc.sbuf_tensor
        self.t = [es.enter_context(mk(k.name(name), shape, dtype)) for _ in range(n)]
        self.i = 0

    def next(self):
        t = self.t[self.i]
        self.i = (self.i + 1) % len(self.t)
        return t


def rmsnorm_rows(k, xt, hn, junk, st, eng_scale="dve"):
    k.act(junk[:], xt[:], AF.Square, accum=st[:, 0:1])
    k.act(st[:, 1:2], st[:, 0:1], AF.Sqrt, bias=EPS, scale=1.0 / D)
    k.recip(st[:, 2:3], st[:, 1:2])
    k.ts(eng_scale, hn[:], xt[:], st[:, 2:3], ALU.mult)


def phase_inproj(k, S, l, dr, cst):
    nc = k.nc
    NG = S // 512
    with ExitStack() as es:
        w = es.enter_context(nc.sbuf_tensor(k.name("win"), [128, 8, P_IN], BF16))
        nw = es.enter_context(nc.sbuf_tensor(k.name("nw"), [128, 8], F32))
        wfull = es.enter_context(nc.sbuf_tensor(k.name("wfull"), [128, 8, 128], BF16))
        ones = es.enter_context(nc.sbuf_tensor(k.name("ones"), [128, 128], F32))
        xt_r = Ring(k, es, "xt", 2, [128, D], F32)
        hn_r = Ring(k, es, "hn", 2, [128, D], BF16)
        junk = es.enter_context(nc.sbuf_tensor(k.name("junk"), [128, D], BF16))
        st_r = Ring(k, es, "st", 2, [128, 4], F32)
        hT_r = Ring(k, es, "hT", 2, [128, 8, 512], BF16)
        fmst_r = Ring(k, es, "fmst", 4, [128, 512], BF16)
        gst_r = Ring(k, es, "gst", 2, [12, 512], F32)
        tmst_r = Ring(k, es, "tmst", 2, [128, TM_COLS], BF16)
        gtst_r = Ring(k, es, "gtst", 2, [128, 40], F32)
        pt_r = Ring(k, es, "ptr", 2, [128, 8, 128], BF16, psum=True)
        pm_r = Ring(k, es, "pmm", 4, [128, 512], F32, psum=True)

        wsrc = dr["w_in"].ap()[l].rearrange("(c p) n -> p c n", p=128)
        for (a, b, m) in WIN_BLOCKS:
            for c0 in range(0, 8, 4):
                k.dma("pool", w[:, c0:c0 + 4, m:m + (b - a)], wsrc[:, c0:c0 + 4, a:b])
        k.dma("sp", nw[:], dr["norm_mix_w"].ap()[l].rearrange("(c p) -> p c", p=128), slow=True)
        k.memset("dve", ones[:], 1.0)
        for c in range(8):
            k.ts("dve", wfull[:, c, :], ones[:], nw[:, c:c + 1], ALU.mult)

        xsrc = dr["x_cur"].ap().rearrange("(n p) d -> n p d", p=128)
        fm = dr["fm"].ap()
        tm = dr["tm"].ap().rearrange("(n p) c -> n p c", p=128)
        ev = 0
        for g in range(NG):
            hT = hT_r.next()
            for j in range(4):
                t = g * 4 + j
                xt = xt_r.next(); hn = hn_r.next(); st = st_r.next()
                k.dma("sp", xt[:], xsrc[t])
                rmsnorm_rows(k, xt, hn, junk, st)
                pt = pt_r.next()
                for c in range(8):
                    k.tr(pt[:, c, :], hn[:, c * 128:(c + 1) * 128], cst["ident_bf"][:])
                k.tt("dve", hT[:, :, j * 128:(j + 1) * 128], pt[:], wfull[:], ALU.mult)
            for m in range(19):
                ps = pm_r.next()
                for c in range(8):
                    k.mm(ps[:], w[:, c, m * 128:(m + 1) * 128], hT[:, c, :], start=(c == 0), stop=(c == 7))
                stg = fmst_r.next()
                k.copy("act" if ev % 2 == 0 else "dve", stg[:], ps[:])
                ev += 1
                k.dma("pool", fm[m * 128:(m + 1) * 128, g * 512:(g + 1) * 512], stg[:])
            for j in range(4):
                t = g * 4 + j
                stg = tmst_r.next()
                for (c0, n, o) in [(2472, 512, 0), (2984, 512, 512), (3496, 256, 1024), (1024, 256, 1280)]:
                    ps = pm_r.next()
                    for c in range(8):
                        k.mm(ps[:, 0:n], hT[:, c, j * 128:(j + 1) * 128], w[:, c, c0:c0 + n],
                             start=(c == 0), stop=(c == 7))
                    k.copy("act" if ev % 2 == 0 else "dve", stg[:, o:o + n], ps[:, 0:n])
                    ev += 1
                k.dma("pool", tm[t], stg[:])
                ps = pm_r.next()
                for c in range(8):
                    k.mm(ps[:, 0:40], hT[:, c, j * 128:(j + 1) * 128], w[:, c, 2432:2472],
                         start=(c == 0), stop=(c == 7))
                gs = gtst_r.next()
                k.copy("dve", gs[:], ps[:, 0:40])
                k.dma("pool", dr["gt"].ap()[t * 128:(t + 1) * 128, :], gs[:])
        k.P.barrier()
        k.P.emit()


def make_consts_host():
    c = {}
    c["c_ident"] = np.eye(128, dtype=np.float32)
    p = np.arange(128)[:, None]
    f = np.arange(128)[None, :]
    big = np.float32(30000.0)
    z = np.float32(0.0)
    c["c_masks"] = np.stack([
        (p <= f).astype(np.float32), (p >= f).astype(np.float32),
        np.where(p > f, z, big), np.where(f > p, z, big),
        np.where(f > p, z, -big), np.where(p > f, z, -big),
        np.where(f >= p, z, -big), np.where(f <= p, z, -big)], axis=1).astype(np.float32)
    es = np.zeros((128, 2, 64), np.float32)
    es[127, 0, :] = 1.0
    es[0, 1, :] = 1.0
    c["c_esel"] = es
    c["c_blk2"] = np.kron(np.eye(2, dtype=np.float32), np.ones((64, 64), np.float32))
    return c


def setup_consts(k, es, dr):
    nc = k.nc
    cst = {}
    idf = es.enter_context(nc.sbuf_tensor("ident_f", [128, 128], F32))
    idb = es.enter_context(nc.sbuf_tensor("ident_bf", [128, 128], BF16))
    k.dma("sp", idf[:], dr["c_ident"].ap())
    k.copy("dve", idb[:], idf[:])
    cst["ident_f"], cst["ident_bf"] = idf, idb
    for nm, shp in [("c_masks", [128, 8, 128]), ("c_esel", [128, 2, 64]), ("c_blk2", [128, 128])]:
        t = es.enter_context(nc.sbuf_tensor(nm + "_sb", shp, F32))
        k.dma("sp", t[:], dr[nm].ap())
        cst[nm] = t
    blkb = es.enter_context(nc.sbuf_tensor("blk2_bf", [128, 128], BF16))
    k.copy("dve", blkb[:], cst["c_blk2"][:])
    cst["blk2_bf"] = blkb
    k.P.barrier()
    k.P.emit()
    return cst


WEIGHT_SHAPES = {
    "norm_mix_w": (DEPTH, D), "w_in": (DEPTH, D, P_IN), "ml_i_bias": (DEPTH, 2, 4), "ml_f_bias": (DEPTH, 2, 4),
    "ml_norm_w": (DEPTH, 256), "gdn_conv_w": (DEPTH, 5, 1152), "gdn_a_log": (DEPTH, 2, 6),
    "gdn_dt_bias": (DEPTH, 2, 6), "gdn_norm_w": (DEPTH, 384), "w_out": (DEPTH, D, D),
    "norm_xa_w": (DEPTH, D), "norm_mem_w": (DEPTH, D), "w_xq": (DEPTH, D, D), "w_xkv": (DEPTH, D, 2 * D),
    "w_xo": (DEPTH, D, D), "norm_ffn_w": (DEPTH, D), "w_ff1": (DEPTH, D, DFF), "w_ff2": (DEPTH, DFF, D),
    "norm_out_w": (D,),
}


def declare_dram(nc, S, debug=(), ext_in=()):
    dr = {}
    dr["x"] = nc.dram_tensor("x", [S, D], F32, kind="ExternalInput")
    dr["mem"] = nc.dram_tensor("mem", [NMEM, D], F32, kind="ExternalInput")
    for n, shp in WEIGHT_SHAPES.items():
        dr[n] = nc.dram_tensor(n, list(shp), F32, kind="ExternalInput")
    for n, a in make_consts_host().items():
        dr[n] = nc.dram_tensor(n, list(a.shape), F32, kind="ExternalInput")
    dr["nab"] = nc.dram_tensor("nab", [DEPTH * 6, 128, 21 * 128], F32, kind="ExternalInput")

    def scratch(name, shape, dt):
        kind = "ExternalOutput" if name in debug else ("ExternalInput" if name in ext_in else "Internal")
        dr[name] = nc.dram_tensor(name, shape, dt, kind=kind)

    scratch("fm", [FM_ROWS, S], BF16)
    scratch("tm", [S, TM_COLS], BF16)
    scratch("gt", [S, 40], F32)
    scratch("hml", [S, 256], F32)
    scratch("ogd", [S, 384], F32)
    scratch("gqT", [384, S], BF16)
    scratch("gkT", [384, S], BF16)
    scratch("gk_tm", [S, 384], BF16)
    scratch("gv_tm", [S, 384], BF16)
    scratch("y", [S, D], BF16)
    scratch("xs", [S, D], F32)
    dr["out"] = nc.dram_tensor("out", [S, D], F32, kind="ExternalOutput")
    return dr


def load_w(k, dst, src2d, kc, ncols, split=4):
    v = src2d.rearrange("(c p) n -> p c n", p=128)
    for c0 in range(0, kc, split):
        k.dma("pool", dst[:, c0:c0 + split, :], v[:, c0:c0 + split, :])


def norm_to_T(k, xt, nwfull, hT, col0, hn_r, st_r, junk, pt_r, cst):
    hn = hn_r.next(); st = st_r.next()
    rmsnorm_rows(k, xt, hn, junk, st)
    pt = pt_r.next()
    for c in range(8):
        k.tr(pt[:, c, :], hn[:, c * 128:(c + 1) * 128], cst["ident_bf"][:])
    k.tt("dve", hT[:, :, col0:col0 + 128], pt[:], nwfull[:], ALU.mult)


def make_nwfull(k, es, src1d, ones):
    nc = k.nc
    nw = es.enter_context(nc.sbuf_tensor(k.name("nw"), [128, 8], F32))
    wfull = es.enter_context(nc.sbuf_tensor(k.name("wfull"), [128, 8, 128], BF16))
    k.dma("sp", nw[:], src1d.rearrange("(c p) -> p c", p=128), slow=True)
    for c in range(8):
        k.ts("dve", wfull[:, c, :], ones[:], nw[:, c:c + 1], ALU.mult)
    return wfull


def phase_mix_xattn(k, S, l, dr, cst, x_in, x_out):
    nc = k.nc
    NG = S // 512
    with ExitStack() as es:
        wout = es.enter_context(nc.sbuf_tensor(k.name("wout"), [128, 8, D], BF16))
        wxq = es.enter_context(nc.sbuf_tensor(k.name("wxq"), [128, 8, D], BF16))
        wxo = es.enter_context(nc.sbuf_tensor(k.name("wxo"), [128, 8, D], BF16))
        wkv = es.enter_context(nc.sbuf_tensor(k.name("wkv"), [128, 8, 2 * D], BF16))
        kkT = es.enter_context(nc.sbuf_tensor(k.name("kkT"), [128, 8, NMEM], BF16))
        vv = es.enter_context(nc.sbuf_tensor(k.name("vv"), [128, 2, D], BF16))
        memT = es.enter_context(nc.sbuf_tensor(k.name("memT"), [128, 8, NMEM], BF16))
        ones = es.enter_context(nc.sbuf_tensor(k.name("ones"), [128, 128], F32))
        ones_bf = es.enter_context(nc.sbuf_tensor(k.name("onesb"), [128, 128], BF16))
        xg = es.enter_context(nc.sbuf_tensor(k.name("xg"), [128, 4, D], F32))
        yt_r = Ring(k, es, "yt", 2, [128, D], BF16)
        yT = es.enter_context(nc.sbuf_tensor(k.name("yT"), [128, 8, 512], BF16))
        h2T = es.enter_context(nc.sbuf_tensor(k.name("h2T"), [128, 8, 512], BF16))
        qT = es.enter_context(nc.sbuf_tensor(k.name("qT"), [128, 8, 512], BF16))
        oT = es.enter_context(nc.sbuf_tensor(k.name("oT"), [128, 8, 512], BF16))
        PT_r = Ring(k, es, "PT", 4, [128, 512], BF16)
        rden_r = Ring(k, es, "rden", 2, [128, 512], F32)
        hn_r = Ring(k, es, "hn", 2, [128, D], BF16)
        junk = es.enter_context(nc.sbuf_tensor(k.name("junk"), [128, D], BF16))
        st_r = Ring(k, es, "st", 2, [128, 4], F32)
        mt_r = Ring(k, es, "mt", 2, [128, D], F32)
        pt_r = Ring(k, es, "ptr", 2, [128, 8, 128], BF16, psum=True)
        pm_r = Ring(k, es, "pmm", 5, [128, 512], F32, psum=True)

        k.memset("dve", ones[:], 1.0)
        k.memset("dve", ones_bf[:], 1.0)
        load_w(k, wkv, dr["w_xkv"].ap()[l], 8, 2 * D, split=2)
        load_w(k, wout, dr["w_out"].ap()[l], 8, D)
        load_w(k, wxq, dr["w_xq"].ap()[l], 8, D)
        load_w(k, wxo, dr["w_xo"].ap()[l], 8, D)
        nw_mem = make_nwfull(k, es, dr["norm_mem_w"].ap()[l], ones)
        nw_xa = make_nwfull(k, es, dr["norm_xa_w"].ap()[l], ones)
        msrc = dr["mem"].ap().rearrange("(n p) d -> n p d", p=128)
        for mt in range(2):
            m_t = mt_r.next()
            k.dma("sp", m_t[:], msrc[mt])
            norm_to_T(k, m_t, nw_mem, memT, mt * 128, hn_r, st_r, junk, pt_r, cst)
        ev = 0
        for m in range(8):
            ps = pm_r.next()
            for c in range(8):
                k.mm(ps[:, 0:NMEM], wkv[:, c, m * 128:(m + 1) * 128], memT[:, c, :], start=(c == 0), stop=(c == 7))
            k.copy("act", kkT[:, m, :], ps[:, 0:NMEM])
        for mt in range(2):
            for n in range(2):
                ps = pm_r.next()
                for c in range(8):
                    k.mm(ps[:], memT[:, c, mt * 128:(mt + 1) * 128], wkv[:, c, D + n * 512:D + (n + 1) * 512],
                         start=(c == 0), stop=(c == 7))
                k.copy("dve", vv[:, mt, n * 512:(n + 1) * 512], ps[:])

        ysrc = dr["y"].ap().rearrange("(n p) d -> n p d", p=128)
        xsrc = x_in.ap().rearrange("(n p) d -> n p d", p=128)
        xdst = x_out.ap().rearrange("(n p) d -> n p d", p=128)
        for g in range(NG):
            for j in range(4):
                t = g * 4 + j
                yt = yt_r.next()
                k.dma("sp", yt[:], ysrc[t])
                k.dma("sp", xg[:, j, :], xsrc[t])
                pt = pt_r.next()
                for c in range(8):
                    k.tr(pt[:, c, :], yt[:, c * 128:(c + 1) * 128], cst["ident_bf"][:])
                k.copy("act", yT[:, :, j * 128:(j + 1) * 128], pt[:])
            for j in range(4):
                for n in range(2):
                    ps = pm_r.next()
                    for c in range(8):
                        k.mm(ps[:], yT[:, c, j * 128:(j + 1) * 128], wout[:, c, n * 512:(n + 1) * 512],
                             start=(c == 0), stop=(c == 7))
                    k.tt("dve", xg[:, j, n * 512:(n + 1) * 512], ps[:], xg[:, j, n * 512:(n + 1) * 512], ALU.add)
                norm_to_T(k, xg[:, j, :], nw_xa, h2T, j * 128, hn_r, st_r, junk, pt_r, cst)
            for m in range(8):
                ps = pm_r.next()
                for c in range(8):
                    k.mm(ps[:], wxq[:, c, m * 128:(m + 1) * 128], h2T[:, c, :], start=(c == 0), stop=(c == 7))
                k.copy("act" if m % 2 == 0 else "dve", qT[:, m, :], ps[:])
            for hh in range(4):
                PTs = []
                for mt in range(2):
                    ps = pm_r.next()
                    for dc in range(2):
                        k.mm(ps[:], kkT[:, 2 * hh + dc, mt * 128:(mt + 1) * 128], qT[:, 2 * hh + dc, :],
                             start=(dc == 0), stop=(dc == 1))
                    PT = PT_r.next()
                    k.act(PT[:], ps[:], AF.Exp, scale=1.0 / 16.0)
                    PTs.append(PT)
                ps = pm_r.next()
                for mt in range(2):
                    k.mm(ps[:], ones_bf[:], PTs[mt][:], start=(mt == 0), stop=(mt == 1))
                rden = rden_r.next()
                k.recip(rden[:], ps[:])
                for dc in range(2):
                    ps = pm_r.next()
                    for mt in range(2):
                        k.mm(ps[:], vv[:, mt, hh * 256 + dc * 128:hh * 256 + (dc + 1) * 128], PTs[mt][:],
                             start=(mt == 0), stop=(mt == 1))
                    k.tt("dve", oT[:, 2 * hh + dc, :], ps[:], rden[:], ALU.mult)
            for j in range(4):
                t = g * 4 + j
                for n in range(2):
                    ps = pm_r.next()
                    for c in range(8):
                        k.mm(ps[:], oT[:, c, j * 128:(j + 1) * 128], wxo[:, c, n * 512:(n + 1) * 512],
                             start=(c == 0), stop=(c == 7))
                    k.tt("dve", xg[:, j, n * 512:(n + 1) * 512], ps[:], xg[:, j, n * 512:(n + 1) * 512], ALU.add)
                k.dma("pool", xdst[t], xg[:, j, :])
        k.P.barrier()
        k.P.emit()


def phase_ffn(k, S, l, dr, cst, x_io, final_out=None):
    nc = k.nc
    GT = 2
    NG = S // (128 * GT)
    with ExitStack() as es:
        w1 = es.enter_context(nc.sbuf_tensor(k.name("w1"), [128, 8, DFF], BF16))
        w2 = es.enter_context(nc.sbuf_tensor(k.name("w2"), [128, 32, D], BF16))
        ones = es.enter_context(nc.sbuf_tensor(k.name("ones"), [128, 128], F32))
        xg = es.enter_context(nc.sbuf_tensor(k.name("xg"), [128, GT, D], F32))
        h3T = es.enter_context(nc.sbuf_tensor(k.name("h3T"), [128, 8, 128 * GT], BF16))
        uT = es.enter_context(nc.sbuf_tensor(k.name("uT"), [128, 32, 128 * GT], BF16))
        r_r = Ring(k, es, "relu", 3, [128, 128 * GT], BF16)
        hn_r = Ring(k, es, "hn", 2, [128, D], BF16)
        junk = es.enter_context(nc.sbuf_tensor(k.name("junk"), [128, D], BF16))
        st_r = Ring(k, es, "st", 2, [128, 4], F32)
        pt_r = Ring(k, es, "ptr", 2, [128, 8, 128], BF16, psum=True)
        pm_r = Ring(k, es, "pmm", 5, [128, 512], F32, psum=True)
        k.memset("dve", ones[:], 1.0)
        load_w(k, w1, dr["w_ff1"].ap()[l], 8, DFF, split=1)
        load_w(k, w2, dr["w_ff2"].ap()[l], 32, D, split=4)
        nw = make_nwfull(k, es, dr["norm_ffn_w"].ap()[l], ones)
        if final_out is not None:
            nwo = es.enter_context(nc.sbuf_tensor(k.name("nwo"), [128, D], F32))
            src = dr["norm_out_w"]
            k.dma("sp", nwo[:], bass.AP(src, 0, [[0, 128], [1, D]]))
            fo_r = Ring(k, es, "fo", 2, [128, D], F32)
            fdst = final_out.ap().rearrange("(n p) d -> n p d", p=128)
        xv = x_io.ap().rearrange("(n p) d -> n p d", p=128)
        W = 128 * GT
        for g in range(NG):
            for j in range(GT):
                t = g * GT + j
                k.dma("sp", xg[:, j, :], xv[t])
                norm_to_T(k, xg[:, j, :], nw, h3T, j * 128, hn_r, st_r, junk, pt_r, cst)
            for f in range(32):
                ps = pm_r.next()
                for c in range(8):
                    k.mm(ps[:, 0:W], w1[:, c, f * 128:(f + 1) * 128], h3T[:, c, :], start=(c == 0), stop=(c == 7))
                r = r_r.next()
                k.act(r[:], ps[:, 0:W], AF.Relu)
                k.tt("pool" if f % 2 == 0 else "dve", uT[:, f, :], r[:], r[:], ALU.mult)
            for j in range(GT):
                t = g * GT + j
                for n in range(2):
                    ps = pm_r.next()
                    for f in range(32):
                        k.mm(ps[:], uT[:, f, j * 128:(j + 1) * 128], w2[:, f, n * 512:(n + 1) * 512],
                             start=(f == 0), stop=(f == 31))
                    k.tt("dve", xg[:, j, n * 512:(n + 1) * 512], ps[:], xg[:, j, n * 512:(n + 1) * 512], ALU.add)
                if final_out is None:
                    k.dma("pool", xv[t], xg[:, j, :])
                else:
                    st = st_r.next(); fo = fo_r.next()
                    k.act(junk[:], xg[:, j, :], AF.Square, accum=st[:, 0:1])
                    k.act(st[:, 1:2], st[:, 0:1], AF.Sqrt, bias=EPS, scale=1.0 / D)
                    k.recip(st[:, 2:3], st[:, 1:2])
                    k.stt(fo[:], xg[:, j, :], st[:, 2:3], nwo[:], ALU.mult, ALU.mult)
                    k.dma("pool", fdst[t], fo[:])
        k.P.barrier()
        k.P.emit()


NA_CLASSES = {"int": (0, [-2, -1, 0, 1, 2]), "top0": (5, [0, 1, 2, 3]), "top1": (9, [-1, 0, 1, 2]),
              "bot1": (13, [-2, -1, 0, 1]), "bot0": (17, [-3, -2, -1, 0])}
NEG = -30000.0


def na_class(qt, NT):
    if qt == 0:
        return "top0"
    if qt == 1:
        return "top1"
    if qt == NT - 2:
        return "bot1"
    if qt == NT - 1:
        return "bot0"
    return "int"


def na_bias_host(rel_bias, S):
    L = rel_bias.shape[0]
    R, NT = S // 64, S // 128
    out = np.full((L, 6, 21, 128, 128), NEG, np.float32)
    rep = {"int": 2, "top0": 0, "top1": 1, "bot1": NT - 2, "bot0": NT - 1}
    j = np.arange(128)
    for cls, (base, offs) in NA_CLASSES.items():
        qt = rep[cls]
        for n, o in enumerate(offs):
            kt = qt + o
            kr, kc = 2 * kt + j // 64, j % 64
            qr, qc = 2 * qt + j // 64, j % 64
            r0 = np.clip(qr - 4, 0, R - 8)
            c0 = np.clip(qc - 8, 0, 48)
            inw = ((kr[:, None] >= r0[None, :]) & (kr[:, None] <= r0[None, :] + 7) &
                   (kc[:, None] >= c0[None, :]) & (kc[:, None] <= c0[None, :] + 15))
            drr = np.clip(kr[:, None] - qr[None, :] + 7, 0, 14)
            dcc = np.clip(kc[:, None] - qc[None, :] + 15, 0, 30)
            vals = rel_bias[:, :, drr, dcc]
            out[:, :, base + n] = np.where(inw[None, None], vals, np.float32(NEG))
    return np.ascontiguousarray(out.transpose(0, 1, 3, 2, 4).reshape(L * 6, 128, 21 * 128))


def phase_na(k, S, l, dr, cst):
    nc = k.nc
    NT = S // 128
    with ExitStack() as es:
        qT_r = Ring(k, es, "naq", 2, [64, S], BF16)
        kT_r = Ring(k, es, "nak", 2, [64, S], BF16)
        va_r = Ring(k, es, "nav", 2, [128, NT, 65], BF16)
        nb_r = Ring(k, es, "nab", 2, [128, 21 * 128], F32)
        lg_r = Ring(k, es, "nalg", 2, [128, 640], F32)
        PT_r = Ring(k, es, "naPT", 2, [128, 640], BF16)
        yo_r = Ring(k, es, "nayo", 3, [128, 64], BF16)
        rd_r = Ring(k, es, "nard", 3, [128, 1], F32)
        psA_r = Ring(k, es, "psA", 2, [128, 512], F32, psum=True)
        psB_r = Ring(k, es, "psB", 2, [128, 512], F32, psum=True)
        po_r = Ring(k, es, "pso", 2, [128, 512], F32, psum=True)
        fm = dr["fm"].ap()
        tmv = dr["tm"].ap().rearrange("(n p) c -> p n c", p=128)
        yv = dr["y"].ap().rearrange("(n p) c -> n p c", p=128)
        for h in range(6):
            qT = qT_r.next(); kT = kT_r.next(); va = va_r.next(); nb = nb_r.next()
            k.dma("sp", qT[:], fm[FM_NAQ + h * 64:FM_NAQ + (h + 1) * 64, :])
            k.dma("sp", kT[:], fm[FM_NAK + h * 64:FM_NAK + (h + 1) * 64, :])
            for t0 in range(0, NT, 8):
                k.dma("sp", va[:, t0:t0 + 8, 0:64], tmv[:, t0:t0 + 8, TM_NAV + h * 64:TM_NAV + (h + 1) * 64])
            k.memset("pool", va[:, :, 64:65], 1.0)
            k.dma("sp", nb[:], dr["nab"].ap()[l * 6 + h])
            for qt in range(NT):
                base, offs = NA_CLASSES[na_class(qt, NT)]
                n = len(offs)
                psA = psA_r.next(); psB = psB_r.next(); lg = lg_r.next(); PT = PT_r.next()
                for i, o in enumerate(offs):
                    kt = qt + o
                    dst = psA[:, i * 128:(i + 1) * 128] if i < 4 else psB[:, 0:128]
                    k.mm(dst, kT[:, kt * 128:(kt + 1) * 128], qT[:, qt * 128:(qt + 1) * 128])
                na = min(n, 4) * 128
                k.stt(lg[:, 0:na], psA[:, 0:na], 0.125, nb[:, base * 128:base * 128 + na], ALU.mult, ALU.add)
                k.act(PT[:, 0:na], lg[:, 0:na], AF.Exp)
                if n == 5:
                    k.stt(lg[:, 512:640], psB[:, 0:128], 0.125, nb[:, (base + 4) * 128:(base + 5) * 128],
                          ALU.mult, ALU.add)
                    k.act(PT[:, 512:640], lg[:, 512:640], AF.Exp)
                po = po_r.next()
                for i, o in enumerate(offs):
                    kt = qt + o
                    k.mm(po[:, 0:65], PT[:, i * 128:(i + 1) * 128], va[:, kt, :], start=(i == 0), stop=(i == n - 1))
                rd = rd_r.next(); yo = yo_r.next()
                k.recip(rd[:], po[:, 64:65])
                k.ts("dve", yo[:], po[:, 0:64], rd[:], ALU.mult)
                k.dma("pool", yv[qt][:, h * 64:(h + 1) * 64], yo[:])
        k.P.barrier()
        k.P.emit()


def bcast_rows(k, dst, src_handle, off, n):
    k.dma("sp", dst, bass.AP(src_handle, off, [[0, 128], [1, n]]))


class GatePool:
    def __init__(self, k, es, l, dr, cst):
        nc = k.nc
        self.k, self.cst, self.dr = k, cst, dr
        self.bias = es.enter_context(nc.sbuf_tensor(k.name("gbias"), [128, 40], F32))
        self.nA = es.enter_context(nc.sbuf_tensor(k.name("gnA"), [128, 12], F32))
        self.ones = es.enter_context(nc.sbuf_tensor(k.name("gones"), [128, 128], F32))
        k.memset("dve", self.bias[:], 0.0)
        k.memset("dve", self.ones[:], 1.0)
        bcast_rows(k, self.bias[:, 0:8], dr["ml_i_bias"], l * 8, 8)
        bcast_rows(k, self.bias[:, 8:16], dr["ml_f_bias"], l * 8, 8)
        bcast_rows(k, self.bias[:, 28:40], dr["gdn_dt_bias"], l * 12, 12)
        bcast_rows(k, self.nA[:], dr["gdn_a_log"], l * 12, 12)
        k.act(self.nA[:], self.nA[:], AF.Exp)
        k.ts("dve", self.nA[:], self.nA[:], -1.0, ALU.mult)
        self.gt_r = Ring(k, es, "gtt", 2, [128, 40], F32)
        self.a_r = Ring(k, es, "gta", 2, [128, 40], F32)
        self.e_r = Ring(k, es, "gte", 2, [128, 40], F32)
        self.sp_r = Ring(k, es, "gtsp", 2, [128, 40], F32)
        self.w_r = Ring(k, es, "gtw", 2, [128, 32], F32)
        self.x_r = Ring(k, es, "gtx", 2, [128, 32], F32)
        self.bt_r = Ring(k, es, "gtbt", 2, [64, 8], F32)
        self.pg_r = Ring(k, es, "gtpg", 1, [128, 512], F32, psum=True)

    def pre(self, t):
        k = self.k
        gt = self.gt_r.next(); a = self.a_r.next(); e = self.e_r.next(); sp = self.sp_r.next()
        k.dma("sp", gt[:], self.dr["gt"].ap()[t * 128:(t + 1) * 128, :])
        k.tt("dve", a[:], gt[:], self.bias[:], ALU.add)
        k.act(e[:, 8:28], a[:, 8:28], AF.Exp, scale=-1.0)
        k.act(e[:, 28:40], a[:, 28:40], AF.Exp)
        k.act(sp[:, 8:40], e[:, 8:40], AF.Ln, bias=1.0)
        return a, sp

    def mlstm(self, t, d):
        k = self.k
        a, sp = self.pre(t)
        pg = self.pg_r.next()
        lo = 8 + d * 4
        k.mm(pg[:, 0:4], self.cst["c_masks"][:, d, :], sp[:, lo:lo + 4])
        k.mm(pg[0:64, 8:12], self.ones[:, 0:64], sp[:, lo:lo + 4])
        w = self.w_r.next(); x = self.x_r.next(); bt = self.bt_r.next()
        k.ts("dve", w[:, 0:4], pg[:, 0:4], -1.0, ALU.mult)
        k.tt("dve", w[:, 4:8], pg[:, 0:4], a[:, d * 4:d * 4 + 4], ALU.add)
        k.act(x[:, 0:8], w[:, 0:8], AF.Exp)
        k.act(bt[:, 0:4], pg[0:64, 8:12], AF.Exp, scale=-1.0)
        return x, bt

    def gdn(self, t, d):
        k = self.k
        a, sp = self.pre(t)
        w = self.w_r.next(); x = self.x_r.next(); bt = self.bt_r.next()
        g = self.e_r.next()
        lo = 28 + d * 6
        k.tt("dve", g[:, 0:6], sp[:, lo:lo + 6], self.nA[:, d * 6:d * 6 + 6], ALU.mult)
        pg = self.pg_r.next()
        k.mm(pg[:, 0:6], self.cst["c_masks"][:, d, :], g[:, 0:6])
        k.mm(pg[:, 8:14], self.ones[:], g[:, 0:6])
        lb = 16 + d * 6
        k.copy("dve", w[:, 0:6], pg[:, 0:6])
        k.tt("dve", w[:, 6:12], pg[:, 0:6], sp[:, lb:lb + 6], ALU.subtract)
        k.tt("dve", w[:, 12:18], pg[:, 8:14], w[:, 0:6], ALU.subtract)
        k.ts("dve", w[:, 18:24], sp[:, lb:lb + 6], -1.0, ALU.mult)
        k.act(x[:, 0:24], w[:, 0:24], AF.Exp)
        k.act(bt[:, 0:6], pg[0:64, 8:14], AF.Exp)
        return w, x, bt


def phase_mlstm(k, S, l, dr, cst):
    nc = k.nc
    NT = S // 128
    with ExitStack() as es:
        GP = GatePool(k, es, l, dr, cst)
        qT_r = Ring(k, es, "mlq", 2, [64, 4, 128], BF16)
        kT_r = Ring(k, es, "mlk", 2, [64, 4, 128], BF16)
        ktm_r = Ring(k, es, "mlktm", 2, [128, 256], BF16)
        va_r = Ring(k, es, "mlva", 2, [128, 4, 65], BF16)
        vp_r = Ring(k, es, "mlvp", 3, [128, 65], BF16)
        pm_r = Ring(k, es, "mlpm", 3, [128, 128], BF16)
        C32 = [es.enter_context(nc.sbuf_tensor(k.name("C32"), [64, 65], F32)) for _ in range(4)]
        Cbf = [es.enter_context(nc.sbuf_tensor(k.name("Cbf"), [64, 65], BF16)) for _ in range(4)]
        tmp_r = Ring(k, es, "mltmp", 3, [64, 65], F32)
        sm_r = Ring(k, es, "mlsm", 4, [128, 4], F32)
        hb_r = Ring(k, es, "mlh", 2, [128, 256], F32)
        hf_r = Ring(k, es, "mlhf", 2, [128, 256], F32)
        ot_r = Ring(k, es, "mlo", 2, [128, 256], BF16)
        gw_r = Ring(k, es, "mlgw", 2, [128, 256], F32)
        yo_r = Ring(k, es, "mly", 2, [128, 256], BF16)
        junk = es.enter_context(nc.sbuf_tensor(k.name("mljunk"), [128, 64], F32))
        nwb = es.enter_context(nc.sbuf_tensor(k.name("mlnw"), [128, 256], F32))
        ps_r = Ring(k, es, "mlps", 2, [128, 512], F32, psum=True)
        po_r = Ring(k, es, "mlpo", 2, [128, 512], F32, psum=True)
        pc_r = Ring(k, es, "mlpc", 2, [128, 512], F32, psum=True)
        bcast_rows(k, nwb[:], dr["ml_norm_w"], l * 256, 256)
        for t_ in va_r.t:
            k.memset("dve", t_[:, :, 64:65], 1.0)
        fm = dr["fm"].ap()
        tmv = dr["tm"].ap().rearrange("(n p) c -> n p c", p=128)
        hml = dr["hml"].ap().rearrange("(n p) c -> n p c", p=128)
        yv = dr["y"].ap().rearrange("(n p) c -> n p c", p=128)
        for d in range(2):
            for h in range(4):
                k.memset("dve", C32[h][:], 0.0)
                k.memset("dve", Cbf[h][:], 0.0)
            mask = cst["c_masks"][:, d, :]
            order = range(NT) if d == 0 else range(NT - 1, -1, -1)
            for t in order:
                ex, ebt = GP.mlstm(t, d)
                qT = qT_r.next(); kT = kT_r.next(); ktm = ktm_r.next(); va = va_r.next()
                cs = slice(t * 128, (t + 1) * 128)
                k.dma("sp", qT[:], fm[FM_MLQ:FM_MLQ + 256, cs].rearrange("(h d) t -> d h t", d=64))
                k.dma("sp", kT[:], fm[FM_MLK:FM_MLK + 256, cs].rearrange("(h d) t -> d h t", d=64))
                k.dma("sp", ktm[:], tmv[t][:, TM_MLK:TM_MLK + 256])
                k.dma("sp", va[:, :, 0:64], tmv[t][:, TM_MLV:TM_MLV + 256].rearrange("p (h d) -> p h d", d=64))
                hb = hb_r.next()
                for h in range(4):
                    vp = vp_r.next()
                    k.ts("dve", vp[:], va[:, h, :], ex[:, 4 + h:5 + h], ALU.mult, 0.125, ALU.mult)
                    ps = ps_r.next()
                    k.mm(ps[:, 0:128], kT[:, h, :], qT[:, h, :])
                    pm = pm_r.next()
                    k.tt("dve", pm[:], ps[:, 0:128], mask, ALU.mult)
                    po = po_r.next()
                    k.mm(po[:, 0:65], pm[:], vp[:], start=True, stop=False)
                    k.mm(po[:, 0:65], qT[:, h, :], Cbf[h][:], start=False, stop=True)
                    pc = pc_r.next()
                    k.mm(pc[0:64, 0:65], ktm[:, h * 64:(h + 1) * 64], vp[:])
                    tmp = tmp_r.next()
                    k.tt("dve", tmp[:], pc[0:64, 0:65], C32[h][:], ALU.add)
                    k.ts("dve", C32[h][:], tmp[:], ebt[:, h:h + 1], ALU.mult)
                    k.act(Cbf[h][:], tmp[:], AF.Copy, scale=ebt[:, h:h + 1])
                    sm = sm_r.next()
                    k.act(sm[:, 3:4], po[:, 64:65], AF.Abs, scale=ex[:, h:h + 1])
                    k.ts("dve", sm[:, 0:1], sm[:, 3:4], 1.0, ALU.max)
                    k.recip(sm[:, 1:2], sm[:, 0:1])
                    k.tt("dve", sm[:, 2:3], sm[:, 1:2], ex[:, h:h + 1], ALU.mult)
                    k.ts("dve", hb[:, h * 64:(h + 1) * 64], po[:, 0:64], sm[:, 2:3], ALU.mult)
                if d == 0:
                    k.dma("pool", hml[t], hb[:])
                else:
                    hf = hf_r.next(); ot = ot_r.next(); gw = gw_r.next(); yo = yo_r.next()
                    k.dma("sp", hf[:], hml[t])
                    k.dma("sp", ot[:], tmv[t][:, TM_MLO:TM_MLO + 256])
                    k.tt("dve", hb[:], hb[:], hf[:], ALU.add)
                    k.act(gw[:], ot[:], AF.Sigmoid)
                    k.tt("dve", gw[:], gw[:], nwb[:], ALU.mult)
                    sm = sm_r.next()
                    for h in range(4):
                        k.act(junk[:], hb[:, h * 64:(h + 1) * 64], AF.Square, accum=sm[:, h:h + 1])
                    sm2 = sm_r.next()
                    k.act(sm2[:], sm[:], AF.Sqrt, bias=EPS, scale=1.0 / 64.0)
                    sm3 = sm_r.next()
                    k.recip(sm3[:], sm2[:])
                    for h in range(4):
                        k.stt(yo[:, h * 64:(h + 1) * 64], hb[:, h * 64:(h + 1) * 64], sm3[:, h:h + 1],
                              gw[:, h * 64:(h + 1) * 64], ALU.mult, ALU.mult)
                    k.dma("pool", yv[t][:, 384:640], yo[:])
        k.P.barrier()
        k.P.emit()


def phase_gdn_prep(k, S, l, dr, cst):
    nc = k.nc
    NCH = S // 512
    with ExitStack() as es:
        cwr = es.enter_context(nc.sbuf_tensor(k.name("cwr"), [5, 1152], F32))
        cw = es.enter_context(nc.sbuf_tensor(k.name("cw"), [128, 9, 5], F32))
        xin_r = Ring(k, es, "gxin", 2, [128, 516], BF16)
        acc_r = Ring(k, es, "gacc", 2, [128, 512], F32)
        s_r = Ring(k, es, "gs", 2, [128, 512], F32)
        sq_r = Ring(k, es, "gsq", 2, [128, 512], BF16)
        rt_r = Ring(k, es, "grt", 2, [128, 512], F32)
        sn_r = Ring(k, es, "gsn", 2, [128, 512], BF16)
        st_r = Ring(k, es, "gst", 2, [128, 4, 128], BF16)
        pcw = es.enter_context(nc.psum_tensor(k.name("pcw"), [128, 512], F32))
        ps_r = Ring(k, es, "gpps", 2, [128, 512], F32, psum=True)
        pt_r = Ring(k, es, "gppt", 2, [128, 4, 128], BF16, psum=True)
        k.dma("sp", cwr[:], dr["gdn_conv_w"].ap()[l])
        for g in range(9):
            k.tr(pcw[:, g * 8:g * 8 + 5], cwr[0:5, g * 128:(g + 1) * 128], cst["ident_f"][0:5, 0:5])
        for g in range(9):
            k.copy("dve", cw[:, g, :], pcw[:, g * 8:g * 8 + 5])
        fm = dr["fm"].ap()
        dsts = {0: dr["gqT"], 1: dr["gkT"]}
        tms = {1: dr["gk_tm"], 2: dr["gv_tm"]}
        for g in range(9):
            kind, gi = g // 3, g % 3
            for c in range(NCH):
                xin = xin_r.next(); acc = acc_r.next(); s = s_r.next(); sn = sn_r.next()
                lo, hi = max(c * 512 - 2, 0), min(c * 512 + 514, S)
                off = lo - (c * 512 - 2)
                if c == 0:
                    k.memset("pool", xin[:, 0:2], 0.0)
                if c == NCH - 1:
                    k.memset("pool", xin[:, 514:516], 0.0)
                k.dma("sp", xin[:, off:off + hi - lo], fm[FM_GDQ + g * 128:FM_GDQ + (g + 1) * 128, lo:hi])
                k.ts("dve", acc[:], xin[:, 0:512], cw[:, g, 0:1], ALU.mult)
                for kk in range(1, 5):
                    k.stt(acc[:], xin[:, kk:kk + 512], cw[:, g, kk:kk + 1], acc[:], ALU.mult, ALU.add)
                k.act(s[:], acc[:], AF.Silu)
                if kind < 2:
                    sq = sq_r.next(); rt = rt_r.next(); ps = ps_r.next()
                    k.tt("pool", sq[:], s[:], s[:], ALU.mult)
                    k.mm(ps[:], cst["blk2_bf"][:], sq[:])
                    k.act(rt[:], ps[:], AF.Sqrt, bias=EPS)
                    k.recip(rt[:], rt[:])
                    if kind == 0:
                        k.stt(sn[:], s[:], 0.125, rt[:], ALU.mult, ALU.mult)
                    else:
                        k.tt("dve", sn[:], s[:], rt[:], ALU.mult)
                    k.dma("pool", dsts[kind].ap()[gi * 128:(gi + 1) * 128, c * 512:(c + 1) * 512], sn[:])
                else:
                    k.copy("pool", sn[:], s[:])
                if kind >= 1:
                    pt = pt_r.next(); st = st_r.next()
                    for j in range(4):
                        k.tr(pt[:, j, :], sn[:, j * 128:(j + 1) * 128], cst["ident_bf"][:])
                    k.copy("act", st[:], pt[:])
                    dv = tms[kind].ap().rearrange("(n p) c -> p n c", p=128)
                    k.dma("pool", dv[:, c * 4:(c + 1) * 4, gi * 128:(gi + 1) * 128], st[:])
        k.P.barrier()
        k.P.emit()


def phase_gdn(k, S, l, dr, cst, stage=9):
    nc = k.nc
    NT = S // 128
    M = cst["c_masks"]
    with ExitStack() as es:
        GP = GatePool(k, es, l, dr, cst)
        qT_r = Ring(k, es, "gdq", 2, [64, 6, 128], BF16)
        kT_r = Ring(k, es, "gdk", 2, [64, 6, 128], BF16)
        ktm_r = Ring(k, es, "gdktm", 2, [128, 384], BF16)
        vtm_r = Ring(k, es, "gdvtm", 2, [128, 384], BF16)
        dg_r = Ring(k, es, "gddg", 4, [128, 128], F32)
        xp_r = Ring(k, es, "gdxp", 6, [128, 128], F32)
        A_r = Ring(k, es, "gdA", 4, [128, 128], F32)
        B_r = Ring(k, es, "gdB", 4, [128, 128], F32)
        N_r = Ring(k, es, "gdN", 4, [128, 128], F32)
        at_r = Ring(k, es, "gdat", 2, [128, 128], BF16)
        sc_r = Ring(k, es, "gdsc", 2, [128, 2, 64], F32)
        ks_r = Ring(k, es, "gdks", 2, [128, 64], BF16)
        u_r = Ring(k, es, "gdu", 2, [128, 64], F32)
        wk_r = Ring(k, es, "gdwk", 2, [64, 128], BF16)
        vn_r = Ring(k, es, "gdvn", 2, [128, 64], BF16)
        o2_r = Ring(k, es, "gdo2", 2, [128, 64], F32)
        ob_r = Ring(k, es, "gdob", 2, [128, 384], F32)
        of_r = Ring(k, es, "gdof", 2, [128, 384], F32)
        z_r = Ring(k, es, "gdz", 2, [128, 384], BF16)
        gz_r = Ring(k, es, "gdgz", 2, [128, 384], F32)
        yo_r = Ring(k, es, "gdyo", 2, [128, 384], BF16)
        sm_r = Ring(k, es, "gdsm", 4, [128, 8], F32)
        junk = es.enter_context(nc.sbuf_tensor(k.name("gdjunk"), [128, 64], F32))
        nwb = es.enter_context(nc.sbuf_tensor(k.name("gdnw"), [128, 384], F32))
        S32 = [es.enter_context(nc.sbuf_tensor(k.name("S32"), [64, 64], F32)) for _ in range(6)]
        Sbf = [es.enter_context(nc.sbuf_tensor(k.name("Sbf"), [64, 64], BF16)) for _ in range(6)]
        pA_r = Ring(k, es, "gdpA", 1, [128, 512], F32, psum=True)
        pB_r = Ring(k, es, "gdpB", 1, [128, 512], F32, psum=True)
        pc_r = Ring(k, es, "gdpc", 3, [128, 512], F32, psum=True)
        pu_r = Ring(k, es, "gdpu", 1, [128, 512], F32, psum=True)
        pv_r = Ring(k, es, "gdpv", 1, [128, 512], F32, psum=True)
        bcast_rows(k, nwb[:], dr["gdn_norm_w"], l * 384, 384)
        idf, idb, ones = cst["ident_f"], cst["ident_bf"], GP.ones
        tmv = dr["tm"].ap().rearrange("(n p) c -> n p c", p=128)
        ktv = dr["gk_tm"].ap().rearrange("(n p) c -> n p c", p=128)
        vtv = dr["gv_tm"].ap().rearrange("(n p) c -> n p c", p=128)
        ogd = dr["ogd"].ap().rearrange("(n p) c -> n p c", p=128)
        yv = dr["y"].ap().rearrange("(n p) c -> n p c", p=128)
        for d in range(2):
            mA, mB, mC = (M[:, 2, :], M[:, 4, :], M[:, 6, :]) if d == 0 else (M[:, 3, :], M[:, 5, :], M[:, 7, :])
            for h in range(6):
                k.memset("dve", S32[h][:], 0.0)
                k.memset("dve", Sbf[h][:], 0.0)
            order = range(NT) if d == 0 else range(NT - 1, -1, -1)
            for t in order:
                raw, ex, egl = GP.gdn(t, d)
                qT = qT_r.next(); kT = kT_r.next(); ktm = ktm_r.next(); vtm = vtm_r.next()
                cs = slice(t * 128, (t + 1) * 128)
                k.dma("sp", qT[:], dr["gqT"].ap()[:, cs].rearrange("(h d) t -> d h t", d=64))
                k.dma("sp", kT[:], dr["gkT"].ap()[:, cs].rearrange("(h d) t -> d h t", d=64))
                k.dma("sp", ktm[:], ktv[t])
                k.dma("sp", vtm[:], vtv[t])
                ob = ob_r.next()
                if stage < 9:
                    k.memset("dve", ob[:], 0.0)
                for h in range(6):
                    if stage < 1:
                        continue
                    hs = slice(h * 64, (h + 1) * 64)
                    pA = pA_r.next(); pB = pB_r.next()
                    k.mm(pA[:, 0:128], kT[:, h, :], kT[:, h, :])
                    k.mm(pA[:, 128:256], kT[:, h, :], qT[:, h, :])
                    dg1 = dg_r.next(); dg2 = dg_r.next()
                    k.ts("dve", dg1[:], idf[:], raw[:, h:h + 1], ALU.mult)
                    k.ts("dve", dg2[:], idf[:], raw[:, 6 + h:7 + h], ALU.mult)
                    k.mm(pB[:, 0:128], ones[:], dg1[:])
                    k.mm(pB[:, 128:256], ones[:], dg2[:])
                    xa = xp_r.next(); xb = xp_r.next(); xc = xp_r.next()
                    k.stt(xa[:], pB[:, 0:128], raw[:, 6 + h:7 + h], mA, ALU.subtract, ALU.add)
                    k.stt(xb[:], pB[:, 128:256], raw[:, h:h + 1], mB, ALU.subtract, ALU.add)
                    k.stt(xc[:], pB[:, 0:128], raw[:, h:h + 1], mC, ALU.subtract, ALU.add)
                    k.act(xa[:], xa[:], AF.Exp, scale=-1.0)
                    k.act(xb[:], xb[:], AF.Exp)
                    k.act(xc[:], xc[:], AF.Exp)
                    A = A_r.next(); B = B_r.next(); at = at_r.next(); N = N_r.next()
                    k.tt("dve", A[:], pA[:, 0:128], xa[:], ALU.mult)
                    k.tt("dve", B[:], pA[:, 0:128], xb[:], ALU.mult)
                    k.tt("dve", at[:], pA[:, 128:256], xc[:], ALU.mult)
                    k.tt("dve", N[:], idf[:], B[:], ALU.subtract)
                    if stage < 2:
                        continue
                    for lv in range(1, 7):
                        pc = pc_r.next()
                        A2 = A_r.next()
                        k.mm(pc[:, 0:128], B[:], A[:])
                        if lv < 6:
                            B2 = B_r.next()
                            k.mm(pc[:, 128:256], A[:], B[:])
                        k.copy("act", A2[:], pc[:, 0:128])
                        if lv < 6:
                            k.copy("dve", B2[:], pc[:, 128:256])
                        k.mm(pc[:, 256:384], A2[:], N[:])
                        N2 = N_r.next()
                        k.tt("dve", N2[:], pc[:, 256:384], N[:], ALU.add)
                        A, N = A2, N2
                        if lv < 6:
                            B = B2
                    if stage < 3:
                        continue
                    sc = sc_r.next()
                    k.ts("dve", sc[:, 0, :], vtm[:, hs], ex[:, 18 + h:19 + h], ALU.mult)
                    k.ts("dve", sc[:, 1, :], ktm[:, hs], ex[:, 6 + h:7 + h], ALU.mult)
                    ks = ks_r.next()
                    k.ts("dve", ks[:], ktm[:, hs], ex[:, 12 + h:13 + h], ALU.mult)
                    pu = pu_r.next()
                    k.mm(pu[:, 0:64], N[:], sc[:, 0, :])
                    k.mm(pu[0:64, 64:192], sc[:, 1, :], N[:])
                    u = u_r.next(); wk = wk_r.next()
                    k.copy("act", u[:], pu[:, 0:64])
                    k.copy("dve", wk[:], pu[0:64, 64:192])
                    pv = pv_r.next()
                    k.mm(pv[:, 0:64], wk[:], Sbf[h][:])
                    vn = vn_r.next()
                    k.tt("dve", vn[:], u[:], pv[:, 0:64], ALU.subtract)
                    k.mm(pv[:, 64:128], qT[:, h, :], Sbf[h][:])
                    k.mm(pv[:, 128:192], at[:], vn[:])
                    k.mm(pv[0:64, 192:256], ks[:], vn[:])
                    o2 = o2_r.next()
                    k.copy("act", o2[:], pv[:, 128:192])
                    k.stt(ob[:, hs], pv[:, 64:128], ex[:, h:h + 1], o2[:], ALU.mult, ALU.add)
                    k.stt(S32[h][:], S32[h][:], egl[:, h:h + 1], pv[0:64, 192:256], ALU.mult, ALU.add)
                    k.copy("act", Sbf[h][:], S32[h][:])
                if d == 0:
                    k.dma("pool", ogd[t], ob[:])
                else:
                    of = of_r.next(); z = z_r.next(); gz = gz_r.next(); yo = yo_r.next()
                    k.dma("sp", of[:], ogd[t])
                    k.dma("sp", z[:], tmv[t][:, TM_GDZ:TM_GDZ + 384])
                    k.tt("dve", ob[:], ob[:], of[:], ALU.add)
                    k.act(gz[:], z[:], AF.Silu)
                    k.tt("dve", gz[:], gz[:], nwb[:], ALU.mult)
                    sm = sm_r.next()
                    for h in range(6):
                        k.act(junk[:], ob[:, h * 64:(h + 1) * 64], AF.Square, accum=sm[:, h:h + 1])
                    sm2 = sm_r.next()
                    k.act(sm2[:, 0:6], sm[:, 0:6], AF.Sqrt, bias=EPS, scale=1.0 / 64.0)
                    sm3 = sm_r.next()
                    k.recip(sm3[:, 0:6], sm2[:, 0:6])
                    for h in range(6):
                        hs = slice(h * 64, (h + 1) * 64)
                        k.stt(yo[:, hs], ob[:, hs], sm3[:, h:h + 1], gz[:, hs], ALU.mult, ALU.mult)
                    k.dma("pool", yv[t][:, 640:1024], yo[:])
        k.P.barrier()
        k.P.emit()


S_FULL = 8192


def build_program(S):
    nc = bass.Bass("TRN2", target_bir_lowering=False)
    dr = declare_dram(nc, S)
    with ExitStack() as es:
        k = K(nc, es)
        cst = setup_consts(k, es, dr)
        for l in range(DEPTH):
            dr["x_cur"] = dr["x"] if l == 0 else dr["xs"]
            phase_inproj(k, S, l, dr, cst)
            phase_na(k, S, l, dr, cst)
            phase_mlstm(k, S, l, dr, cst)
            phase_gdn_prep(k, S, l, dr, cst)
            phase_gdn(k, S, l, dr, cst)
            phase_mix_xattn(k, S, l, dr, cst, dr["x_cur"], dr["xs"])
            phase_ffn(k, S, l, dr, cst, dr["xs"], final_out=(dr["out"] if l == DEPTH - 1 else None))
    return nc


def kernel(**inputs):
    x = np.asarray(inputs["x"], dtype=np.float32)
    mem = np.asarray(inputs["mem"], dtype=np.float32)
    B, S, _ = x.shape
    nc = build_program(S)
    shared = {n: np.ascontiguousarray(np.asarray(inputs[n], dtype=np.float32)) for n in WEIGHT_SHAPES}
    shared.update(make_consts_host())
    shared["nab"] = na_bias_host(np.asarray(inputs["na_rel_bias"], dtype=np.float32), S)
    in_maps = []
    for b in range(B):
        m = dict(shared)
        m["x"] = np.ascontiguousarray(x[b])
        m["mem"] = np.ascontiguousarray(mem[b])
        in_maps.append(m)
    res = run_bass_kernel_spmd(nc, in_maps, core_ids=list(range(B)))
    return np.stack([np.asarray(r["out"], dtype=np.float32) for r in res.results], axis=0)
```

```python
import numpy as np
from contextlib import ExitStack

import concourse.bass as bass
import concourse.mybir as mybir
from concourse.bass_utils import run_bass_kernel_spmd

F32 = mybir.dt.float32
BF16 = mybir.dt.bfloat16
AF = mybir.ActivationFunctionType
ALU = mybir.AluOpType
AX = mybir.AxisListType

ENGS = ("pe", "dve", "act", "pool", "sp")
SEM_CAP = 30000
N_DMA_SEM = 12


def _prod(v):
    r = 1
    for a in v:
        r *= int(a)
    return r


def region(ap):
    t = ap.tensor
    shape = [int(s) for s in t.shape]
    rowlen = _prod(shape[1:])
    off = int(ap.offset)
    p0 = off // rowlen
    c0 = off % rowlen
    p1, c1 = p0, c0
    for step, cnt in ap.ap:
        step, cnt = int(step), int(cnt)
        if cnt <= 1 or step == 0:
            continue
        ext = step * (cnt - 1)
        if step % rowlen == 0:
            p1 += ext // rowlen
        else:
            c1 += ext
    if c1 >= rowlen:
        tot = off + (p1 - p0) * rowlen + (c1 - c0)
        p1 = tot // rowlen
        c0, c1 = 0, rowlen - 1
    if type(t).__name__ == "PSumTensorHandle":
        return (t.name, 0, 127, 0, rowlen - 1)
    return (t.name, p0, p1, c0, c1)


def _overlap(a, b):
    return not (a[2] < b[1] or b[2] < a[1] or a[4] < b[3] or b[4] < a[3])


def _contains(a, b):
    return a[1] <= b[1] and a[2] >= b[2] and a[3] <= b[3] and a[4] >= b[4]


class Op:
    __slots__ = ("eng", "fn", "dma", "seq", "deps", "signal", "sig_idx", "dsem", "dval",
                 "clock", "prewait")


class Prog:
    def __init__(self, nc, es):
        self.nc = nc
        self.es = es
        self.ops = []
        self.recs = {}
        self.nseq = {e: 0 for e in ENGS}
        self.clock = {e: {x: 0 for x in ENGS} for e in ENGS}
        self.known_dma = {e: set() for e in ENGS}
        self.last_compute = {e: None for e in ENGS}
        self.pending_dma = []
        self.esems = {e: [] for e in ENGS}
        self.nsig = {e: 0 for e in ENGS}
        self.dsems = {}
        for q in ("sp", "act", "pool"):
            self.dsems[q] = [[es.enter_context(nc.semaphore(f"d_{q}_{i}")), 0, None]
                             for i in range(N_DMA_SEM)]
        self.dma_rr = {q: 0 for q in self.dsems}
        self.emitted = 0

    def _need(self, op, dep):
        if dep is op:
            return
        c = op.eng
        if dep.dma:
            if dep in self.known_dma[c]:
                return
            self.known_dma[c].add(dep)
            op.deps.append(dep)
            clk = dep.clock
        else:
            if self.clock[c][dep.eng] >= dep.seq:
                return
            op.deps.append(dep)
            dep.signal = True
            clk = dict(dep.clock)
            clk[dep.eng] = max(clk[dep.eng], dep.seq)
        mine = self.clock[c]
        for e in ENGS:
            if clk[e] > mine[e]:
                mine[e] = clk[e]

    def add(self, eng, fn, reads=(), writes=(), dma=False):
        op = Op()
        op.eng, op.fn, op.dma = eng, fn, dma
        op.deps, op.signal, op.sig_idx = [], False, None
        op.dsem = op.dval = None
        op.prewait = None
        self.nseq[eng] += 1
        op.seq = self.nseq[eng]
        if dma:
            q = eng
            i = self.dma_rr[q]
            self.dma_rr[q] = (i + 1) % N_DMA_SEM
            slot = self.dsems[q][i]
            if slot[2] is not None:
                self._need(op, slot[2])
            slot[1] += 16
            slot[2] = op
            op.dsem, op.dval = slot[0], slot[1]
        accs = ([(region(a), type(a.tensor).__name__ == "PSumTensorHandle") for a in reads] +
                [(region(a), True) for a in writes])
        for box, is_w in accs:
            for rbox, rop, rw in self.recs.get(box[0], ()):
                if _overlap(box, rbox) and (is_w or rw):
                    same = (rop.eng == eng and not rop.dma and not dma)
                    if same and eng == "pe":
                        pass
                    else:
                        self._need(op, rop)
        for box, is_w in accs:
            keep = []
            for rec in self.recs.get(box[0], ()):
                rbox, rop, rw = rec
                if is_w and _contains(box, rbox) and rop is not op:
                    continue
                if (not is_w) and (not rw) and rbox == box and rop.eng == eng and not rop.dma and not dma:
                    continue
                keep.append(rec)
            keep.append((box, op, is_w))
            self.recs[box[0]] = keep
        op.clock = dict(self.clock[eng])
        self.ops.append(op)
        if dma:
            self.pending_dma.append(op)
        else:
            self.last_compute[eng] = op
        return op

    def barrier(self):
        for e in ENGS:
            op = Op()
            op.eng, op.fn, op.dma = e, None, False
            op.deps, op.signal, op.sig_idx = [], False, None
            op.dsem = op.dval = None
            op.prewait = None
            self.nseq[e] += 1
            op.seq = self.nseq[e]
            for o in ENGS:
                lc = self.last_compute[o]
                if lc is not None:
                    self._need(op, lc)
            for q in self.dsems:
                for slot in self.dsems[q]:
                    if slot[2] is not None:
                        self._need(op, slot[2])
            op.clock = dict(self.clock[e])
            self.ops.append(op)
        self.pending_dma = []
        self.recs = {}

    def _sem_for(self, eng, idx):
        k = (idx - 1) // SEM_CAP
        while len(self.esems[eng]) <= k:
            self.esems[eng].append(self.es.enter_context(
                self.nc.semaphore(f"s_{eng}_{len(self.esems[eng])}")))
        return self.esems[eng][k], idx - k * SEM_CAP

    def emit(self):
        ops = self.ops[self.emitted:]
        self.emitted = len(self.ops)
        for op in ops:
            if op.signal and not op.dma:
                self.nsig[op.eng] += 1
                op.sig_idx = self.nsig[op.eng]
        per = {e: [o for o in ops if o.eng == e] for e in ENGS}
        prog = self

        def run(e, h):
            for op in per[e]:
                for d in op.deps:
                    if d.dma:
                        h.wait_ge(d.dsem, d.dval)
                    else:
                        s, v = prog._sem_for(d.eng, d.sig_idx)
                        h.wait_ge(s, v)
                if op.fn is None:
                    if op.signal:
                        s, v = prog._sem_for(e, op.sig_idx)
                        h.sem_inc(s, 1)
                    continue
                inst = op.fn(h)
                if op.dma:
                    inst.then_inc(op.dsem, 16)
                elif op.signal:
                    s, v = prog._sem_for(e, op.sig_idx)
                    inst.then_inc(s, 1)

        for op in ops:
            if op.signal and not op.dma:
                self._sem_for(op.eng, op.sig_idx)
        with self.nc.Block() as block:
            @block.tensor
            def _(h):
                run("pe", h)

            @block.vector
            def _(h):
                run("dve", h)

            @block.scalar
            def _(h):
                run("act", h)

            @block.gpsimd
            def _(h):
                run("pool", h)

            @block.sync
            def _(h):
                run("sp", h)


class K:
    def __init__(self, nc, es):
        self.nc = nc
        self.P = Prog(nc, es)
        self.uid = 0

    def name(self, base):
        self.uid += 1
        return f"{base}_{self.uid}"

    def mm(self, out, lhsT, rhs, start=True, stop=True):
        self.P.add("pe", lambda h: h.matmul(out, lhsT, rhs, start=start, stop=stop),
                   reads=[lhsT, rhs], writes=[out])

    def tr(self, out, in_, ident):
        self.P.add("pe", lambda h: h.transpose(out, in_, ident), reads=[in_, ident], writes=[out])

    def act(self, out, in_, func, bias=None, scale=None, accum=None):
        kw = {}
        rd = [in_]
        wr = [out]
        if bias is not None:
            kw["bias"] = bias
            if not isinstance(bias, (int, float)):
                rd.append(bias)
        if scale is not None:
            kw["scale"] = scale
            if not isinstance(scale, (int, float)):
                rd.append(scale)
        if accum is not None:
            kw["accum_out"] = accum
            wr.append(accum)
        self.P.add("act", lambda h: h.activation(out, in_, func, **kw), reads=rd, writes=wr)

    def ts(self, eng, out, in0, s1, op0, s2=None, op1=None, accum=None):
        rd = [in0]
        if not isinstance(s1, (int, float)):
            rd.append(s1)
        if s2 is not None and not isinstance(s2, (int, float)):
            rd.append(s2)
        wr = [out]
        kw = {}
        if op1 is not None:
            kw["op1"] = op1
        if accum is not None:
            kw["accum_out"] = accum
            wr.append(accum)
        self.P.add(eng, lambda h: h.tensor_scalar(out, in0, s1, s2, op0, **kw), reads=rd, writes=wr)

    def tt(self, eng, out, in0, in1, op):
        self.P.add(eng, lambda h: h.tensor_tensor(out, in0, in1, op), reads=[in0, in1], writes=[out])

    def stt(self, out, in0, scalar, in1, op0, op1):
        rd = [in0, in1]
        if not isinstance(scalar, (int, float)):
            rd.append(scalar)
        self.P.add("dve", lambda h: h.scalar_tensor_tensor(out, in0, scalar, in1, op0, op1),
                   reads=rd, writes=[out])

    def copy(self, eng, out, in_):
        if eng == "act":
            self.P.add("act", lambda h: h.copy(out, in_), reads=[in_], writes=[out])
        else:
            self.P.add(eng, lambda h: h.tensor_copy(out, in_), reads=[in_], writes=[out])

    def memset(self, eng, out, val):
        self.P.add(eng, lambda h: h.memset(out, val), reads=[], writes=[out])

    def recip(self, out, in_):
        self.P.add("dve", lambda h: h.reciprocal(out, in_), reads=[in_], writes=[out])

    def scan(self, out, d0, d1, init, op0, op1):
        self.P.add("dve", lambda h: h.tensor_tensor_scan(out, d0, d1, init, op0, op1),
                   reads=[d0, d1], writes=[out])

    def dma(self, q, out, in_, slow=False):
        if slow:
            self.P.add(q, lambda h: h.dma_start(out=out, in_=in_, allow_slow_non_contiguous=True),
                       reads=[in_], writes=[out], dma=True)
        else:
            self.P.add(q, lambda h: h.dma_start(out=out, in_=in_), reads=[in_], writes=[out], dma=True)


D = 1024
DEPTH = 2
P_IN = 3752
NMEM = 256
DFF = 4096
EPS = 1e-6
WIN_BLOCKS = [(0, 768, 0), (1152, 1664, 768), (2192, 3344, 1280), (2176, 2192, 2432),
              (3728, 3752, 2448), (768, 1152, 2472), (1664, 2176, 2856), (3344, 3728, 3368)]
FM_ROWS = 2432
TM_COLS = 1536
FM_NAQ, FM_NAK, FM_MLQ, FM_MLK, FM_GDQ, FM_GDK, FM_GDV = 0, 384, 768, 1024, 1280, 1664, 2048
TM_NAV, TM_MLV, TM_MLO, TM_GDZ, TM_MLK = 0, 384, 640, 896, 1280


class Ring:
    def __init__(self, k, es, name, n, shape, dtype, psum=False):
        mk = k.nc.psum_tensor if psum else k.nc.sbuf_tensor
        self.t = [es.enter_context(mk(k.name(name), shape, dtype)) for _ in range(n)]
        self.i = 0

    def next(self):
        t = self.t[self.i]
        self.i = (self.i + 1) % len(self.t)
        return t


def rmsnorm_rows(k, xt, hn, junk, st, eng_scale="dve"):
    k.act(junk[:], xt[:], AF.Square, accum=st[:, 0:1])
    k.act(st[:, 1:2], st[:, 0:1], AF.Sqrt, bias=EPS, scale=1.0 / D)
    k.recip(st[:, 2:3], st[:, 1:2])
    k.ts(eng_scale, hn[:], xt[:], st[:, 2:3], ALU.mult)


def phase_inproj(k, S, l, dr, cst):
    nc = k.nc
    NG = S // 512
    with ExitStack() as es:
        w = es.enter_context(nc.sbuf_tensor(k.name("win"), [128, 8, P_IN], BF16))
        nw = es.enter_context(nc.sbuf_tensor(k.name("nw"), [128, 8], F32))
        wfull = es.enter_context(nc.sbuf_tensor(k.name("wfull"), [128, 8, 128], BF16))
        ones = es.enter_context(nc.sbuf_tensor(k.name("ones"), [128, 128], F32))
        xt_r = Ring(k, es, "xt", 2, [128, D], F32)
        hn_r = Ring(k, es, "hn", 2, [128, D], BF16)
        junk = es.enter_context(nc.sbuf_tensor(k.name("junk"), [128, D], BF16))
        st_r = Ring(k, es, "st", 2, [128, 4], F32)
        hT_r = Ring(k, es, "hT", 2, [128, 8, 512], BF16)
        fmst_r = Ring(k, es, "fmst", 4, [128, 512], BF16)
        gst_r = Ring(k, es, "gst", 2, [12, 512], F32)
        tmst_r = Ring(k, es, "tmst", 2, [128, TM_COLS], BF16)
        gtst_r = Ring(k, es, "gtst", 2, [128, 40], F32)
        pt_r = Ring(k, es, "ptr", 2, [128, 8, 128], BF16, psum=True)
        pm_r = Ring(k, es, "pmm", 4, [128, 512], F32, psum=True)

        wsrc = dr["w_in"].ap()[l].rearrange("(c p) n -> p c n", p=128)
        for (a, b, m) in WIN_BLOCKS:
            for c0 in range(0, 8, 4):
                k.dma("pool", w[:, c0:c0 + 4, m:m + (b - a)], wsrc[:, c0:c0 + 4, a:b])
        k.dma("sp", nw[:], dr["norm_mix_w"].ap()[l].rearrange("(c p) -> p c", p=128), slow=True)
        k.memset("dve", ones[:], 1.0)
        for c in range(8):
            k.ts("dve", wfull[:, c, :], ones[:], nw[:, c:c + 1], ALU.mult)

        xsrc = dr["x_cur"].ap().rearrange("(n p) d -> n p d", p=128)
        fm = dr["fm"].ap()
        tm = dr["tm"].ap().rearrange("(n p) c -> n p c", p=128)
        ev = 0
        for g in range(NG):
            hT = hT_r.next()
            for j in range(4):
                t = g * 4 + j
                xt = xt_r.next(); hn = hn_r.next(); st = st_r.next()
                k.dma("sp", xt[:], xsrc[t])
                rmsnorm_rows(k, xt, hn, junk, st)
                pt = pt_r.next()
                for c in range(8):
                    k.tr(pt[:, c, :], hn[:, c * 128:(c + 1) * 128], cst["ident_bf"][:])
                k.tt("dve", hT[:, :, j * 128:(j + 1) * 128], pt[:], wfull[:], ALU.mult)
            for m in range(19):
                ps = pm_r.next()
                for c in range(8):
                    k.mm(ps[:], w[:, c, m * 128:(m + 1) * 128], hT[:, c, :], start=(c == 0), stop=(c == 7))
                stg = fmst_r.next()
                k.copy("act" if ev % 2 == 0 else "dve", stg[:], ps[:])
                ev += 1
                k.dma("pool", fm[m * 128:(m + 1) * 128, g * 512:(g + 1) * 512], stg[:])
            for j in range(4):
                t = g * 4 + j
                stg = tmst_r.next()
                for (c0, n, o) in [(2472, 512, 0), (2984, 512, 512), (3496, 256, 1024), (1024, 256, 1280)]:
                    ps = pm_r.next()
                    for c in range(8):
                        k.mm(ps[:, 0:n], hT[:, c, j * 128:(j + 1) * 128], w[:, c, c0:c0 + n],
                             start=(c == 0), stop=(c == 7))
                    k.copy("act" if ev % 2 == 0 else "dve", stg[:, o:o + n], ps[:, 0:n])
                    ev += 1
                k.dma("pool", tm[t], stg[:])
                ps = pm_r.next()
                for c in range(8):
                    k.mm(ps[:, 0:40], hT[:, c, j * 128:(j + 1) * 128], w[:, c, 2432:2472],
                         start=(c == 0), stop=(c == 7))
                gs = gtst_r.next()
                k.copy("dve", gs[:], ps[:, 0:40])
                k.dma("pool", dr["gt"].ap()[t * 128:(t + 1) * 128, :], gs[:])
        k.P.barrier()
        k.P.emit()


def make_consts_host():
    c = {}
    c["c_ident"] = np.eye(128, dtype=np.float32)
    p = np.arange(128)[:, None]
    f = np.arange(128)[None, :]
    big = np.float32(30000.0)
    z = np.float32(0.0)
    c["c_masks"] = np.stack([
        (p <= f).astype(np.float32), (p >= f).astype(np.float32),
        np.where(p > f, z, big), np.where(f > p, z, big),
        np.where(f > p, z, -big), np.where(p > f, z, -big),
        np.where(f >= p, z, -big), np.where(f <= p, z, -big)], axis=1).astype(np.float32)
    es = np.zeros((128, 2, 64), np.float32)
    es[127, 0, :] = 1.0
    es[0, 1, :] = 1.0
    c["c_esel"] = es
    c["c_blk2"] = np.kron(np.eye(2, dtype=np.float32), np.ones((64, 64), np.float32))
    return c


def setup_consts(k, es, dr):
    nc = k.nc
    cst = {}
    idf = es.enter_context(nc.sbuf_tensor("ident_f", [128, 128], F32))
    idb = es.enter_context(nc.sbuf_tensor("ident_bf", [128, 128], BF16))
    k.dma("sp", idf[:], dr["c_ident"].ap())
    k.copy("dve", idb[:], idf[:])
    cst["ident_f"], cst["ident_bf"] = idf, idb
    for nm, shp in [("c_masks", [128, 8, 128]), ("c_esel", [128, 2, 64]), ("c_blk2", [128, 128])]:
        t = es.enter_context(nc.sbuf_tensor(nm + "_sb", shp, F32))
        k.dma("sp", t[:], dr[nm].ap())
        cst[nm] = t
    blkb = es.enter_context(nc.sbuf_tensor("blk2_bf", [128, 128], BF16))
    k.copy("dve", blkb[:], cst["c_blk2"][:])
    cst["blk2_bf"] = blkb
    k.P.barrier()
    k.P.emit()
    return cst


WEIGHT_SHAPES = {
    "norm_mix_w": (DEPTH, D), "w_in": (DEPTH, D, P_IN), "ml_i_bias": (DEPTH, 2, 4), "ml_f_bias": (DEPTH, 2, 4),
    "ml_norm_w": (DEPTH, 256), "gdn_conv_w": (DEPTH, 5, 1152), "gdn_a_log": (DEPTH, 2, 6),
    "gdn_dt_bias": (DEPTH, 2, 6), "gdn_norm_w": (DEPTH, 384), "w_out": (DEPTH, D, D),
    "norm_xa_w": (DEPTH, D), "norm_mem_w": (DEPTH, D), "w_xq": (DEPTH, D, D), "w_xkv": (DEPTH, D, 2 * D),
    "w_xo": (DEPTH, D, D), "norm_ffn_w": (DEPTH, D), "w_ff1": (DEPTH, D, DFF), "w_ff2": (DEPTH, DFF, D),
    "norm_out_w": (D,),
}


def declare_dram(nc, S, debug=(), ext_in=()):
    dr = {}
    dr["x"] = nc.dram_tensor("x", [S, D], F32, kind="ExternalInput")
    dr["mem"] = nc.dram_tensor("mem", [NMEM, D], F32, kind="ExternalInput")
    for n, shp in WEIGHT_SHAPES.items():
        dr[n] = nc.dram_tensor(n, list(shp), F32, kind="ExternalInput")
    for n, a in make_consts_host().items():
        dr[n] = nc.dram_tensor(n, list(a.shape), F32, kind="ExternalInput")
    dr["nab"] = nc.dram_tensor("nab", [DEPTH * 6, 128, 21 * 128], F32, kind="ExternalInput")

    def scratch(name, shape, dt):
        kind = "ExternalOutput" if name in debug else ("ExternalInput" if name in ext_in else "Internal")
        dr[name] = nc.dram_tensor(name, shape, dt, kind=kind)

    scratch("fm", [FM_ROWS, S], BF16)
    scratch("tm", [S, TM_COLS], BF16)
    scratch("gt", [S, 40], F32)
    scratch("hml", [S, 256], F32)
    scratch("ogd", [S, 384], F32)
    scratch("gqT", [384, S], BF16)
    scratch("gkT", [384, S], BF16)
    scratch("gk_tm", [S, 384], BF16)
    scratch("gv_tm", [S, 384], BF16)
    scratch("y", [S, D], BF16)
    scratch("xs", [S, D], F32)
    dr["out"] = nc.dram_tensor("out", [S, D], F32, kind="ExternalOutput")
    return dr


def load_w(k, dst, src2d, kc, ncols, split=4):
    v = src2d.rearrange("(c p) n -> p c n", p=128)
    for c0 in range(0, kc, split):
        k.dma("pool", dst[:, c0:c0 + split, :], v[:, c0:c0 + split, :])


def norm_to_T(k, xt, nwfull, hT, col0, hn_r, st_r, junk, pt_r, cst):
    hn = hn_r.next(); st = st_r.next()
    rmsnorm_rows(k, xt, hn, junk, st)
    pt = pt_r.next()
    for c in range(8):
        k.tr(pt[:, c, :], hn[:, c * 128:(c + 1) * 128], cst["ident_bf"][:])
    k.tt("dve", hT[:, :, col0:col0 + 128], pt[:], nwfull[:], ALU.mult)


def make_nwfull(k, es, src1d, ones):
    nc = k.nc
    nw = es.enter_context(nc.sbuf_tensor(k.name("nw"), [128, 8], F32))
    wfull = es.enter_context(nc.sbuf_tensor(k.name("wfull"), [128, 8, 128], BF16))
    k.dma("sp", nw[:], src1d.rearrange("(c p) -> p c", p=128), slow=True)
    for c in range(8):
        k.ts("dve", wfull[:, c, :], ones[:], nw[:, c:c + 1], ALU.mult)
    return wfull


def phase_mix_xattn(k, S, l, dr, cst, x_in, x_out):
    nc = k.nc
    NG = S // 512
    with ExitStack() as es:
        wout = es.enter_context(nc.sbuf_tensor(k.name("wout"), [128, 8, D], BF16))
        wxq = es.enter_context(nc.sbuf_tensor(k.name("wxq"), [128, 8, D], BF16))
        wxo = es.enter_context(nc.sbuf_tensor(k.name("wxo"), [128, 8, D], BF16))
        wkv = es.enter_context(nc.sbuf_tensor(k.name("wkv"), [128, 8, 2 * D], BF16))
        kkT = es.enter_context(nc.sbuf_tensor(k.name("kkT"), [128, 8, NMEM], BF16))
        vv = es.enter_context(nc.sbuf_tensor(k.name("vv"), [128, 2, D], BF16))
        memT = es.enter_context(nc.sbuf_tensor(k.name("memT"), [128, 8, NMEM], BF16))
        ones = es.enter_context(nc.sbuf_tensor(k.name("ones"), [128, 128], F32))
        ones_bf = es.enter_context(nc.sbuf_tensor(k.name("onesb"), [128, 128], BF16))
        xg = es.enter_context(nc.sbuf_tensor(k.name("xg"), [128, 4, D], F32))
        yt_r = Ring(k, es, "yt", 2, [128, D], BF16)
        yT = es.enter_context(nc.sbuf_tensor(k.name("yT"), [128, 8, 512], BF16))
        h2T = es.enter_context(nc.sbuf_tensor(k.name("h2T"), [128, 8, 512], BF16))
        qT = es.enter_context(nc.sbuf_tensor(k.name("qT"), [128, 8, 512], BF16))
        oT = es.enter_context(nc.sbuf_tensor(k.name("oT"), [128, 8, 512], BF16))
        PT_r = Ring(k, es, "PT", 4, [128, 512], BF16)
        rden_r = Ring(k, es, "rden", 2, [128, 512], F32)
        hn_r = Ring(k, es, "hn", 2, [128, D], BF16)
        junk = es.enter_context(nc.sbuf_tensor(k.name("junk"), [128, D], BF16))
        st_r = Ring(k, es, "st", 2, [128, 4], F32)
        mt_r = Ring(k, es, "mt", 2, [128, D], F32)
        pt_r = Ring(k, es, "ptr", 2, [128, 8, 128], BF16, psum=True)
        pm_r = Ring(k, es, "pmm", 5, [128, 512], F32, psum=True)

        k.memset("dve", ones[:], 1.0)
        k.memset("dve", ones_bf[:], 1.0)
        load_w(k, wkv, dr["w_xkv"].ap()[l], 8, 2 * D, split=2)
        load_w(k, wout, dr["w_out"].ap()[l], 8, D)
        load_w(k, wxq, dr["w_xq"].ap()[l], 8, D)
        load_w(k, wxo, dr["w_xo"].ap()[l], 8, D)
        nw_mem = make_nwfull(k, es, dr["norm_mem_w"].ap()[l], ones)
        nw_xa = make_nwfull(k, es, dr["norm_xa_w"].ap()[l], ones)
        msrc = dr["mem"].ap().rearrange("(n p) d -> n p d", p=128)
        for mt in range(2):
            m_t = mt_r.next()
            k.dma("sp", m_t[:], msrc[mt])
            norm_to_T(k, m_t, nw_mem, memT, mt * 128, hn_r, st_r, junk, pt_r, cst)
        ev = 0
        for m in range(8):
            ps = pm_r.next()
            for c in range(8):
                k.mm(ps[:, 0:NMEM], wkv[:, c, m * 128:(m + 1) * 128], memT[:, c, :], start=(c == 0), stop=(c == 7))
            k.copy("act", kkT[:, m, :], ps[:, 0:NMEM])
        for mt in range(2):
            for n in range(2):
                ps = pm_r.next()
                for c in range(8):
                    k.mm(ps[:], memT[:, c, mt * 128:(mt + 1) * 128], wkv[:, c, D + n * 512:D + (n + 1) * 512],
                         start=(c == 0), stop=(c == 7))
                k.copy("dve", vv[:, mt, n * 512:(n + 1) * 512], ps[:])

        ysrc = dr["y"].ap().rearrange("(n p) d -> n p d", p=128)
        xsrc = x_in.ap().rearrange("(n p) d -> n p d", p=128)
        xdst = x_out.ap().rearrange("(n p) d -> n p d", p=128)
        for g in range(NG):
            for j in range(4):
                t = g * 4 + j
                yt = yt_r.next()
                k.dma("sp", yt[:], ysrc[t])
                k.dma("sp", xg[:, j, :], xsrc[t])
                pt = pt_r.next()
                for c in range(8):
                    k.tr(pt[:, c, :], yt[:, c * 128:(c + 1) * 128], cst["ident_bf"][:])
                k.copy("act", yT[:, :, j * 128:(j + 1) * 128], pt[:])
            for j in range(4):
                for n in range(2):
                    ps = pm_r.next()
                    for c in range(8):
                        k.mm(ps[:], yT[:, c, j * 128:(j + 1) * 128], wout[:, c, n * 512:(n + 1) * 512],
                             start=(c == 0), stop=(c == 7))
                    k.tt("dve", xg[:, j, n * 512:(n + 1) * 512], ps[:], xg[:, j, n * 512:(n + 1) * 512], ALU.add)
                norm_to_T(k, xg[:, j, :], nw_xa, h2T, j * 128, hn_r, st_r, junk, pt_r, cst)
            for m in range(8):
                ps = pm_r.next()
                for c in range(8):
                    k.mm(ps[:], wxq[:, c, m * 128:(m + 1) * 128], h2T[:, c, :], start=(c == 0), stop=(c == 7))
                k.copy("act" if m % 2 == 0 else "dve", qT[:, m, :], ps[:])
            for hh in range(4):
                PTs = []
                for mt in range(2):
                    ps = pm_r.next()
                    for dc in range(2):
                        k.mm(ps[:], kkT[:, 2 * hh + dc, mt * 128:(mt + 1) * 128], qT[:, 2 * hh + dc, :],
                             start=(dc == 0), stop=(dc == 1))
                    PT = PT_r.next()
                    k.act(PT[:], ps[:], AF.Exp, scale=1.0 / 16.0)
                    PTs.append(PT)
                ps = pm_r.next()
                for mt in range(2):
                    k.mm(ps[:], ones_bf[:], PTs[mt][:], start=(mt == 0), stop=(mt == 1))
                rden = rden_r.next()
                k.recip(rden[:], ps[:])
                for dc in range(2):
                    ps = pm_r.next()
                    for mt in range(2):
                        k.mm(ps[:], vv[:, mt, hh * 256 + dc * 128:hh * 256 + (dc + 1) * 128], PTs[mt][:],
                             start=(mt == 0), stop=(mt == 1))
                    k.tt("dve", oT[:, 2 * hh + dc, :], ps[:], rden[:], ALU.mult)
            for j in range(4):
                t = g * 4 + j
                for n in range(2):
                    ps = pm_r.next()
                    for c in range(8):
                        k.mm(ps[:], oT[:, c, j * 128:(j + 1) * 128], wxo[:, c, n * 512:(n + 1) * 512],
                             start=(c == 0), stop=(c == 7))
                    k.tt("dve", xg[:, j, n * 512:(n + 1) * 512], ps[:], xg[:, j, n * 512:(n + 1) * 512], ALU.add)
                k.dma("pool", xdst[t], xg[:, j, :])
        k.P.barrier()
        k.P.emit()


def phase_ffn(k, S, l, dr, cst, x_io, final_out=None):
    nc = k.nc
    GT = 2
    NG = S // (128 * GT)
    with ExitStack() as es:
        w1 = es.enter_context(nc.sbuf_tensor(k.name("w1"), [128, 8, DFF], BF16))
        w2 = es.enter_context(nc.sbuf_tensor(k.name("w2"), [128, 32, D], BF16))
        ones = es.enter_context(nc.sbuf_tensor(k.name("ones"), [128, 128], F32))
        xg = es.enter_context(nc.sbuf_tensor(k.name("xg"), [128, GT, D], F32))
        h3T = es.enter_context(nc.sbuf_tensor(k.name("h3T"), [128, 8, 128 * GT], BF16))
        uT = es.enter_context(nc.sbuf_tensor(k.name("uT"), [128, 32, 128 * GT], BF16))
        r_r = Ring(k, es, "relu", 3, [128, 128 * GT], BF16)
        hn_r = Ring(k, es, "hn", 2, [128, D], BF16)
        junk = es.enter_context(nc.sbuf_tensor(k.name("junk"), [128, D], BF16))
        st_r = Ring(k, es, "st", 2, [128, 4], F32)
        pt_r = Ring(k, es, "ptr", 2, [128, 8, 128], BF16, psum=True)
        pm_r = Ring(k, es, "pmm", 5, [128, 512], F32, psum=True)
        k.memset("dve", ones[:], 1.0)
        load_w(k, w1, dr["w_ff1"].ap()[l], 8, DFF, split=1)
        load_w(k, w2, dr["w_ff2"].ap()[l], 32, D, split=4)
        nw = make_nwfull(k, es, dr["norm_ffn_w"].ap()[l], ones)
        if final_out is not None:
            nwo = es.enter_context(nc.sbuf_tensor(k.name("nwo"), [128, D], F32))
            src = dr["norm_out_w"]
            k.dma("sp", nwo[:], bass.AP(src, 0, [[0, 128], [1, D]]))
            fo_r = Ring(k, es, "fo", 2, [128, D], F32)
            fdst = final_out.ap().rearrange("(n p) d -> n p d", p=128)
        xv = x_io.ap().rearrange("(n p) d -> n p d", p=128)
        W = 128 * GT
        for g in range(NG):
            for j in range(GT):
                t = g * GT + j
                k.dma("sp", xg[:, j, :], xv[t])
                norm_to_T(k, xg[:, j, :], nw, h3T, j * 128, hn_r, st_r, junk, pt_r, cst)
            for f in range(32):
                ps = pm_r.next()
                for c in range(8):
                    k.mm(ps[:, 0:W], w1[:, c, f * 128:(f + 1) * 128], h3T[:, c, :], start=(c == 0), stop=(c == 7))
                r = r_r.next()
                k.act(r[:], ps[:, 0:W], AF.Relu)
                k.tt("pool" if f % 2 == 0 else "dve", uT[:, f, :], r[:], r[:], ALU.mult)
            for j in range(GT):
                t = g * GT + j
                for n in range(2):
                    ps = pm_r.next()
                    for f in range(32):
                        k.mm(ps[:], uT[:, f, j * 128:(j + 1) * 128], w2[:, f, n * 512:(n + 1) * 512],
                             start=(f == 0), stop=(f == 31))
                    k.tt("dve", xg[:, j, n * 512:(n + 1) * 512], ps[:], xg[:, j, n * 512:(n + 1) * 512], ALU.add)
                if final_out is None:
                    k.dma("pool", xv[t], xg[:, j, :])
                else:
                    st = st_r.next(); fo = fo_r.next()
                    k.act(junk[:], xg[:, j, :], AF.Square, accum=st[:, 0:1])
                    k.act(st[:, 1:2], st[:, 0:1], AF.Sqrt, bias=EPS, scale=1.0 / D)
                    k.recip(st[:, 2:3], st[:, 1:2])
                    k.stt(fo[:], xg[:, j, :], st[:, 2:3], nwo[:], ALU.mult, ALU.mult)
                    k.dma("pool", fdst[t], fo[:])
        k.P.barrier()
        k.P.emit()


NA_CLASSES = {"int": (0, [-2, -1, 0, 1, 2]), "top0": (5, [0, 1, 2, 3]), "top1": (9, [-1, 0, 1, 2]),
              "bot1": (13, [-2, -1, 0, 1]), "bot0": (17, [-3, -2, -1, 0])}
NEG = -30000.0


def na_class(qt, NT):
    if qt == 0:
        return "top0"
    if qt == 1:
        return "top1"
    if qt == NT - 2:
        return "bot1"
    if qt == NT - 1:
        return "bot0"
    return "int"


def na_bias_host(rel_bias, S):
    L = rel_bias.shape[0]
    R, NT = S // 64, S // 128
    out = np.full((L, 6, 21, 128, 128), NEG, np.float32)
    rep = {"int": 2, "top0": 0, "top1": 1, "bot1": NT - 2, "bot0": NT - 1}
    j = np.arange(128)
    for cls, (base, offs) in NA_CLASSES.items():
        qt = rep[cls]
        for n, o in enumerate(offs):
            kt = qt + o
            kr, kc = 2 * kt + j // 64, j % 64
            qr, qc = 2 * qt + j // 64, j % 64
            r0 = np.clip(qr - 4, 0, R - 8)
            c0 = np.clip(qc - 8, 0, 48)
            inw = ((kr[:, None] >= r0[None, :]) & (kr[:, None] <= r0[None, :] + 7) &
                   (kc[:, None] >= c0[None, :]) & (kc[:, None] <= c0[None, :] + 15))
            drr = np.clip(kr[:, None] - qr[None, :] + 7, 0, 14)
            dcc = np.clip(kc[:, None] - qc[None, :] + 15, 0, 30)
            vals = rel_bias[:, :, drr, dcc]
            out[:, :, base + n] = np.where(inw[None, None], vals, np.float32(NEG))
    return np.ascontiguousarray(out.transpose(0, 1, 3, 2, 4).reshape(L * 6, 128, 21 * 128))


def phase_na(k, S, l, dr, cst):
    nc = k.nc
    NT = S // 128
    with ExitStack() as es:
        qT_r = Ring(k, es, "naq", 2, [64, S], BF16)
        kT_r = Ring(k, es, "nak", 2, [64, S], BF16)
        va_r = Ring(k, es, "nav", 2, [128, NT, 65], BF16)
        nb_r = Ring(k, es, "nab", 2, [128, 21 * 128], F32)
        lg_r = Ring(k, es, "nalg", 2, [128, 640], F32)
        PT_r = Ring(k, es, "naPT", 2, [128, 640], BF16)
        yo_r = Ring(k, es, "nayo", 3, [128, 64], BF16)
        rd_r = Ring(k, es, "nard", 3, [128, 1], F32)
        psA_r = Ring(k, es, "psA", 2, [128, 512], F32, psum=True)
        psB_r = Ring(k, es, "psB", 2, [128, 512], F32, psum=True)
        po_r = Ring(k, es, "pso", 2, [128, 512], F32, psum=True)
        fm = dr["fm"].ap()
        tmv = dr["tm"].ap().rearrange("(n p) c -> p n c", p=128)
        yv = dr["y"].ap().rearrange("(n p) c -> n p c", p=128)
        for h in range(6):
            qT = qT_r.next(); kT = kT_r.next(); va = va_r.next(); nb = nb_r.next()
            k.dma("sp", qT[:], fm[FM_NAQ + h * 64:FM_NAQ + (h + 1) * 64, :])
            k.dma("sp", kT[:], fm[FM_NAK + h * 64:FM_NAK + (h + 1) * 64, :])
            for t0 in range(0, NT, 8):
                k.dma("sp", va[:, t0:t0 + 8, 0:64], tmv[:, t0:t0 + 8, TM_NAV + h * 64:TM_NAV + (h + 1) * 64])
            k.memset("pool", va[:, :, 64:65], 1.0)
            k.dma("sp", nb[:], dr["nab"].ap()[l * 6 + h])
            for qt in range(NT):
                base, offs = NA_CLASSES[na_class(qt, NT)]
                n = len(offs)
                psA = psA_r.next(); psB = psB_r.next(); lg = lg_r.next(); PT = PT_r.next()
                for i, o in enumerate(offs):
                    kt = qt + o
                    dst = psA[:, i * 128:(i + 1) * 128] if i < 4 else psB[:, 0:128]
                    k.mm(dst, kT[:, kt * 128:(kt + 1) * 128], qT[:, qt * 128:(qt + 1) * 128])
                na = min(n, 4) * 128
                k.stt(lg[:, 0:na], psA[:, 0:na], 0.125, nb[:, base * 128:base * 128 + na], ALU.mult, ALU.add)
                k.act(PT[:, 0:na], lg[:, 0:na], AF.Exp)
                if n == 5:
                    k.stt(lg[:, 512:640], psB[:, 0:128], 0.125, nb[:, (base + 4) * 128:(base + 5) * 128],
                          ALU.mult, ALU.add)
                    k.act(PT[:, 512:640], lg[:, 512:640], AF.Exp)
                po = po_r.next()
                for i, o in enumerate(offs):
                    kt = qt + o
                    k.mm(po[:, 0:65], PT[:, i * 128:(i + 1) * 128], va[:, kt, :], start=(i == 0), stop=(i == n - 1))
                rd = rd_r.next(); yo = yo_r.next()
                k.recip(rd[:], po[:, 64:65])
                k.ts("dve", yo[:], po[:, 0:64], rd[:], ALU.mult)
                k.dma("pool", yv[qt][:, h * 64:(h + 1) * 64], yo[:])
        k.P.barrier()
        k.P.emit()


def bcast_rows(k, dst, src_handle, off, n):
    k.dma("sp", dst, bass.AP(src_handle, off, [[0, 128], [1, n]]))


class GatePool:
    def __init__(self, k, es, l, dr, cst):
        nc = k.nc
        self.k, self.cst, self.dr = k, cst, dr
        self.bias = es.enter_context(nc.sbuf_tensor(k.name("gbias"), [128, 40], F32))
        self.nA = es.enter_context(nc.sbuf_tensor(k.name("gnA"), [128, 12], F32))
        self.ones = es.enter_context(nc.sbuf_tensor(k.name("gones"), [128, 128], F32))
        k.memset("dve", self.bias[:], 0.0)
        k.memset("dve", self.ones[:], 1.0)
        bcast_rows(k, self.bias[:, 0:8], dr["ml_i_bias"], l * 8, 8)
        bcast_rows(k, self.bias[:, 8:16], dr["ml_f_bias"], l * 8, 8)
        bcast_rows(k, self.bias[:, 28:40], dr["gdn_dt_bias"], l * 12, 12)
        bcast_rows(k, self.nA[:], dr["gdn_a_log"], l * 12, 12)
        k.act(self.nA[:], self.nA[:], AF.Exp)
        k.ts("dve", self.nA[:], self.nA[:], -1.0, ALU.mult)
        self.gt_r = Ring(k, es, "gtt", 2, [128, 40], F32)
        self.a_r = Ring(k, es, "gta", 2, [128, 40], F32)
        self.e_r = Ring(k, es, "gte", 2, [128, 40], F32)
        self.sp_r = Ring(k, es, "gtsp", 2, [128, 40], F32)
        self.w_r = Ring(k, es, "gtw", 2, [128, 32], F32)
        self.x_r = Ring(k, es, "gtx", 2, [128, 32], F32)
        self.bt_r = Ring(k, es, "gtbt", 2, [64, 8], F32)
        self.pg_r = Ring(k, es, "gtpg", 1, [128, 512], F32, psum=True)

    def pre(self, t):
        k = self.k
        gt = self.gt_r.next(); a = self.a_r.next(); e = self.e_r.next(); sp = self.sp_r.next()
        k.dma("sp", gt[:], self.dr["gt"].ap()[t * 128:(t + 1) * 128, :])
        k.tt("dve", a[:], gt[:], self.bias[:], ALU.add)
        k.act(e[:, 8:28], a[:, 8:28], AF.Exp, scale=-1.0)
        k.act(e[:, 28:40], a[:, 28:40], AF.Exp)
        k.act(sp[:, 8:40], e[:, 8:40], AF.Ln, bias=1.0)
        return a, sp

    def mlstm(self, t, d):
        k = self.k
        a, sp = self.pre(t)
        pg = self.pg_r.next()
        lo = 8 + d * 4
        k.mm(pg[:, 0:4], self.cst["c_masks"][:, d, :], sp[:, lo:lo + 4])
        k.mm(pg[0:64, 8:12], self.ones[:, 0:64], sp[:, lo:lo + 4])
        w = self.w_r.next(); x = self.x_r.next(); bt = self.bt_r.next()
        k.ts("dve", w[:, 0:4], pg[:, 0:4], -1.0, ALU.mult)
        k.tt("dve", w[:, 4:8], pg[:, 0:4], a[:, d * 4:d * 4 + 4], ALU.add)
        k.act(x[:, 0:8], w[:, 0:8], AF.Exp)
        k.act(bt[:, 0:4], pg[0:64, 8:12], AF.Exp, scale=-1.0)
        return x, bt

    def gdn(self, t, d):
        k = self.k
        a, sp = self.pre(t)
        w = self.w_r.next(); x = self.x_r.next(); bt = self.bt_r.next()
        g = self.e_r.next()
        lo = 28 + d * 6
        k.tt("dve", g[:, 0:6], sp[:, lo:lo + 6], self.nA[:, d * 6:d * 6 + 6], ALU.mult)
        pg = self.pg_r.next()
        k.mm(pg[:, 0:6], self.cst["c_masks"][:, d, :], g[:, 0:6])
        k.mm(pg[:, 8:14], self.ones[:], g[:, 0:6])
        lb = 16 + d * 6
        k.copy("dve", w[:, 0:6], pg[:, 0:6])
        k.tt("dve", w[:, 6:12], pg[:, 0:6], sp[:, lb:lb + 6], ALU.subtract)
        k.tt("dve", w[:, 12:18], pg[:, 8:14], w[:, 0:6], ALU.subtract)
        k.ts("dve", w[:, 18:24], sp[:, lb:lb + 6], -1.0, ALU.mult)
        k.act(x[:, 0:24], w[:, 0:24], AF.Exp)
        k.act(bt[:, 0:6], pg[0:64, 8:14], AF.Exp)
        return w, x, bt


def phase_mlstm(k, S, l, dr, cst):
    nc = k.nc
    NT = S // 128
    with ExitStack() as es:
        GP = GatePool(k, es, l, dr, cst)
        qT_r = Ring(k, es, "mlq", 2, [64, 4, 128], BF16)
        kT_r = Ring(k, es, "mlk", 2, [64, 4, 128], BF16)
        ktm_r = Ring(k, es, "mlktm", 2, [128, 256], BF16)
        va_r = Ring(k, es, "mlva", 2, [128, 4, 65], BF16)
        vp_r = Ring(k, es, "mlvp", 3, [128, 65], BF16)
        pm_r = Ring(k, es, "mlpm", 3, [128, 128], BF16)
        C32 = [es.enter_context(nc.sbuf_tensor(k.name("C32"), [64, 65], F32)) for _ in range(4)]
        Cbf = [es.enter_context(nc.sbuf_tensor(k.name("Cbf"), [64, 65], BF16)) for _ in range(4)]
        tmp_r = Ring(k, es, "mltmp", 3, [64, 65], F32)
        sm_r = Ring(k, es, "mlsm", 4, [128, 4], F32)
        hb_r = Ring(k, es, "mlh", 2, [128, 256], F32)
        hf_r = Ring(k, es, "mlhf", 2, [128, 256], F32)
        ot_r = Ring(k, es, "mlo", 2, [128, 256], BF16)
        gw_r = Ring(k, es, "mlgw", 2, [128, 256], F32)
        yo_r = Ring(k, es, "mly", 2, [128, 256], BF16)
        junk = es.enter_context(nc.sbuf_tensor(k.name("mljunk"), [128, 64], F32))
        nwb = es.enter_context(nc.sbuf_tensor(k.name("mlnw"), [128, 256], F32))
        ps_r = Ring(k, es, "mlps", 2, [128, 512], F32, psum=True)
        po_r = Ring(k, es, "mlpo", 2, [128, 512], F32, psum=True)
        pc_r = Ring(k, es, "mlpc", 2, [128, 512], F32, psum=True)
        bcast_rows(k, nwb[:], dr["ml_norm_w"], l * 256, 256)
        for t_ in va_r.t:
            k.memset("dve", t_[:, :, 64:65], 1.0)
        fm = dr["fm"].ap()
        tmv = dr["tm"].ap().rearrange("(n p) c -> n p c", p=128)
        hml = dr["hml"].ap().rearrange("(n p) c -> n p c", p=128)
        yv = dr["y"].ap().rearrange("(n p) c -> n p c", p=128)
        for d in range(2):
            for h in range(4):
                k.memset("dve", C32[h][:], 0.0)
                k.memset("dve", Cbf[h][:], 0.0)
            mask = cst["c_masks"][:, d, :]
            order = range(NT) if d == 0 else range(NT - 1, -1, -1)
            for t in order:
                ex, ebt = GP.mlstm(t, d)
                qT = qT_r.next(); kT = kT_r.next(); ktm = ktm_r.next(); va = va_r.next()
                cs = slice(t * 128, (t + 1) * 128)
                k.dma("sp", qT[:], fm[FM_MLQ:FM_MLQ + 256, cs].rearrange("(h d) t -> d h t", d=64))
                k.dma("sp", kT[:], fm[FM_MLK:FM_MLK + 256, cs].rearrange("(h d) t -> d h t", d=64))
                k.dma("sp", ktm[:], tmv[t][:, TM_MLK:TM_MLK + 256])
                k.dma("sp", va[:, :, 0:64], tmv[t][:, TM_MLV:TM_MLV + 256].rearrange("p (h d) -> p h d", d=64))
                hb = hb_r.next()
                for h in range(4):
                    vp = vp_r.next()
                    k.ts("dve", vp[:], va[:, h, :], ex[:, 4 + h:5 + h], ALU.mult, 0.125, ALU.mult)
                    ps = ps_r.next()
                    k.mm(ps[:, 0:128], kT[:, h, :], qT[:, h, :])
                    pm = pm_r.next()
                    k.tt("dve", pm[:], ps[:, 0:128], mask, ALU.mult)
                    po = po_r.next()
                    k.mm(po[:, 0:65], pm[:], vp[:], start=True, stop=False)
                    k.mm(po[:, 0:65], qT[:, h, :], Cbf[h][:], start=False, stop=True)
                    pc = pc_r.next()
                    k.mm(pc[0:64, 0:65], ktm[:, h * 64:(h + 1) * 64], vp[:])
                    tmp = tmp_r.next()
                    k.tt("dve", tmp[:], pc[0:64, 0:65], C32[h][:], ALU.add)
                    k.ts("dve", C32[h][:], tmp[:], ebt[:, h:h + 1], ALU.mult)
                    k.act(Cbf[h][:], tmp[:], AF.Copy, scale=ebt[:, h:h + 1])
                    sm = sm_r.next()
                    k.act(sm[:, 3:4], po[:, 64:65], AF.Abs, scale=ex[:, h:h + 1])
                    k.ts("dve", sm[:, 0:1], sm[:, 3:4], 1.0, ALU.max)
                    k.recip(sm[:, 1:2], sm[:, 0:1])
                    k.tt("dve", sm[:, 2:3], sm[:, 1:2], ex[:, h:h + 1], ALU.mult)
                    k.ts("dve", hb[:, h * 64:(h + 1) * 64], po[:, 0:64], sm[:, 2:3], ALU.mult)
                if d == 0:
                    k.dma("pool", hml[t], hb[:])
                else:
                    hf = hf_r.next(); ot = ot_r.next(); gw = gw_r.next(); yo = yo_r.next()
                    k.dma("sp", hf[:], hml[t])
                    k.dma("sp", ot[:], tmv[t][:, TM_MLO:TM_MLO + 256])
                    k.tt("dve", hb[:], hb[:], hf[:], ALU.add)
                    k.act(gw[:], ot[:], AF.Sigmoid)
                    k.tt("dve", gw[:], gw[:], nwb[:], ALU.mult)
                    sm = sm_r.next()
                    for h in range(4):
                        k.act(junk[:], hb[:, h * 64:(h + 1) * 64], AF.Square, accum=sm[:, h:h + 1])
                    sm2 = sm_r.next()
                    k.act(sm2[:], sm[:], AF.Sqrt, bias=EPS, scale=1.0 / 64.0)
                    sm3 = sm_r.next()
                    k.recip(sm3[:], sm2[:])
                    for h in range(4):
                        k.stt(yo[:, h * 64:(h + 1) * 64], hb[:, h * 64:(h + 1) * 64], sm3[:, h:h + 1],
                              gw[:, h * 64:(h + 1) * 64], ALU.mult, ALU.mult)
                    k.dma("pool", yv[t][:, 384:640], yo[:])
        k.P.barrier()
        k.P.emit()


def phase_gdn_prep(k, S, l, dr, cst):
    nc = k.nc
    NCH = S // 512
    with ExitStack() as es:
        cwr = es.enter_context(nc.sbuf_tensor(k.name("cwr"), [5, 1152], F32))
        cw = es.enter_context(nc.sbuf_tensor(k.name("cw"), [128, 9, 5], F32))
        xin_r = Ring(k, es, "gxin", 2, [128, 516], BF16)
        acc_r = Ring(k, es, "gacc", 2, [128, 512], F32)
        s_r = Ring(k, es, "gs", 2, [128, 512], F32)
        sq_r = Ring(k, es, "gsq", 2, [128, 512], BF16)
        rt_r = Ring(k, es, "grt", 2, [128, 512], F32)
        sn_r = Ring(k, es, "gsn", 2, [128, 512], BF16)
        st_r = Ring(k, es, "gst", 2, [128, 4, 128], BF16)
        pcw = es.enter_context(nc.psum_tensor(k.name("pcw"), [128, 512], F32))
        ps_r = Ring(k, es, "gpps", 2, [128, 512], F32, psum=True)
        pt_r = Ring(k, es, "gppt", 2, [128, 4, 128], BF16, psum=True)
        k.dma("sp", cwr[:], dr["gdn_conv_w"].ap()[l])
        for g in range(9):
            k.tr(pcw[:, g * 8:g * 8 + 5], cwr[0:5, g * 128:(g + 1) * 128], cst["ident_f"][0:5, 0:5])
        for g in range(9):
            k.copy("dve", cw[:, g, :], pcw[:, g * 8:g * 8 + 5])
        fm = dr["fm"].ap()
        dsts = {0: dr["gqT"], 1: dr["gkT"]}
        tms = {1: dr["gk_tm"], 2: dr["gv_tm"]}
        for g in range(9):
            kind, gi = g // 3, g % 3
            for c in range(NCH):
                xin = xin_r.next(); acc = acc_r.next(); s = s_r.next(); sn = sn_r.next()
                lo, hi = max(c * 512 - 2, 0), min(c * 512 + 514, S)
                off = lo - (c * 512 - 2)
                if c == 0:
                    k.memset("pool", xin[:, 0:2], 0.0)
                if c == NCH - 1:
                    k.memset("pool", xin[:, 514:516], 0.0)
                k.dma("sp", xin[:, off:off + hi - lo], fm[FM_GDQ + g * 128:FM_GDQ + (g + 1) * 128, lo:hi])
                k.ts("dve", acc[:], xin[:, 0:512], cw[:, g, 0:1], ALU.mult)
                for kk in range(1, 5):
                    k.stt(acc[:], xin[:, kk:kk + 512], cw[:, g, kk:kk + 1], acc[:], ALU.mult, ALU.add)
                k.act(s[:], acc[:], AF.Silu)
                if kind < 2:
                    sq = sq_r.next(); rt = rt_r.next(); ps = ps_r.next()
                    k.tt("pool", sq[:], s[:], s[:], ALU.mult)
                    k.mm(ps[:], cst["blk2_bf"][:], sq[:])
                    k.act(rt[:], ps[:], AF.Sqrt, bias=EPS)
                    k.recip(rt[:], rt[:])
                    if kind == 0:
                        k.stt(sn[:], s[:], 0.125, rt[:], ALU.mult, ALU.mult)
                    else:
                        k.tt("dve", sn[:], s[:], rt[:], ALU.mult)
                    k.dma("pool", dsts[kind].ap()[gi * 128:(gi + 1) * 128, c * 512:(c + 1) * 512], sn[:])
                else:
                    k.copy("pool", sn[:], s[:])
                if kind >= 1:
                    pt = pt_r.next(); st = st_r.next()
                    for j in range(4):
                        k.tr(pt[:, j, :], sn[:, j * 128:(j + 1) * 128], cst["ident_bf"][:])
                    k.copy("act", st[:], pt[:])
                    dv = tms[kind].ap().rearrange("(n p) c -> p n c", p=128)
                    k.dma("pool", dv[:, c * 4:(c + 1) * 4, gi * 128:(gi + 1) * 128], st[:])
        k.P.barrier()
        k.P.emit()


def phase_gdn(k, S, l, dr, cst, stage=9):
    nc = k.nc
    NT = S // 128
    M = cst["c_masks"]
    with ExitStack() as es:
        GP = GatePool(k, es, l, dr, cst)
        qT_r = Ring(k, es, "gdq", 2, [64, 6, 128], BF16)
        kT_r = Ring(k, es, "gdk", 2, [64, 6, 128], BF16)
        ktm_r = Ring(k, es, "gdktm", 2, [128, 384], BF16)
        vtm_r = Ring(k, es, "gdvtm", 2, [128, 384], BF16)
        dg_r = Ring(k, es, "gddg", 4, [128, 128], F32)
        xp_r = Ring(k, es, "gdxp", 9, [128, 128], F32)
        A_r = Ring(k, es, "gdA", 9, [128, 128], F32)
        B_r = Ring(k, es, "gdB", 9, [128, 128], F32)
        N_r = Ring(k, es, "gdN", 9, [128, 128], F32)
        at_r = Ring(k, es, "gdat", 6, [128, 128], BF16)
        sc_r = Ring(k, es, "gdsc", 2, [128, 2, 64], F32)
        ks_r = Ring(k, es, "gdks", 2, [128, 64], BF16)
        u_r = Ring(k, es, "gdu", 2, [128, 64], F32)
        wk_r = Ring(k, es, "gdwk", 2, [64, 128], BF16)
        vn_r = Ring(k, es, "gdvn", 2, [128, 64], BF16)
        o2_r = Ring(k, es, "gdo2", 2, [128, 64], F32)
        ob_r = Ring(k, es, "gdob", 2, [128, 384], F32)
        of_r = Ring(k, es, "gdof", 2, [128, 384], F32)
        z_r = Ring(k, es, "gdz", 2, [128, 384], BF16)
        gz_r = Ring(k, es, "gdgz", 2, [128, 384], F32)
        yo_r = Ring(k, es, "gdyo", 2, [128, 384], BF16)
        sm_r = Ring(k, es, "gdsm", 4, [128, 8], F32)
        junk = es.enter_context(nc.sbuf_tensor(k.name("gdjunk"), [128, 64], F32))
        nwb = es.enter_context(nc.sbuf_tensor(k.name("gdnw"), [128, 384], F32))
        S32 = [es.enter_context(nc.sbuf_tensor(k.name("S32"), [64, 64], F32)) for _ in range(6)]
        Sbf = [es.enter_context(nc.sbuf_tensor(k.name("Sbf"), [64, 64], BF16)) for _ in range(6)]
        pA_r = Ring(k, es, "gdpA", 1, [128, 512], F32, psum=True)
        pB_r = Ring(k, es, "gdpB", 1, [128, 512], F32, psum=True)
        pc_r = Ring(k, es, "gdpc", 3, [128, 512], F32, psum=True)
        pu_r = Ring(k, es, "gdpu", 1, [128, 512], F32, psum=True)
        pv_r = Ring(k, es, "gdpv", 1, [128, 512], F32, psum=True)
        bcast_rows(k, nwb[:], dr["gdn_norm_w"], l * 384, 384)
        idf, idb, ones = cst["ident_f"], cst["ident_bf"], GP.ones
        tmv = dr["tm"].ap().rearrange("(n p) c -> n p c", p=128)
        ktv = dr["gk_tm"].ap().rearrange("(n p) c -> n p c", p=128)
        vtv = dr["gv_tm"].ap().rearrange("(n p) c -> n p c", p=128)
        ogd = dr["ogd"].ap().rearrange("(n p) c -> n p c", p=128)
        yv = dr["y"].ap().rearrange("(n p) c -> n p c", p=128)
        for d in range(2):
            mA, mB, mC = (M[:, 2, :], M[:, 4, :], M[:, 6, :]) if d == 0 else (M[:, 3, :], M[:, 5, :], M[:, 7, :])
            for h in range(6):
                k.memset("dve", S32[h][:], 0.0)
                k.memset("dve", Sbf[h][:], 0.0)
            order = range(NT) if d == 0 else range(NT - 1, -1, -1)
            for t in order:
                raw, ex, egl = GP.gdn(t, d)
                qT = qT_r.next(); kT = kT_r.next(); ktm = ktm_r.next(); vtm = vtm_r.next()
                cs = slice(t * 128, (t + 1) * 128)
                k.dma("sp", qT[:], dr["gqT"].ap()[:, cs].rearrange("(h d) t -> d h t", d=64))
                k.dma("sp", kT[:], dr["gkT"].ap()[:, cs].rearrange("(h d) t -> d h t", d=64))
                k.dma("sp", ktm[:], ktv[t])
                k.dma("sp", vtm[:], vtv[t])
                ob = ob_r.next()
                for g0 in range(0, 6, 3):
                    grp = list(range(g0, g0 + 3))
                    st = {}
                    for h in grp:
                        pA = pA_r.next(); pB = pB_r.next()
                        k.mm(pA[:, 0:128], kT[:, h, :], kT[:, h, :])
                        k.mm(pA[:, 128:256], kT[:, h, :], qT[:, h, :])
                        dg1 = dg_r.next(); dg2 = dg_r.next()
                        k.ts("dve", dg1[:], idf[:], raw[:, h:h + 1], ALU.mult)
                        k.ts("dve", dg2[:], idf[:], raw[:, 6 + h:7 + h], ALU.mult)
                        k.mm(pB[:, 0:128], ones[:], dg1[:])
                        k.mm(pB[:, 128:256], ones[:], dg2[:])
                        xa = xp_r.next(); xb = xp_r.next(); xc = xp_r.next()
                        k.stt(xa[:], pB[:, 0:128], raw[:, 6 + h:7 + h], mA, ALU.subtract, ALU.add)
                        k.stt(xb[:], pB[:, 128:256], raw[:, h:h + 1], mB, ALU.subtract, ALU.add)
                        k.stt(xc[:], pB[:, 0:128], raw[:, h:h + 1], mC, ALU.subtract, ALU.add)
                        k.act(xa[:], xa[:], AF.Exp, scale=-1.0)
                        k.act(xb[:], xb[:], AF.Exp)
                        k.act(xc[:], xc[:], AF.Exp)
                        A = A_r.next(); B = B_r.next(); at = at_r.next(); N = N_r.next()
                        k.tt("dve", A[:], pA[:, 0:128], xa[:], ALU.mult)
                        k.tt("dve", B[:], pA[:, 0:128], xb[:], ALU.mult)
                        k.tt("dve", at[:], pA[:, 128:256], xc[:], ALU.mult)
                        k.tt("dve", N[:], idf[:], B[:], ALU.subtract)
                        st[h] = [A, B, N, at]
                    for lv in range(1, 7):
                        for hi, h in enumerate(grp):
                            A, B, N, at = st[h]
                            pc = pc_r.t[hi]
                            A2 = A_r.next()
                            k.mm(pc[:, 0:128], B[:], A[:])
                            if lv < 6:
                                B2 = B_r.next()
                                k.mm(pc[:, 128:256], A[:], B[:])
                            k.copy("act", A2[:], pc[:, 0:128])
                            if lv < 6:
                                k.copy("dve", B2[:], pc[:, 128:256])
                            k.mm(pc[:, 256:384], A2[:], N[:])
                            N2 = N_r.next()
                            k.tt("dve", N2[:], pc[:, 256:384], N[:], ALU.add)
                            st[h] = [A2, B2 if lv < 6 else B, N2, at]
                    for h in grp:
                        A, B, N, at = st[h]
                        hs = slice(h * 64, (h + 1) * 64)
                        sc = sc_r.next()
                        k.ts("dve", sc[:, 0, :], vtm[:, hs], ex[:, 18 + h:19 + h], ALU.mult)
                        k.ts("dve", sc[:, 1, :], ktm[:, hs], ex[:, 6 + h:7 + h], ALU.mult)
                        ks = ks_r.next()
                        k.ts("dve", ks[:], ktm[:, hs], ex[:, 12 + h:13 + h], ALU.mult)
                        pu = pu_r.next()
                        k.mm(pu[:, 0:64], N[:], sc[:, 0, :])
                        k.mm(pu[0:64, 64:192], sc[:, 1, :], N[:])
                        u = u_r.next(); wk = wk_r.next()
                        k.copy("act", u[:], pu[:, 0:64])
                        k.copy("dve", wk[:], pu[0:64, 64:192])
                        pv = pv_r.next()
                        k.mm(pv[:, 0:64], wk[:], Sbf[h][:])
                        vn = vn_r.next()
                        k.tt("dve", vn[:], u[:], pv[:, 0:64], ALU.subtract)
                        k.mm(pv[:, 64:128], qT[:, h, :], Sbf[h][:])
                        k.mm(pv[:, 128:192], at[:], vn[:])
                        k.mm(pv[0:64, 192:256], ks[:], vn[:])
                        o2 = o2_r.next()
                        k.copy("act", o2[:], pv[:, 128:192])
                        k.stt(ob[:, hs], pv[:, 64:128], ex[:, h:h + 1], o2[:], ALU.mult, ALU.add)
                        k.stt(S32[h][:], S32[h][:], egl[:, h:h + 1], pv[0:64, 192:256], ALU.mult, ALU.add)
                        k.copy("act", Sbf[h][:], S32[h][:])
                if d == 0:
                    k.dma("pool", ogd[t], ob[:])
                else:
                    of = of_r.next(); z = z_r.next(); gz = gz_r.next(); yo = yo_r.next()
                    k.dma("sp", of[:], ogd[t])
                    k.dma("sp", z[:], tmv[t][:, TM_GDZ:TM_GDZ + 384])
                    k.tt("dve", ob[:], ob[:], of[:], ALU.add)
                    k.act(gz[:], z[:], AF.Silu)
                    k.tt("dve", gz[:], gz[:], nwb[:], ALU.mult)
                    sm = sm_r.next()
                    for h in range(6):
                        k.act(junk[:], ob[:, h * 64:(h + 1) * 64], AF.Square, accum=sm[:, h:h + 1])
                    sm2 = sm_r.next()
                    k.act(sm2[:, 0:6], sm[:, 0:6], AF.Sqrt, bias=EPS, scale=1.0 / 64.0)
                    sm3 = sm_r.next()
                    k.recip(sm3[:, 0:6], sm2[:, 0:6])
                    for h in range(6):
                        hs = slice(h * 64, (h + 1) * 64)
                        k.stt(yo[:, hs], ob[:, hs], sm3[:, h:h + 1], gz[:, hs], ALU.mult, ALU.mult)
                    k.dma("pool", yv[t][:, 640:1024], yo[:])
        k.P.barrier()
        k.P.emit()


S_FULL = 8192


def build_program(S):
    nc = bass.Bass("TRN2", target_bir_lowering=False)
    dr = declare_dram(nc, S)
    with ExitStack() as es:
        k = K(nc, es)
        cst = setup_consts(k, es, dr)
        for l in range(DEPTH):
            dr["x_cur"] = dr["x"] if l == 0 else dr["xs"]
            phase_inproj(k, S, l, dr, cst)
            phase_na(k, S, l, dr, cst)
            phase_mlstm(k, S, l, dr, cst)
            phase_gdn_prep(k, S, l, dr, cst)
            phase_gdn(k, S, l, dr, cst)
            phase_mix_xattn(k, S, l, dr, cst, dr["x_cur"], dr["xs"])
            phase_ffn(k, S, l, dr, cst, dr["xs"], final_out=(dr["out"] if l == DEPTH - 1 else None))
    return nc


def kernel(**inputs):
    x = np.asarray(inputs["x"], dtype=np.float32)
    mem = np.asarray(inputs["mem"], dtype=np.float32)
    B, S, _ = x.shape
    nc = build_program(S)
    shared = {n: np.ascontiguousarray(np.asarray(inputs[n], dtype=np.float32)) for n in WEIGHT_SHAPES}
    shared.update(make_consts_host())
    shared["nab"] = na_bias_host(np.asarray(inputs["na_rel_bias"], dtype=np.float32), S)
    in_maps = []
    for b in range(B):
        m = dict(shared)
        m["x"] = np.ascontiguousarray(x[b])
        m["mem"] = np.ascontiguousarray(mem[b])
        in_maps.append(m)
    res = run_bass_kernel_spmd(nc, in_maps, core_ids=list(range(B)))
    return np.stack([np.asarray(r["out"], dtype=np.float32) for r in res.results], axis=0)
```

```python
import numpy as np
from contextlib import ExitStack

import concourse.bass as bass
import concourse.mybir as mybir
from concourse.bass_utils import run_bass_kernel_spmd

F32 = mybir.dt.float32
BF16 = mybir.dt.bfloat16
AF = mybir.ActivationFunctionType
ALU = mybir.AluOpType
AX = mybir.AxisListType

ENGS = ("pe", "dve", "act", "pool", "sp")
SEM_CAP = 30000
N_DMA_SEM = 12


def _prod(v):
    r = 1
    for a in v:
        r *= int(a)
    return r


def region(ap):
    t = ap.tensor
    shape = [int(s) for s in t.shape]
    rowlen = _prod(shape[1:])
    off = int(ap.offset)
    p0 = off // rowlen
    c0 = off % rowlen
    p1, c1 = p0, c0
    for step, cnt in ap.ap:
        step, cnt = int(step), int(cnt)
        if cnt <= 1 or step == 0:
            continue
        ext = step * (cnt - 1)
        if step % rowlen == 0:
            p1 += ext // rowlen
        else:
            c1 += ext
    if c1 >= rowlen:
        tot = off + (p1 - p0) * rowlen + (c1 - c0)
        p1 = tot // rowlen
        c0, c1 = 0, rowlen - 1
    if type(t).__name__ == "PSumTensorHandle":
        return (t.name, 0, 127, 0, rowlen - 1)
    return (t.name, p0, p1, c0, c1)


def _overlap(a, b):
    return not (a[2] < b[1] or b[2] < a[1] or a[4] < b[3] or b[4] < a[3])


def _contains(a, b):
    return a[1] <= b[1] and a[2] >= b[2] and a[3] <= b[3] and a[4] >= b[4]


class Op:
    __slots__ = ("eng", "fn", "dma", "seq", "deps", "signal", "sig_idx", "dsem", "dval",
                 "clock", "prewait")


class Prog:
    def __init__(self, nc, es):
        self.nc = nc
        self.es = es
        self.ops = []
        self.recs = {}
        self.nseq = {e: 0 for e in ENGS}
        self.clock = {e: {x: 0 for x in ENGS} for e in ENGS}
        self.known_dma = {e: set() for e in ENGS}
        self.last_compute = {e: None for e in ENGS}
        self.pending_dma = []
        self.esems = {e: [] for e in ENGS}
        self.nsig = {e: 0 for e in ENGS}
        self.dsems = {}
        for q in ("sp", "act", "pool"):
            self.dsems[q] = [[es.enter_context(nc.semaphore(f"d_{q}_{i}")), 0, None]
                             for i in range(N_DMA_SEM)]
        self.dma_rr = {q: 0 for q in self.dsems}
        self.emitted = 0

    def _need(self, op, dep):
        if dep is op:
            return
        c = op.eng
        if dep.dma:
            if dep in self.known_dma[c]:
                return
            self.known_dma[c].add(dep)
            op.deps.append(dep)
            clk = dep.clock
        else:
            if self.clock[c][dep.eng] >= dep.seq:
                return
            op.deps.append(dep)
            dep.signal = True
            clk = dict(dep.clock)
            clk[dep.eng] = max(clk[dep.eng], dep.seq)
        mine = self.clock[c]
        for e in ENGS:
            if clk[e] > mine[e]:
                mine[e] = clk[e]

    def add(self, eng, fn, reads=(), writes=(), dma=False):
        op = Op()
        op.eng, op.fn, op.dma = eng, fn, dma
        op.deps, op.signal, op.sig_idx = [], False, None
        op.dsem = op.dval = None
        op.prewait = None
        self.nseq[eng] += 1
        op.seq = self.nseq[eng]
        if dma:
            q = eng
            i = self.dma_rr[q]
            self.dma_rr[q] = (i + 1) % N_DMA_SEM
            slot = self.dsems[q][i]
            if slot[2] is not None:
                self._need(op, slot[2])
            slot[1] += 16
            slot[2] = op
            op.dsem, op.dval = slot[0], slot[1]
        accs = ([(region(a), type(a.tensor).__name__ == "PSumTensorHandle") for a in reads] +
                [(region(a), True) for a in writes])
        for box, is_w in accs:
            for rbox, rop, rw in self.recs.get(box[0], ()):
                if _overlap(box, rbox) and (is_w or rw):
                    same = (rop.eng == eng and not rop.dma and not dma)
                    if same and eng == "pe":
                        pass
                    else:
                        self._need(op, rop)
        for box, is_w in accs:
            keep = []
            for rec in self.recs.get(box[0], ()):
                rbox, rop, rw = rec
                if is_w and _contains(box, rbox) and rop is not op:
                    continue
                if (not is_w) and (not rw) and rbox == box and rop.eng == eng and not rop.dma and not dma:
                    continue
                keep.append(rec)
            keep.append((box, op, is_w))
            self.recs[box[0]] = keep
        op.clock = dict(self.clock[eng])
        self.ops.append(op)
        if dma:
            self.pending_dma.append(op)
        else:
            self.last_compute[eng] = op
        return op

    def barrier(self):
        for e in ENGS:
            op = Op()
            op.eng, op.fn, op.dma = e, None, False
            op.deps, op.signal, op.sig_idx = [], False, None
            op.dsem = op.dval = None
            op.prewait = None
            self.nseq[e] += 1
            op.seq = self.nseq[e]
            for o in ENGS:
                lc = self.last_compute[o]
                if lc is not None:
                    self._need(op, lc)
            for q in self.dsems:
                for slot in self.dsems[q]:
                    if slot[2] is not None:
                        self._need(op, slot[2])
            op.clock = dict(self.clock[e])
            self.ops.append(op)
        self.pending_dma = []
        self.recs = {}

    def _sem_for(self, eng, idx):
        k = (idx - 1) // SEM_CAP
        while len(self.esems[eng]) <= k:
            self.esems[eng].append(self.es.enter_context(
                self.nc.semaphore(f"s_{eng}_{len(self.esems[eng])}")))
        return self.esems[eng][k], idx - k * SEM_CAP

    def emit(self):
        ops = self.ops[self.emitted:]
        self.emitted = len(self.ops)
        for op in ops:
            if op.signal and not op.dma:
                self.nsig[op.eng] += 1
                op.sig_idx = self.nsig[op.eng]
        per = {e: [o for o in ops if o.eng == e] for e in ENGS}
        prog = self

        def run(e, h):
            for op in per[e]:
                for d in op.deps:
                    if d.dma:
                        h.wait_ge(d.dsem, d.dval)
                    else:
                        s, v = prog._sem_for(d.eng, d.sig_idx)
                        h.wait_ge(s, v)
                if op.fn is None:
                    if op.signal:
                        s, v = prog._sem_for(e, op.sig_idx)
                        h.sem_inc(s, 1)
                    continue
                inst = op.fn(h)
                if op.dma:
                    inst.then_inc(op.dsem, 16)
                elif op.signal:
                    s, v = prog._sem_for(e, op.sig_idx)
                    inst.then_inc(s, 1)

        for op in ops:
            if op.signal and not op.dma:
                self._sem_for(op.eng, op.sig_idx)
        with self.nc.Block() as block:
            @block.tensor
            def _(h):
                run("pe", h)

            @block.vector
            def _(h):
                run("dve", h)

            @block.scalar
            def _(h):
                run("act", h)

            @block.gpsimd
            def _(h):
                run("pool", h)

            @block.sync
            def _(h):
                run("sp", h)


class K:
    def __init__(self, nc, es):
        self.nc = nc
        self.P = Prog(nc, es)
        self.uid = 0

    def name(self, base):
        self.uid += 1
        return f"{base}_{self.uid}"

    def mm(self, out, lhsT, rhs, start=True, stop=True):
        self.P.add("pe", lambda h: h.matmul(out, lhsT, rhs, start=start, stop=stop),
                   reads=[lhsT, rhs], writes=[out])

    def tr(self, out, in_, ident):
        self.P.add("pe", lambda h: h.transpose(out, in_, ident), reads=[in_, ident], writes=[out])

    def act(self, out, in_, func, bias=None, scale=None, accum=None):
        kw = {}
        rd = [in_]
        wr = [out]
        if bias is not None:
            kw["bias"] = bias
            if not isinstance(bias, (int, float)):
                rd.append(bias)
        if scale is not None:
            kw["scale"] = scale
            if not isinstance(scale, (int, float)):
                rd.append(scale)
        if accum is not None:
            kw["accum_out"] = accum
            wr.append(accum)
        self.P.add("act", lambda h: h.activation(out, in_, func, **kw), reads=rd, writes=wr)

    def ts(self, eng, out, in0, s1, op0, s2=None, op1=None, accum=None):
        rd = [in0]
        if not isinstance(s1, (int, float)):
            rd.append(s1)
        if s2 is not None and not isinstance(s2, (int, float)):
            rd.append(s2)
        wr = [out]
        kw = {}
        if op1 is not None:
            kw["op1"] = op1
        if accum is not None:
            kw["accum_out"] = accum
            wr.append(accum)
        self.P.add(eng, lambda h: h.tensor_scalar(out, in0, s1, s2, op0, **kw), reads=rd, writes=wr)

    def tt(self, eng, out, in0, in1, op):
        self.P.add(eng, lambda h: h.tensor_tensor(out, in0, in1, op), reads=[in0, in1], writes=[out])

    def stt(self, out, in0, scalar, in1, op0, op1):
        rd = [in0, in1]
        if not isinstance(scalar, (int, float)):
            rd.append(scalar)
        self.P.add("dve", lambda h: h.scalar_tensor_tensor(out, in0, scalar, in1, op0, op1),
                   reads=rd, writes=[out])

    def copy(self, eng, out, in_):
        if eng == "act":
            self.P.add("act", lambda h: h.copy(out, in_), reads=[in_], writes=[out])
        else:
            self.P.add(eng, lambda h: h.tensor_copy(out, in_), reads=[in_], writes=[out])

    def memset(self, eng, out, val):
        self.P.add(eng, lambda h: h.memset(out, val), reads=[], writes=[out])

    def recip(self, out, in_):
        self.P.add("dve", lambda h: h.reciprocal(out, in_), reads=[in_], writes=[out])

    def scan(self, out, d0, d1, init, op0, op1):
        self.P.add("dve", lambda h: h.tensor_tensor_scan(out, d0, d1, init, op0, op1),
                   reads=[d0, d1], writes=[out])

    def dma(self, q, out, in_, slow=False):
        if slow:
            self.P.add(q, lambda h: h.dma_start(out=out, in_=in_, allow_slow_non_contiguous=True),
                       reads=[in_], writes=[out], dma=True)
        else:
            self.P.add(q, lambda h: h.dma_start(out=out, in_=in_), reads=[in_], writes=[out], dma=True)


D = 1024
DEPTH = 2
P_IN = 3752
NMEM = 256
DFF = 4096
EPS = 1e-6
WIN_BLOCKS = [(0, 768, 0), (1152, 1664, 768), (2192, 3344, 1280), (2176, 2192, 2432),
              (3728, 3752, 2448), (768, 1152, 2472), (1664, 2176, 2856), (3344, 3728, 3368)]
FM_ROWS = 2432
TM_COLS = 1536
FM_NAQ, FM_NAK, FM_MLQ, FM_MLK, FM_GDQ, FM_GDK, FM_GDV = 0, 384, 768, 1024, 1280, 1664, 2048
TM_NAV, TM_MLV, TM_MLO, TM_GDZ, TM_MLK = 0, 384, 640, 896, 1280


class Ring:
    def __init__(self, k, es, name, n, shape, dtype, psum=False):
        mk = k.nc.psum_tensor if psum else k.nc.sbuf_tensor
        self.t = [es.enter_context(mk(k.name(name), shape, dtype)) for _ in range(n)]
        self.i = 0

    def next(self):
        t = self.t[self.i]
        self.i = (self.i + 1) % len(self.t)
        return t


def rmsnorm_rows(k, xt, hn, junk, st, eng_scale="dve"):
    k.act(junk[:], xt[:], AF.Square, accum=st[:, 0:1])
    k.act(st[:, 1:2], st[:, 0:1], AF.Sqrt, bias=EPS, scale=1.0 / D)
    k.recip(st[:, 2:3], st[:, 1:2])
    k.ts(eng_scale, hn[:], xt[:], st[:, 2:3], ALU.mult)


def phase_inproj(k, S, l, dr, cst):
    nc = k.nc
    NG = S // 512
    with ExitStack() as es:
        w = es.enter_context(nc.sbuf_tensor(k.name("win"), [128, 8, P_IN], BF16))
        nw = es.enter_context(nc.sbuf_tensor(k.name("nw"), [128, 8], F32))
        wfull = es.enter_context(nc.sbuf_tensor(k.name("wfull"), [128, 8, 128], BF16))
        ones = es.enter_context(nc.sbuf_tensor(k.name("ones"), [128, 128], F32))
        xt_r = Ring(k, es, "xt", 2, [128, D], F32)
        hn_r = Ring(k, es, "hn", 2, [128, D], BF16)
        junk = es.enter_context(nc.sbuf_tensor(k.name("junk"), [128, D], BF16))
        st_r = Ring(k, es, "st", 2, [128, 4], F32)
        hT_r = Ring(k, es, "hT", 2, [128, 8, 512], BF16)
        fmst_r = Ring(k, es, "fmst", 4, [128, 512], BF16)
        gst_r = Ring(k, es, "gst", 2, [12, 512], F32)
        tmst_r = Ring(k, es, "tmst", 2, [128, TM_COLS], BF16)
        gtst_r = Ring(k, es, "gtst", 2, [128, 40], F32)
        pt_r = Ring(k, es, "ptr", 2, [128, 8, 128], BF16, psum=True)
        pm_r = Ring(k, es, "pmm", 4, [128, 512], F32, psum=True)

        wsrc = dr["w_in"].ap()[l].rearrange("(c p) n -> p c n", p=128)
        for (a, b, m) in WIN_BLOCKS:
            for c0 in range(0, 8, 4):
                k.dma("pool", w[:, c0:c0 + 4, m:m + (b - a)], wsrc[:, c0:c0 + 4, a:b])
        k.dma("sp", nw[:], dr["norm_mix_w"].ap()[l].rearrange("(c p) -> p c", p=128), slow=True)
        k.memset("dve", ones[:], 1.0)
        for c in range(8):
            k.ts("dve", wfull[:, c, :], ones[:], nw[:, c:c + 1], ALU.mult)

        xsrc = dr["x_cur"].ap().rearrange("(n p) d -> n p d", p=128)
        fm = dr["fm"].ap()
        tm = dr["tm"].ap().rearrange("(n p) c -> n p c", p=128)
        ev = 0
        for g in range(NG):
            hT = hT_r.next()
            for j in range(4):
                t = g * 4 + j
                xt = xt_r.next(); hn = hn_r.next(); st = st_r.next()
                k.dma("sp", xt[:], xsrc[t])
                rmsnorm_rows(k, xt, hn, junk, st)
                pt = pt_r.next()
                for c in range(8):
                    k.tr(pt[:, c, :], hn[:, c * 128:(c + 1) * 128], cst["ident_bf"][:])
                k.tt("dve", hT[:, :, j * 128:(j + 1) * 128], pt[:], wfull[:], ALU.mult)
            for m in range(19):
                ps = pm_r.next()
                for c in range(8):
                    k.mm(ps[:], w[:, c, m * 128:(m + 1) * 128], hT[:, c, :], start=(c == 0), stop=(c == 7))
                stg = fmst_r.next()
                k.copy("act" if ev % 2 == 0 else "dve", stg[:], ps[:])
                ev += 1
                k.dma("pool", fm[m * 128:(m + 1) * 128, g * 512:(g + 1) * 512], stg[:])
            for j in range(4):
                t = g * 4 + j
                stg = tmst_r.next()
                for (c0, n, o) in [(2472, 512, 0), (2984, 512, 512), (3496, 256, 1024), (1024, 256, 1280)]:
                    ps = pm_r.next()
                    for c in range(8):
                        k.mm(ps[:, 0:n], hT[:, c, j * 128:(j + 1) * 128], w[:, c, c0:c0 + n],
                             start=(c == 0), stop=(c == 7))
                    k.copy("act" if ev % 2 == 0 else "dve", stg[:, o:o + n], ps[:, 0:n])
                    ev += 1
                k.dma("pool", tm[t], stg[:])
                ps = pm_r.next()
                for c in range(8):
                    k.mm(ps[:, 0:40], hT[:, c, j * 128:(j + 1) * 128], w[:, c, 2432:2472],
                         start=(c == 0), stop=(c == 7))
                gs = gtst_r.next()
                k.copy("dve", gs[:], ps[:, 0:40])
                k.dma("pool", dr["gt"].ap()[t * 128:(t + 1) * 128, :], gs[:])
        k.P.barrier()
        k.P.emit()


def make_consts_host():
    c = {}
    c["c_ident"] = np.eye(128, dtype=np.float32)
    p = np.arange(128)[:, None]
    f = np.arange(128)[None, :]
    big = np.float32(30000.0)
    z = np.float32(0.0)
    c["c_masks"] = np.stack([
        (p <= f).astype(np.float32), (p >= f).astype(np.float32),
        np.where(p > f, z, big), np.where(f > p, z, big),
        np.where(f > p, z, -big), np.where(p > f, z, -big),
        np.where(f >= p, z, -big), np.where(f <= p, z, -big)], axis=1).astype(np.float32)
    es = np.zeros((128, 2, 64), np.float32)
    es[127, 0, :] = 1.0
    es[0, 1, :] = 1.0
    c["c_esel"] = es
    c["c_blk2"] = np.kron(np.eye(2, dtype=np.float32), np.ones((64, 64), np.float32))
    return c


def setup_consts(k, es, dr):
    nc = k.nc
    cst = {}
    idf = es.enter_context(nc.sbuf_tensor("ident_f", [128, 128], F32))
    idb = es.enter_context(nc.sbuf_tensor("ident_bf", [128, 128], BF16))
    k.dma("sp", idf[:], dr["c_ident"].ap())
    k.copy("dve", idb[:], idf[:])
    cst["ident_f"], cst["ident_bf"] = idf, idb
    for nm, shp in [("c_masks", [128, 8, 128]), ("c_esel", [128, 2, 64]), ("c_blk2", [128, 128])]:
        t = es.enter_context(nc.sbuf_tensor(nm + "_sb", shp, F32))
        k.dma("sp", t[:], dr[nm].ap())
        cst[nm] = t
    blkb = es.enter_context(nc.sbuf_tensor("blk2_bf", [128, 128], BF16))
    k.copy("dve", blkb[:], cst["c_blk2"][:])
    cst["blk2_bf"] = blkb
    k.P.barrier()
    k.P.emit()
    return cst


WEIGHT_SHAPES = {
    "norm_mix_w": (DEPTH, D), "w_in": (DEPTH, D, P_IN), "ml_i_bias": (DEPTH, 2, 4), "ml_f_bias": (DEPTH, 2, 4),
    "ml_norm_w": (DEPTH, 256), "gdn_conv_w": (DEPTH, 5, 1152), "gdn_a_log": (DEPTH, 2, 6),
    "gdn_dt_bias": (DEPTH, 2, 6), "gdn_norm_w": (DEPTH, 384), "w_out": (DEPTH, D, D),
    "norm_xa_w": (DEPTH, D), "norm_mem_w": (DEPTH, D), "w_xq": (DEPTH, D, D), "w_xkv": (DEPTH, D, 2 * D),
    "w_xo": (DEPTH, D, D), "norm_ffn_w": (DEPTH, D), "w_ff1": (DEPTH, D, DFF), "w_ff2": (DEPTH, DFF, D),
    "norm_out_w": (D,),
}


def declare_dram(nc, S, debug=(), ext_in=()):
    dr = {}
    dr["x"] = nc.dram_tensor("x", [S, D], F32, kind="ExternalInput")
    dr["mem"] = nc.dram_tensor("mem", [NMEM, D], F32, kind="ExternalInput")
    for n, shp in WEIGHT_SHAPES.items():
        dr[n] = nc.dram_tensor(n, list(shp), F32, kind="ExternalInput")
    for n, a in make_consts_host().items():
        dr[n] = nc.dram_tensor(n, list(a.shape), F32, kind="ExternalInput")
    dr["nab"] = nc.dram_tensor("nab", [DEPTH * 6, 128, 21 * 128], F32, kind="ExternalInput")

    def scratch(name, shape, dt):
        kind = "ExternalOutput" if name in debug else ("ExternalInput" if name in ext_in else "Internal")
        dr[name] = nc.dram_tensor(name, shape, dt, kind=kind)

    scratch("fm", [FM_ROWS, S], BF16)
    scratch("tm", [S, TM_COLS], BF16)
    scratch("gt", [S, 40], F32)
    scratch("hml", [S, 256], F32)
    scratch("ogd", [S, 384], F32)
    scratch("gqT", [384, S], BF16)
    scratch("gkT", [384, S], BF16)
    scratch("gk_tm", [S, 384], BF16)
    scratch("gv_tm", [S, 384], BF16)
    scratch("y", [S, D], BF16)
    scratch("xs", [S, D], F32)
    dr["out"] = nc.dram_tensor("out", [S, D], F32, kind="ExternalOutput")
    return dr


def load_w(k, dst, src2d, kc, ncols, split=4):
    v = src2d.rearrange("(c p) n -> p c n", p=128)
    for c0 in range(0, kc, split):
        k.dma("pool", dst[:, c0:c0 + split, :], v[:, c0:c0 + split, :])


def norm_to_T(k, xt, nwfull, hT, col0, hn_r, st_r, junk, pt_r, cst):
    hn = hn_r.next(); st = st_r.next()
    rmsnorm_rows(k, xt, hn, junk, st)
    pt = pt_r.next()
    for c in range(8):
        k.tr(pt[:, c, :], hn[:, c * 128:(c + 1) * 128], cst["ident_bf"][:])
    k.tt("dve", hT[:, :, col0:col0 + 128], pt[:], nwfull[:], ALU.mult)


def make_nwfull(k, es, src1d, ones):
    nc = k.nc
    nw = es.enter_context(nc.sbuf_tensor(k.name("nw"), [128, 8], F32))
    wfull = es.enter_context(nc.sbuf_tensor(k.name("wfull"), [128, 8, 128], BF16))
    k.dma("sp", nw[:], src1d.rearrange("(c p) -> p c", p=128), slow=True)
    for c in range(8):
        k.ts("dve", wfull[:, c, :], ones[:], nw[:, c:c + 1], ALU.mult)
    return wfull


def phase_mix_xattn(k, S, l, dr, cst, x_in, x_out):
    nc = k.nc
    NG = S // 512
    with ExitStack() as es:
        wout = es.enter_context(nc.sbuf_tensor(k.name("wout"), [128, 8, D], BF16))
        wxq = es.enter_context(nc.sbuf_tensor(k.name("wxq"), [128, 8, D], BF16))
        wxo = es.enter_context(nc.sbuf_tensor(k.name("wxo"), [128, 8, D], BF16))
        wkv = es.enter_context(nc.sbuf_tensor(k.name("wkv"), [128, 8, 2 * D], BF16))
        kkT = es.enter_context(nc.sbuf_tensor(k.name("kkT"), [128, 8, NMEM], BF16))
        vv = es.enter_context(nc.sbuf_tensor(k.name("vv"), [128, 2, D], BF16))
        memT = es.enter_context(nc.sbuf_tensor(k.name("memT"), [128, 8, NMEM], BF16))
        ones = es.enter_context(nc.sbuf_tensor(k.name("ones"), [128, 128], F32))
        ones_bf = es.enter_context(nc.sbuf_tensor(k.name("onesb"), [128, 128], BF16))
        xg = es.enter_context(nc.sbuf_tensor(k.name("xg"), [128, 4, D], F32))
        yt_r = Ring(k, es, "yt", 2, [128, D], BF16)
        yT = es.enter_context(nc.sbuf_tensor(k.name("yT"), [128, 8, 512], BF16))
        h2T = es.enter_context(nc.sbuf_tensor(k.name("h2T"), [128, 8, 512], BF16))
        qT = es.enter_context(nc.sbuf_tensor(k.name("qT"), [128, 8, 512], BF16))
        oT = es.enter_context(nc.sbuf_tensor(k.name("oT"), [128, 8, 512], BF16))
        PT_r = Ring(k, es, "PT", 4, [128, 512], BF16)
        rden_r = Ring(k, es, "rden", 2, [128, 512], F32)
        hn_r = Ring(k, es, "hn", 2, [128, D], BF16)
        junk = es.enter_context(nc.sbuf_tensor(k.name("junk"), [128, D], BF16))
        st_r = Ring(k, es, "st", 2, [128, 4], F32)
        mt_r = Ring(k, es, "mt", 2, [128, D], F32)
        pt_r = Ring(k, es, "ptr", 2, [128, 8, 128], BF16, psum=True)
        pm_r = Ring(k, es, "pmm", 5, [128, 512], F32, psum=True)

        k.memset("dve", ones[:], 1.0)
        k.memset("dve", ones_bf[:], 1.0)
        load_w(k, wkv, dr["w_xkv"].ap()[l], 8, 2 * D, split=2)
        load_w(k, wout, dr["w_out"].ap()[l], 8, D)
        load_w(k, wxq, dr["w_xq"].ap()[l], 8, D)
        load_w(k, wxo, dr["w_xo"].ap()[l], 8, D)
        nw_mem = make_nwfull(k, es, dr["norm_mem_w"].ap()[l], ones)
        nw_xa = make_nwfull(k, es, dr["norm_xa_w"].ap()[l], ones)
        msrc = dr["mem"].ap().rearrange("(n p) d -> n p d", p=128)
        for mt in range(2):
            m_t = mt_r.next()
            k.dma("sp", m_t[:], msrc[mt])
            norm_to_T(k, m_t, nw_mem, memT, mt * 128, hn_r, st_r, junk, pt_r, cst)
        ev = 0
        for m in range(8):
            ps = pm_r.next()
            for c in range(8):
                k.mm(ps[:, 0:NMEM], wkv[:, c, m * 128:(m + 1) * 128], memT[:, c, :], start=(c == 0), stop=(c == 7))
            k.copy("act", kkT[:, m, :], ps[:, 0:NMEM])
        for mt in range(2):
            for n in range(2):
                ps = pm_r.next()
                for c in range(8):
                    k.mm(ps[:], memT[:, c, mt * 128:(mt + 1) * 128], wkv[:, c, D + n * 512:D + (n + 1) * 512],
                         start=(c == 0), stop=(c == 7))
                k.copy("dve", vv[:, mt, n * 512:(n + 1) * 512], ps[:])

        ysrc = dr["y"].ap().rearrange("(n p) d -> n p d", p=128)
        xsrc = x_in.ap().rearrange("(n p) d -> n p d", p=128)
        xdst = x_out.ap().rearrange("(n p) d -> n p d", p=128)
        for g in range(NG):
            for j in range(4):
                t = g * 4 + j
                yt = yt_r.next()
                k.dma("sp", yt[:], ysrc[t])
                k.dma("sp", xg[:, j, :], xsrc[t])
                pt = pt_r.next()
                for c in range(8):
                    k.tr(pt[:, c, :], yt[:, c * 128:(c + 1) * 128], cst["ident_bf"][:])
                k.copy("act", yT[:, :, j * 128:(j + 1) * 128], pt[:])
            for j in range(4):
                for n in range(2):
                    ps = pm_r.next()
                    for c in range(8):
                        k.mm(ps[:], yT[:, c, j * 128:(j + 1) * 128], wout[:, c, n * 512:(n + 1) * 512],
                             start=(c == 0), stop=(c == 7))
                    k.tt("dve", xg[:, j, n * 512:(n + 1) * 512], ps[:], xg[:, j, n * 512:(n + 1) * 512], ALU.add)
                norm_to_T(k, xg[:, j, :], nw_xa, h2T, j * 128, hn_r, st_r, junk, pt_r, cst)
            for m in range(8):
                ps = pm_r.next()
                for c in range(8):
                    k.mm(ps[:], wxq[:, c, m * 128:(m + 1) * 128], h2T[:, c, :], start=(c == 0), stop=(c == 7))
                k.copy("act" if m % 2 == 0 else "dve", qT[:, m, :], ps[:])
            for hh in range(4):
                PTs = []
                for mt in range(2):
                    ps = pm_r.next()
                    for dc in range(2):
                        k.mm(ps[:], kkT[:, 2 * hh + dc, mt * 128:(mt + 1) * 128], qT[:, 2 * hh + dc, :],
                             start=(dc == 0), stop=(dc == 1))
                    PT = PT_r.next()
                    k.act(PT[:], ps[:], AF.Exp, scale=1.0 / 16.0)
                    PTs.append(PT)
                ps = pm_r.next()
                for mt in range(2):
                    k.mm(ps[:], ones_bf[:], PTs[mt][:], start=(mt == 0), stop=(mt == 1))
                rden = rden_r.next()
                k.recip(rden[:], ps[:])
                for dc in range(2):
                    ps = pm_r.next()
                    for mt in range(2):
                        k.mm(ps[:], vv[:, mt, hh * 256 + dc * 128:hh * 256 + (dc + 1) * 128], PTs[mt][:],
                             start=(mt == 0), stop=(mt == 1))
                    k.tt("dve", oT[:, 2 * hh + dc, :], ps[:], rden[:], ALU.mult)
            for j in range(4):
                t = g * 4 + j
                for n in range(2):
                    ps = pm_r.next()
                    for c in range(8):
                        k.mm(ps[:], oT[:, c, j * 128:(j + 1) * 128], wxo[:, c, n * 512:(n + 1) * 512],
                             start=(c == 0), stop=(c == 7))
                    k.tt("dve", xg[:, j, n * 512:(n + 1) * 512], ps[:], xg[:, j, n * 512:(n + 1) * 512], ALU.add)
                k.dma("pool", xdst[t], xg[:, j, :])
        k.P.barrier()
        k.P.emit()


def phase_ffn(k, S, l, dr, cst, x_io, final_out=None):
    nc = k.nc
    GT = 2
    NG = S // (128 * GT)
    with ExitStack() as es:
        w1 = es.enter_context(nc.sbuf_tensor(k.name("w1"), [128, 8, DFF], BF16))
        w2 = es.enter_context(nc.sbuf_tensor(k.name("w2"), [128, 32, D], BF16))
        ones = es.enter_context(nc.sbuf_tensor(k.name("ones"), [128, 128], F32))
        xg = es.enter_context(nc.sbuf_tensor(k.name("xg"), [128, GT, D], F32))
        h3T = es.enter_context(nc.sbuf_tensor(k.name("h3T"), [128, 8, 128 * GT], BF16))
        uT = es.enter_context(nc.sbuf_tensor(k.name("uT"), [128, 32, 128 * GT], BF16))
        r_r = Ring(k, es, "relu", 3, [128, 128 * GT], BF16)
        hn_r = Ring(k, es, "hn", 2, [128, D], BF16)
        junk = es.enter_context(nc.sbuf_tensor(k.name("junk"), [128, D], BF16))
        st_r = Ring(k, es, "st", 2, [128, 4], F32)
        pt_r = Ring(k, es, "ptr", 2, [128, 8, 128], BF16, psum=True)
        pm_r = Ring(k, es, "pmm", 5, [128, 512], F32, psum=True)
        k.memset("dve", ones[:], 1.0)
        load_w(k, w1, dr["w_ff1"].ap()[l], 8, DFF, split=1)
        load_w(k, w2, dr["w_ff2"].ap()[l], 32, D, split=4)
        nw = make_nwfull(k, es, dr["norm_ffn_w"].ap()[l], ones)
        if final_out is not None:
            nwo = es.enter_context(nc.sbuf_tensor(k.name("nwo"), [128, D], F32))
            src = dr["norm_out_w"]
            k.dma("sp", nwo[:], bass.AP(src, 0, [[0, 128], [1, D]]))
            fo_r = Ring(k, es, "fo", 2, [128, D], F32)
            fdst = final_out.ap().rearrange("(n p) d -> n p d", p=128)
        xv = x_io.ap().rearrange("(n p) d -> n p d", p=128)
        W = 128 * GT
        for g in range(NG):
            for j in range(GT):
                t = g * GT + j
                k.dma("sp", xg[:, j, :], xv[t])
                norm_to_T(k, xg[:, j, :], nw, h3T, j * 128, hn_r, st_r, junk, pt_r, cst)
            for f in range(32):
                ps = pm_r.next()
                for c in range(8):
                    k.mm(ps[:, 0:W], w1[:, c, f * 128:(f + 1) * 128], h3T[:, c, :], start=(c == 0), stop=(c == 7))
                r = r_r.next()
                k.act(r[:], ps[:, 0:W], AF.Relu)
                k.tt("pool" if f % 2 == 0 else "dve", uT[:, f, :], r[:], r[:], ALU.mult)
            for j in range(GT):
                t = g * GT + j
                for n in range(2):
                    ps = pm_r.next()
                    for f in range(32):
                        k.mm(ps[:], uT[:, f, j * 128:(j + 1) * 128], w2[:, f, n * 512:(n + 1) * 512],
                             start=(f == 0), stop=(f == 31))
                    k.tt("dve", xg[:, j, n * 512:(n + 1) * 512], ps[:], xg[:, j, n * 512:(n + 1) * 512], ALU.add)
                if final_out is None:
                    k.dma("pool", xv[t], xg[:, j, :])
                else:
                    st = st_r.next(); fo = fo_r.next()
                    k.act(junk[:], xg[:, j, :], AF.Square, accum=st[:, 0:1])
                    k.act(st[:, 1:2], st[:, 0:1], AF.Sqrt, bias=EPS, scale=1.0 / D)
                    k.recip(st[:, 2:3], st[:, 1:2])
                    k.stt(fo[:], xg[:, j, :], st[:, 2:3], nwo[:], ALU.mult, ALU.mult)
                    k.dma("pool", fdst[t], fo[:])
        k.P.barrier()
        k.P.emit()


NA_CLASSES = {"int": (0, [-2, -1, 0, 1, 2]), "top0": (5, [0, 1, 2, 3]), "top1": (9, [-1, 0, 1, 2]),
              "bot1": (13, [-2, -1, 0, 1]), "bot0": (17, [-3, -2, -1, 0])}
NEG = -30000.0


def na_class(qt, NT):
    if qt == 0:
        return "top0"
    if qt == 1:
        return "top1"
    if qt == NT - 2:
        return "bot1"
    if qt == NT - 1:
        return "bot0"
    return "int"


def na_bias_host(rel_bias, S):
    L = rel_bias.shape[0]
    R, NT = S // 64, S // 128
    out = np.full((L, 6, 21, 128, 128), NEG, np.float32)
    rep = {"int": 2, "top0": 0, "top1": 1, "bot1": NT - 2, "bot0": NT - 1}
    j = np.arange(128)
    for cls, (base, offs) in NA_CLASSES.items():
        qt = rep[cls]
        for n, o in enumerate(offs):
            kt = qt + o
            kr, kc = 2 * kt + j // 64, j % 64
            qr, qc = 2 * qt + j // 64, j % 64
            r0 = np.clip(qr - 4, 0, R - 8)
            c0 = np.clip(qc - 8, 0, 48)
            inw = ((kr[:, None] >= r0[None, :]) & (kr[:, None] <= r0[None, :] + 7) &
                   (kc[:, None] >= c0[None, :]) & (kc[:, None] <= c0[None, :] + 15))
            drr = np.clip(kr[:, None] - qr[None, :] + 7, 0, 14)
            dcc = np.clip(kc[:, None] - qc[None, :] + 15, 0, 30)
            vals = rel_bias[:, :, drr, dcc]
            out[:, :, base + n] = np.where(inw[None, None], vals, np.float32(NEG))
    return np.ascontiguousarray(out.transpose(0, 1, 3, 2, 4).reshape(L * 6, 128, 21 * 128))


def phase_na(k, S, l, dr, cst):
    nc = k.nc
    NT = S // 128
    with ExitStack() as es:
        qT_r = Ring(k, es, "naq", 2, [64, S], BF16)
        kT_r = Ring(k, es, "nak", 2, [64, S], BF16)
        va_r = Ring(k, es, "nav", 2, [128, NT, 65], BF16)
        nb_r = Ring(k, es, "nab", 2, [128, 21 * 128], F32)
        lg_r = Ring(k, es, "nalg", 2, [128, 640], F32)
        PT_r = Ring(k, es, "naPT", 2, [128, 640], BF16)
        yo_r = Ring(k, es, "nayo", 3, [128, 64], BF16)
        rd_r = Ring(k, es, "nard", 3, [128, 1], F32)
        psA_r = Ring(k, es, "psA", 2, [128, 512], F32, psum=True)
        psB_r = Ring(k, es, "psB", 2, [128, 512], F32, psum=True)
        po_r = Ring(k, es, "pso", 2, [128, 512], F32, psum=True)
        fm = dr["fm"].ap()
        tmv = dr["tm"].ap().rearrange("(n p) c -> p n c", p=128)
        yv = dr["y"].ap().rearrange("(n p) c -> n p c", p=128)
        for h in range(6):
            qT = qT_r.next(); kT = kT_r.next(); va = va_r.next(); nb = nb_r.next()
            k.dma("sp", qT[:], fm[FM_NAQ + h * 64:FM_NAQ + (h + 1) * 64, :])
            k.dma("sp", kT[:], fm[FM_NAK + h * 64:FM_NAK + (h + 1) * 64, :])
            for t0 in range(0, NT, 8):
                k.dma("sp", va[:, t0:t0 + 8, 0:64], tmv[:, t0:t0 + 8, TM_NAV + h * 64:TM_NAV + (h + 1) * 64])
            k.memset("pool", va[:, :, 64:65], 1.0)
            k.dma("sp", nb[:], dr["nab"].ap()[l * 6 + h])
            for qt in range(NT):
                base, offs = NA_CLASSES[na_class(qt, NT)]
                n = len(offs)
                psA = psA_r.next(); psB = psB_r.next(); lg = lg_r.next(); PT = PT_r.next()
                for i, o in enumerate(offs):
                    kt = qt + o
                    dst = psA[:, i * 128:(i + 1) * 128] if i < 4 else psB[:, 0:128]
                    k.mm(dst, kT[:, kt * 128:(kt + 1) * 128], qT[:, qt * 128:(qt + 1) * 128])
                na = min(n, 4) * 128
                k.stt(lg[:, 0:na], psA[:, 0:na], 0.125, nb[:, base * 128:base * 128 + na], ALU.mult, ALU.add)
                k.act(PT[:, 0:na], lg[:, 0:na], AF.Exp)
                if n == 5:
                    k.stt(lg[:, 512:640], psB[:, 0:128], 0.125, nb[:, (base + 4) * 128:(base + 5) * 128],
                          ALU.mult, ALU.add)
                    k.act(PT[:, 512:640], lg[:, 512:640], AF.Exp)
                po = po_r.next()
                for i, o in enumerate(offs):
                    kt = qt + o
                    k.mm(po[:, 0:65], PT[:, i * 128:(i + 1) * 128], va[:, kt, :], start=(i == 0), stop=(i == n - 1))
                rd = rd_r.next(); yo = yo_r.next()
                k.recip(rd[:], po[:, 64:65])
                k.ts("dve", yo[:], po[:, 0:64], rd[:], ALU.mult)
                k.dma("pool", yv[qt][:, h * 64:(h + 1) * 64], yo[:])
        k.P.barrier()
        k.P.emit()


def bcast_rows(k, dst, src_handle, off, n):
    k.dma("sp", dst, bass.AP(src_handle, off, [[0, 128], [1, n]]))


class GatePool:
    def __init__(self, k, es, l, dr, cst):
        nc = k.nc
        self.k, self.cst, self.dr = k, cst, dr
        self.bias = es.enter_context(nc.sbuf_tensor(k.name("gbias"), [128, 40], F32))
        self.nA = es.enter_context(nc.sbuf_tensor(k.name("gnA"), [128, 12], F32))
        self.ones = es.enter_context(nc.sbuf_tensor(k.name("gones"), [128, 128], F32))
        k.memset("dve", self.bias[:], 0.0)
        k.memset("dve", self.ones[:], 1.0)
        bcast_rows(k, self.bias[:, 0:8], dr["ml_i_bias"], l * 8, 8)
        bcast_rows(k, self.bias[:, 8:16], dr["ml_f_bias"], l * 8, 8)
        bcast_rows(k, self.bias[:, 28:40], dr["gdn_dt_bias"], l * 12, 12)
        bcast_rows(k, self.nA[:], dr["gdn_a_log"], l * 12, 12)
        k.act(self.nA[:], self.nA[:], AF.Exp)
        k.ts("dve", self.nA[:], self.nA[:], -1.0, ALU.mult)
        self.gt_r = Ring(k, es, "gtt", 2, [128, 40], F32)
        self.a_r = Ring(k, es, "gta", 2, [128, 40], F32)
        self.e_r = Ring(k, es, "gte", 2, [128, 40], F32)
        self.sp_r = Ring(k, es, "gtsp", 2, [128, 40], F32)
        self.w_r = Ring(k, es, "gtw", 2, [128, 32], F32)
        self.x_r = Ring(k, es, "gtx", 2, [128, 32], F32)
        self.bt_r = Ring(k, es, "gtbt", 2, [64, 8], F32)
        self.pg_r = Ring(k, es, "gtpg", 1, [128, 512], F32, psum=True)

    def pre(self, t):
        k = self.k
        gt = self.gt_r.next(); a = self.a_r.next(); e = self.e_r.next(); sp = self.sp_r.next()
        k.dma("sp", gt[:], self.dr["gt"].ap()[t * 128:(t + 1) * 128, :])
        k.tt("dve", a[:], gt[:], self.bias[:], ALU.add)
        k.act(e[:, 8:28], a[:, 8:28], AF.Exp, scale=-1.0)
        k.act(e[:, 28:40], a[:, 28:40], AF.Exp)
        k.act(sp[:, 8:40], e[:, 8:40], AF.Ln, bias=1.0)
        return a, sp

    def mlstm(self, t, d):
        k = self.k
        a, sp = self.pre(t)
        pg = self.pg_r.next()
        lo = 8 + d * 4
        k.mm(pg[:, 0:4], self.cst["c_masks"][:, d, :], sp[:, lo:lo + 4])
        k.mm(pg[0:64, 8:12], self.ones[:, 0:64], sp[:, lo:lo + 4])
        w = self.w_r.next(); x = self.x_r.next(); bt = self.bt_r.next()
        k.ts("dve", w[:, 0:4], pg[:, 0:4], -1.0, ALU.mult)
        k.tt("dve", w[:, 4:8], pg[:, 0:4], a[:, d * 4:d * 4 + 4], ALU.add)
        k.act(x[:, 0:8], w[:, 0:8], AF.Exp)
        k.act(bt[:, 0:4], pg[0:64, 8:12], AF.Exp, scale=-1.0)
        return x, bt

    def gdn(self, t, d):
        k = self.k
        a, sp = self.pre(t)
        w = self.w_r.next(); x = self.x_r.next(); bt = self.bt_r.next()
        g = self.e_r.next()
        lo = 28 + d * 6
        k.tt("dve", g[:, 0:6], sp[:, lo:lo + 6], self.nA[:, d * 6:d * 6 + 6], ALU.mult)
        pg = self.pg_r.next()
        k.mm(pg[:, 0:6], self.cst["c_masks"][:, d, :], g[:, 0:6])
        k.mm(pg[:, 8:14], self.ones[:], g[:, 0:6])
        lb = 16 + d * 6
        k.copy("dve", w[:, 0:6], pg[:, 0:6])
        k.tt("dve", w[:, 6:12], pg[:, 0:6], sp[:, lb:lb + 6], ALU.subtract)
        k.tt("dve", w[:, 12:18], pg[:, 8:14], w[:, 0:6], ALU.subtract)
        k.ts("dve", w[:, 18:24], sp[:, lb:lb + 6], -1.0, ALU.mult)
        k.act(x[:, 0:24], w[:, 0:24], AF.Exp)
        k.act(bt[:, 0:6], pg[0:64, 8:14], AF.Exp)
        return w, x, bt


def phase_mlstm(k, S, l, dr, cst):
    nc = k.nc
    NT = S // 128
    with ExitStack() as es:
        GP = GatePool(k, es, l, dr, cst)
        qT_r = Ring(k, es, "mlq", 2, [64, 4, 128], BF16)
        kT_r = Ring(k, es, "mlk", 2, [64, 4, 128], BF16)
        ktm_r = Ring(k, es, "mlktm", 2, [128, 256], BF16)
        va_r = Ring(k, es, "mlva", 2, [128, 4, 65], BF16)
        vp_r = Ring(k, es, "mlvp", 3, [128, 65], BF16)
        pm_r = Ring(k, es, "mlpm", 3, [128, 128], BF16)
        C32 = [es.enter_context(nc.sbuf_tensor(k.name("C32"), [64, 65], F32)) for _ in range(4)]
        Cbf = [es.enter_context(nc.sbuf_tensor(k.name("Cbf"), [64, 65], BF16)) for _ in range(4)]
        tmp_r = Ring(k, es, "mltmp", 3, [64, 65], F32)
        sm_r = Ring(k, es, "mlsm", 4, [128, 4], F32)
        hb_r = Ring(k, es, "mlh", 2, [128, 256], F32)
        hf_r = Ring(k, es, "mlhf", 2, [128, 256], F32)
        ot_r = Ring(k, es, "mlo", 2, [128, 256], BF16)
        gw_r = Ring(k, es, "mlgw", 2, [128, 256], F32)
        yo_r = Ring(k, es, "mly", 2, [128, 256], BF16)
        junk = es.enter_context(nc.sbuf_tensor(k.name("mljunk"), [128, 64], F32))
        nwb = es.enter_context(nc.sbuf_tensor(k.name("mlnw"), [128, 256], F32))
        ps_r = Ring(k, es, "mlps", 2, [128, 512], F32, psum=True)
        po_r = Ring(k, es, "mlpo", 2, [128, 512], F32, psum=True)
        pc_r = Ring(k, es, "mlpc", 2, [128, 512], F32, psum=True)
        bcast_rows(k, nwb[:], dr["ml_norm_w"], l * 256, 256)
        for t_ in va_r.t:
            k.memset("dve", t_[:, :, 64:65], 1.0)
        fm = dr["fm"].ap()
        tmv = dr["tm"].ap().rearrange("(n p) c -> n p c", p=128)
        hml = dr["hml"].ap().rearrange("(n p) c -> n p c", p=128)
        yv = dr["y"].ap().rearrange("(n p) c -> n p c", p=128)
        for d in range(2):
            for h in range(4):
                k.memset("dve", C32[h][:], 0.0)
                k.memset("dve", Cbf[h][:], 0.0)
            mask = cst["c_masks"][:, d, :]
            order = range(NT) if d == 0 else range(NT - 1, -1, -1)
            for t in order:
                ex, ebt = GP.mlstm(t, d)
                qT = qT_r.next(); kT = kT_r.next(); ktm = ktm_r.next(); va = va_r.next()
                cs = slice(t * 128, (t + 1) * 128)
                k.dma("sp", qT[:], fm[FM_MLQ:FM_MLQ + 256, cs].rearrange("(h d) t -> d h t", d=64))
                k.dma("sp", kT[:], fm[FM_MLK:FM_MLK + 256, cs].rearrange("(h d) t -> d h t", d=64))
                k.dma("sp", ktm[:], tmv[t][:, TM_MLK:TM_MLK + 256])
                k.dma("sp", va[:, :, 0:64], tmv[t][:, TM_MLV:TM_MLV + 256].rearrange("p (h d) -> p h d", d=64))
                hb = hb_r.next()
                for h in range(4):
                    vp = vp_r.next()
                    k.ts("dve", vp[:], va[:, h, :], ex[:, 4 + h:5 + h], ALU.mult, 0.125, ALU.mult)
                    ps = ps_r.next()
                    k.mm(ps[:, 0:128], kT[:, h, :], qT[:, h, :])
                    pm = pm_r.next()
                    k.tt("dve", pm[:], ps[:, 0:128], mask, ALU.mult)
                    po = po_r.next()
                    k.mm(po[:, 0:65], pm[:], vp[:], start=True, stop=False)
                    k.mm(po[:, 0:65], qT[:, h, :], Cbf[h][:], start=False, stop=True)
                    pc = pc_r.next()
                    k.mm(pc[0:64, 0:65], ktm[:, h * 64:(h + 1) * 64], vp[:])
                    tmp = tmp_r.next()
                    k.tt("dve", tmp[:], pc[0:64, 0:65], C32[h][:], ALU.add)
                    k.ts("dve", C32[h][:], tmp[:], ebt[:, h:h + 1], ALU.mult)
                    k.act(Cbf[h][:], tmp[:], AF.Copy, scale=ebt[:, h:h + 1])
                    sm = sm_r.next()
                    k.act(sm[:, 3:4], po[:, 64:65], AF.Abs, scale=ex[:, h:h + 1])
                    k.ts("dve", sm[:, 0:1], sm[:, 3:4], 1.0, ALU.max)
                    k.recip(sm[:, 1:2], sm[:, 0:1])
                    k.tt("dve", sm[:, 2:3], sm[:, 1:2], ex[:, h:h + 1], ALU.mult)
                    k.ts("dve", hb[:, h * 64:(h + 1) * 64], po[:, 0:64], sm[:, 2:3], ALU.mult)
                if d == 0:
                    k.dma("pool", hml[t], hb[:])
                else:
                    hf = hf_r.next(); ot = ot_r.next(); gw = gw_r.next(); yo = yo_r.next()
                    k.dma("sp", hf[:], hml[t])
                    k.dma("sp", ot[:], tmv[t][:, TM_MLO:TM_MLO + 256])
                    k.tt("dve", hb[:], hb[:], hf[:], ALU.add)
                    k.act(gw[:], ot[:], AF.Sigmoid)
                    k.tt("dve", gw[:], gw[:], nwb[:], ALU.mult)
                    sm = sm_r.next()
                    for h in range(4):
                        k.act(junk[:], hb[:, h * 64:(h + 1) * 64], AF.Square, accum=sm[:, h:h + 1])
                    sm2 = sm_r.next()
                    k.act(sm2[:], sm[:], AF.Sqrt, bias=EPS, scale=1.0 / 64.0)
                    sm3 = sm_r.next()
                    k.recip(sm3[:], sm2[:])
                    for h in range(4):
                        k.stt(yo[:, h * 64:(h + 1) * 64], hb[:, h * 64:(h + 1) * 64], sm3[:, h:h + 1],
                              gw[:, h * 64:(h + 1) * 64], ALU.mult, ALU.mult)
                    k.dma("pool", yv[t][:, 384:640], yo[:])
        k.P.barrier()
        k.P.emit()


def phase_gdn_prep(k, S, l, dr, cst):
    nc = k.nc
    NCH = S // 512
    with ExitStack() as es:
        cwr = es.enter_context(nc.sbuf_tensor(k.name("cwr"), [5, 1152], F32))
        cw = es.enter_context(nc.sbuf_tensor(k.name("cw"), [128, 9, 5], F32))
        xin_r = Ring(k, es, "gxin", 2, [128, 516], BF16)
        acc_r = Ring(k, es, "gacc", 2, [128, 512], F32)
        s_r = Ring(k, es, "gs", 2, [128, 512], F32)
        sq_r = Ring(k, es, "gsq", 2, [128, 512], BF16)
        rt_r = Ring(k, es, "grt", 2, [128, 512], F32)
        sn_r = Ring(k, es, "gsn", 2, [128, 512], BF16)
        st_r = Ring(k, es, "gst", 2, [128, 4, 128], BF16)
        pcw = es.enter_context(nc.psum_tensor(k.name("pcw"), [128, 512], F32))
        ps_r = Ring(k, es, "gpps", 2, [128, 512], F32, psum=True)
        pt_r = Ring(k, es, "gppt", 2, [128, 4, 128], BF16, psum=True)
        k.dma("sp", cwr[:], dr["gdn_conv_w"].ap()[l])
        for g in range(9):
            k.tr(pcw[:, g * 8:g * 8 + 5], cwr[0:5, g * 128:(g + 1) * 128], cst["ident_f"][0:5, 0:5])
        for g in range(9):
            k.copy("dve", cw[:, g, :], pcw[:, g * 8:g * 8 + 5])
        fm = dr["fm"].ap()
        dsts = {0: dr["gqT"], 1: dr["gkT"]}
        tms = {1: dr["gk_tm"], 2: dr["gv_tm"]}
        for g in range(9):
            kind, gi = g // 3, g % 3
            for c in range(NCH):
                xin = xin_r.next(); acc = acc_r.next(); s = s_r.next(); sn = sn_r.next()
                lo, hi = max(c * 512 - 2, 0), min(c * 512 + 514, S)
                off = lo - (c * 512 - 2)
                if c == 0:
                    k.memset("pool", xin[:, 0:2], 0.0)
                if c == NCH - 1:
                    k.memset("pool", xin[:, 514:516], 0.0)
                k.dma("sp", xin[:, off:off + hi - lo], fm[FM_GDQ + g * 128:FM_GDQ + (g + 1) * 128, lo:hi])
                k.ts("dve", acc[:], xin[:, 0:512], cw[:, g, 0:1], ALU.mult)
                for kk in range(1, 5):
                    k.stt(acc[:], xin[:, kk:kk + 512], cw[:, g, kk:kk + 1], acc[:], ALU.mult, ALU.add)
                k.act(s[:], acc[:], AF.Silu)
                if kind < 2:
                    sq = sq_r.next(); rt = rt_r.next(); ps = ps_r.next()
                    k.tt("pool", sq[:], s[:], s[:], ALU.mult)
                    k.mm(ps[:], cst["blk2_bf"][:], sq[:])
                    k.act(rt[:], ps[:], AF.Sqrt, bias=EPS)
                    k.recip(rt[:], rt[:])
                    if kind == 0:
                        k.stt(sn[:], s[:], 0.125, rt[:], ALU.mult, ALU.mult)
                    else:
                        k.tt("dve", sn[:], s[:], rt[:], ALU.mult)
                    k.dma("pool", dsts[kind].ap()[gi * 128:(gi + 1) * 128, c * 512:(c + 1) * 512], sn[:])
                else:
                    k.copy("pool", sn[:], s[:])
                if kind >= 1:
                    pt = pt_r.next(); st = st_r.next()
                    for j in range(4):
                        k.tr(pt[:, j, :], sn[:, j * 128:(j + 1) * 128], cst["ident_bf"][:])
                    k.copy("act", st[:], pt[:])
                    dv = tms[kind].ap().rearrange("(n p) c -> p n c", p=128)
                    k.dma("pool", dv[:, c * 4:(c + 1) * 4, gi * 128:(gi + 1) * 128], st[:])
        k.P.barrier()
        k.P.emit()


def phase_gdn(k, S, l, dr, cst, stage=9):
    nc = k.nc
    NT = S // 128
    M = cst["c_masks"]
    with ExitStack() as es:
        GP = GatePool(k, es, l, dr, cst)
        qT_r = Ring(k, es, "gdq", 2, [64, 6, 128], BF16)
        kT_r = Ring(k, es, "gdk", 2, [64, 6, 128], BF16)
        ktm_r = Ring(k, es, "gdktm", 2, [128, 384], BF16)
        vtm_r = Ring(k, es, "gdvtm", 2, [128, 384], BF16)
        dg_r = Ring(k, es, "gddg", 4, [128, 128], F32)
        xp_r = Ring(k, es, "gdxp", 9, [128, 128], F32)
        A_r = Ring(k, es, "gdA", 9, [128, 128], F32)
        B_r = Ring(k, es, "gdB", 9, [128, 128], F32)
        N_r = Ring(k, es, "gdN", 9, [128, 128], F32)
        at_r = Ring(k, es, "gdat", 6, [128, 128], BF16)
        sc_r = Ring(k, es, "gdsc", 2, [128, 2, 64], F32)
        ks_r = Ring(k, es, "gdks", 2, [128, 64], BF16)
        u_r = Ring(k, es, "gdu", 2, [128, 64], F32)
        wk_r = Ring(k, es, "gdwk", 2, [64, 128], BF16)
        vn_r = Ring(k, es, "gdvn", 2, [128, 64], BF16)
        o2_r = Ring(k, es, "gdo2", 2, [128, 64], F32)
        ob_r = Ring(k, es, "gdob", 2, [128, 384], F32)
        of_r = Ring(k, es, "gdof", 2, [128, 384], F32)
        z_r = Ring(k, es, "gdz", 2, [128, 384], BF16)
        gz_r = Ring(k, es, "gdgz", 2, [128, 384], F32)
        yo_r = Ring(k, es, "gdyo", 2, [128, 384], BF16)
        sm_r = Ring(k, es, "gdsm", 4, [128, 8], F32)
        junk = es.enter_context(nc.sbuf_tensor(k.name("gdjunk"), [128, 64], F32))
        nwb = es.enter_context(nc.sbuf_tensor(k.name("gdnw"), [128, 384], F32))
        S32 = [es.enter_context(nc.sbuf_tensor(k.name("S32"), [64, 64], F32)) for _ in range(6)]
        Sbf = [es.enter_context(nc.sbuf_tensor(k.name("Sbf"), [64, 64], BF16)) for _ in range(6)]
        pA_r = Ring(k, es, "gdpA", 1, [128, 512], F32, psum=True)
        pB_r = Ring(k, es, "gdpB", 1, [128, 512], F32, psum=True)
        pc_r = Ring(k, es, "gdpc", 3, [128, 512], F32, psum=True)
        pu_r = Ring(k, es, "gdpu", 1, [128, 512], F32, psum=True)
        pv_r = Ring(k, es, "gdpv", 1, [128, 512], F32, psum=True)
        bcast_rows(k, nwb[:], dr["gdn_norm_w"], l * 384, 384)
        idf, idb, ones = cst["ident_f"], cst["ident_bf"], GP.ones
        tmv = dr["tm"].ap().rearrange("(n p) c -> n p c", p=128)
        ktv = dr["gk_tm"].ap().rearrange("(n p) c -> n p c", p=128)
        vtv = dr["gv_tm"].ap().rearrange("(n p) c -> n p c", p=128)
        ogd = dr["ogd"].ap().rearrange("(n p) c -> n p c", p=128)
        yv = dr["y"].ap().rearrange("(n p) c -> n p c", p=128)
        for d in range(2):
            mA, mB, mC = (M[:, 2, :], M[:, 4, :], M[:, 6, :]) if d == 0 else (M[:, 3, :], M[:, 5, :], M[:, 7, :])
            for h in range(6):
                k.memset("dve", S32[h][:], 0.0)
                k.memset("dve", Sbf[h][:], 0.0)
            order = range(NT) if d == 0 else range(NT - 1, -1, -1)
            for t in order:
                raw, ex, egl = GP.gdn(t, d)
                qT = qT_r.next(); kT = kT_r.next(); ktm = ktm_r.next(); vtm = vtm_r.next()
                cs = slice(t * 128, (t + 1) * 128)
                k.dma("sp", qT[:], dr["gqT"].ap()[:, cs].rearrange("(h d) t -> d h t", d=64))
                k.dma("sp", kT[:], dr["gkT"].ap()[:, cs].rearrange("(h d) t -> d h t", d=64))
                k.dma("sp", ktm[:], ktv[t])
                k.dma("sp", vtm[:], vtv[t])
                ob = ob_r.next()
                for g0 in range(0, 6, 3):
                    grp = list(range(g0, g0 + 3))
                    st = {}
                    for h in grp:
                        pA = pA_r.next(); pB = pB_r.next()
                        k.mm(pA[:, 0:128], kT[:, h, :], kT[:, h, :])
                        k.mm(pA[:, 128:256], kT[:, h, :], qT[:, h, :])
                        dg1 = dg_r.next(); dg2 = dg_r.next()
                        k.act(dg1[:], idf[:], AF.Copy, scale=raw[:, h:h + 1])
                        k.act(dg2[:], idf[:], AF.Copy, scale=raw[:, 6 + h:7 + h])
                        k.mm(pB[:, 0:128], ones[:], dg1[:])
                        k.mm(pB[:, 128:256], ones[:], dg2[:])
                        xa = xp_r.next(); xb = xp_r.next(); xc = xp_r.next()
                        k.stt(xa[:], pB[:, 0:128], raw[:, 6 + h:7 + h], mA, ALU.subtract, ALU.add)
                        k.stt(xb[:], pB[:, 128:256], raw[:, h:h + 1], mB, ALU.subtract, ALU.add)
                        k.stt(xc[:], pB[:, 0:128], raw[:, h:h + 1], mC, ALU.subtract, ALU.add)
                        k.act(xa[:], xa[:], AF.Exp, scale=-1.0)
                        k.act(xb[:], xb[:], AF.Exp)
                        k.act(xc[:], xc[:], AF.Exp)
                        A = A_r.next(); B = B_r.next(); at = at_r.next(); N = N_r.next()
                        k.tt("dve", A[:], pA[:, 0:128], xa[:], ALU.mult)
                        k.tt("dve", B[:], pA[:, 0:128], xb[:], ALU.mult)
                        k.tt("dve", at[:], pA[:, 128:256], xc[:], ALU.mult)
                        k.tt("dve", N[:], idf[:], B[:], ALU.subtract)
                        st[h] = [A, B, N, at]
                    for lv in range(1, 7):
                        for hi, h in enumerate(grp):
                            A, B, N, at = st[h]
                            pc = pc_r.t[hi]
                            A2 = A_r.next()
                            k.mm(pc[:, 0:128], B[:], A[:])
                            if lv < 6:
                                B2 = B_r.next()
                                k.mm(pc[:, 128:256], A[:], B[:])
                            k.copy("act", A2[:], pc[:, 0:128])
                            if lv < 6:
                                k.copy("act" if lv % 2 == 0 else "dve", B2[:], pc[:, 128:256])
                            k.mm(pc[:, 256:384], A2[:], N[:])
                            N2 = N_r.next()
                            k.tt("dve", N2[:], pc[:, 256:384], N[:], ALU.add)
                            st[h] = [A2, B2 if lv < 6 else B, N2, at]
                    for h in grp:
                        A, B, N, at = st[h]
                        hs = slice(h * 64, (h + 1) * 64)
                        sc = sc_r.next()
                        k.act(sc[:, 0, :], vtm[:, hs], AF.Copy, scale=ex[:, 18 + h:19 + h])
                        k.act(sc[:, 1, :], ktm[:, hs], AF.Copy, scale=ex[:, 6 + h:7 + h])
                        ks = ks_r.next()
                        k.ts("dve", ks[:], ktm[:, hs], ex[:, 12 + h:13 + h], ALU.mult)
                        pu = pu_r.next()
                        k.mm(pu[:, 0:64], N[:], sc[:, 0, :])
                        k.mm(pu[0:64, 64:192], sc[:, 1, :], N[:])
                        u = u_r.next(); wk = wk_r.next()
                        k.copy("act", u[:], pu[:, 0:64])
                        k.copy("dve", wk[:], pu[0:64, 64:192])
                        pv = pv_r.next()
                        k.mm(pv[:, 0:64], wk[:], Sbf[h][:])
                        vn = vn_r.next()
                        k.tt("dve", vn[:], u[:], pv[:, 0:64], ALU.subtract)
                        k.mm(pv[:, 64:128], qT[:, h, :], Sbf[h][:])
                        k.mm(pv[:, 128:192], at[:], vn[:])
                        k.mm(pv[0:64, 192:256], ks[:], vn[:])
                        o2 = o2_r.next()
                        k.copy("act", o2[:], pv[:, 128:192])
                        k.stt(ob[:, hs], pv[:, 64:128], ex[:, h:h + 1], o2[:], ALU.mult, ALU.add)
                        k.stt(S32[h][:], S32[h][:], egl[:, h:h + 1], pv[0:64, 192:256], ALU.mult, ALU.add)
                        k.copy("act", Sbf[h][:], S32[h][:])
                if d == 0:
                    k.dma("pool", ogd[t], ob[:])
                else:
                    of = of_r.next(); z = z_r.next(); gz = gz_r.next(); yo = yo_r.next()
                    k.dma("sp", of[:], ogd[t])
                    k.dma("sp", z[:], tmv[t][:, TM_GDZ:TM_GDZ + 384])
                    k.tt("dve", ob[:], ob[:], of[:], ALU.add)
                    k.act(gz[:], z[:], AF.Silu)
                    k.tt("dve", gz[:], gz[:], nwb[:], ALU.mult)
                    sm = sm_r.next()
                    for h in range(6):
                        k.act(junk[:], ob[:, h * 64:(h + 1) * 64], AF.Square, accum=sm[:, h:h + 1])
                    sm2 = sm_r.next()
                    k.act(sm2[:, 0:6], sm[:, 0:6], AF.Sqrt, bias=EPS, scale=1.0 / 64.0)
                    sm3 = sm_r.next()
                    k.recip(sm3[:, 0:6], sm2[:, 0:6])
                    for h in range(6):
                        hs = slice(h * 64, (h + 1) * 64)
                        k.stt(yo[:, hs], ob[:, hs], sm3[:, h:h + 1], gz[:, hs], ALU.mult, ALU.mult)
                    k.dma("pool", yv[t][:, 640:1024], yo[:])
        k.P.barrier()
        k.P.emit()


S_FULL = 8192


def build_program(S):
    nc = bass.Bass("TRN2", target_bir_lowering=False)
    dr = declare_dram(nc, S)
    with ExitStack() as es:
        k = K(nc, es)
        cst = setup_consts(k, es, dr)
        for l in range(DEPTH):
            dr["x_cur"] = dr["x"] if l == 0 else dr["xs"]
            phase_inproj(k, S, l, dr, cst)
            phase_na(k, S, l, dr, cst)
            phase_mlstm(k, S, l, dr, cst)
            phase_gdn_prep(k, S, l, dr, cst)
            phase_gdn(k, S, l, dr, cst)
            phase_mix_xattn(k, S, l, dr, cst, dr["x_cur"], dr["xs"])
            phase_ffn(k, S, l, dr, cst, dr["xs"], final_out=(dr["out"] if l == DEPTH - 1 else None))
    return nc


def kernel(**inputs):
    x = np.asarray(inputs["x"], dtype=np.float32)
    mem = np.asarray(inputs["mem"], dtype=np.float32)
    B, S, _ = x.shape
    nc = build_program(S)
    shared = {n: np.ascontiguousarray(np.asarray(inputs[n], dtype=np.float32)) for n in WEIGHT_SHAPES}
    shared.update(make_consts_host())
    shared["nab"] = na_bias_host(np.asarray(inputs["na_rel_bias"], dtype=np.float32), S)
    in_maps = []
    for b in range(B):
        m = dict(shared)
        m["x"] = np.ascontiguousarray(x[b])
        m["mem"] = np.ascontiguousarray(mem[b])
        in_maps.append(m)
    res = run_bass_kernel_spmd(nc, in_maps, core_ids=list(range(B)))
    return np.stack([np.asarray(r["out"], dtype=np.float32) for r in res.results], axis=0)
```

```python
import numpy as np
from contextlib import ExitStack

import concourse.bass as bass
import concourse.mybir as mybir
from concourse.bass_utils import run_bass_kernel_spmd

F32 = mybir.dt.float32
BF16 = mybir.dt.bfloat16
AF = mybir.ActivationFunctionType
ALU = mybir.AluOpType
AX = mybir.AxisListType

ENGS = ("pe", "dve", "act", "pool", "sp")
SEM_CAP = 30000
N_DMA_SEM = 12


def _prod(v):
    r = 1
    for a in v:
        r *= int(a)
    return r


def region(ap):
    t = ap.tensor
    shape = [int(s) for s in t.shape]
    rowlen = _prod(shape[1:])
    off = int(ap.offset)
    p0 = off // rowlen
    c0 = off % rowlen
    p1, c1 = p0, c0
    for step, cnt in ap.ap:
        step, cnt = int(step), int(cnt)
        if cnt <= 1 or step == 0:
            continue
        ext = step * (cnt - 1)
        if step % rowlen == 0:
            p1 += ext // rowlen
        else:
            c1 += ext
    if c1 >= rowlen:
        tot = off + (p1 - p0) * rowlen + (c1 - c0)
        p1 = tot // rowlen
        c0, c1 = 0, rowlen - 1
    if type(t).__name__ == "PSumTensorHandle":
        return (t.name, 0, 127, 0, rowlen - 1)
    return (t.name, p0, p1, c0, c1)


def _overlap(a, b):
    return not (a[2] < b[1] or b[2] < a[1] or a[4] < b[3] or b[4] < a[3])


def _contains(a, b):
    return a[1] <= b[1] and a[2] >= b[2] and a[3] <= b[3] and a[4] >= b[4]


class Op:
    __slots__ = ("eng", "fn", "dma", "seq", "deps", "signal", "sig_idx", "dsem", "dval",
                 "clock", "prewait")


class Prog:
    def __init__(self, nc, es):
        self.nc = nc
        self.es = es
        self.ops = []
        self.recs = {}
        self.nseq = {e: 0 for e in ENGS}
        self.clock = {e: {x: 0 for x in ENGS} for e in ENGS}
        self.known_dma = {e: set() for e in ENGS}
        self.last_compute = {e: None for e in ENGS}
        self.pending_dma = []
        self.esems = {e: [] for e in ENGS}
        self.nsig = {e: 0 for e in ENGS}
        self.dsems = {}
        for q in ("sp", "act", "pool"):
            self.dsems[q] = [[es.enter_context(nc.semaphore(f"d_{q}_{i}")), 0, None]
                             for i in range(N_DMA_SEM)]
        self.dma_rr = {q: 0 for q in self.dsems}
        self.emitted = 0

    def _need(self, op, dep):
        if dep is op:
            return
        c = op.eng
        if dep.dma:
            if dep in self.known_dma[c]:
                return
            self.known_dma[c].add(dep)
            op.deps.append(dep)
            clk = dep.clock
        else:
            if self.clock[c][dep.eng] >= dep.seq:
                return
            op.deps.append(dep)
            dep.signal = True
            clk = dict(dep.clock)
            clk[dep.eng] = max(clk[dep.eng], dep.seq)
        mine = self.clock[c]
        for e in ENGS:
            if clk[e] > mine[e]:
                mine[e] = clk[e]

    def add(self, eng, fn, reads=(), writes=(), dma=False):
        op = Op()
        op.eng, op.fn, op.dma = eng, fn, dma
        op.deps, op.signal, op.sig_idx = [], False, None
        op.dsem = op.dval = None
        op.prewait = None
        self.nseq[eng] += 1
        op.seq = self.nseq[eng]
        if dma:
            q = eng
            i = self.dma_rr[q]
            self.dma_rr[q] = (i + 1) % N_DMA_SEM
            slot = self.dsems[q][i]
            if slot[2] is not None:
                self._need(op, slot[2])
            slot[1] += 16
            slot[2] = op
            op.dsem, op.dval = slot[0], slot[1]
        accs = ([(region(a), type(a.tensor).__name__ == "PSumTensorHandle") for a in reads] +
                [(region(a), True) for a in writes])
        for box, is_w in accs:
            for rbox, rop, rw in self.recs.get(box[0], ()):
                if _overlap(box, rbox) and (is_w or rw):
                    same = (rop.eng == eng and not rop.dma and not dma)
                    if same and eng == "pe":
                        pass
                    else:
                        self._need(op, rop)
        for box, is_w in accs:
            keep = []
            for rec in self.recs.get(box[0], ()):
                rbox, rop, rw = rec
                if is_w and _contains(box, rbox) and rop is not op:
                    continue
                if (not is_w) and (not rw) and rbox == box and rop.eng == eng and not rop.dma and not dma:
                    continue
                keep.append(rec)
            keep.append((box, op, is_w))
            self.recs[box[0]] = keep
        op.clock = dict(self.clock[eng])
        self.ops.append(op)
        if dma:
            self.pending_dma.append(op)
        else:
            self.last_compute[eng] = op
        return op

    def barrier(self):
        for e in ENGS:
            op = Op()
            op.eng, op.fn, op.dma = e, None, False
            op.deps, op.signal, op.sig_idx = [], False, None
            op.dsem = op.dval = None
            op.prewait = None
            self.nseq[e] += 1
            op.seq = self.nseq[e]
            for o in ENGS:
                lc = self.last_compute[o]
                if lc is not None:
                    self._need(op, lc)
            for q in self.dsems:
                for slot in self.dsems[q]:
                    if slot[2] is not None:
                        self._need(op, slot[2])
            op.clock = dict(self.clock[e])
            self.ops.append(op)
        self.pending_dma = []
        self.recs = {}

    def _sem_for(self, eng, idx):
        k = (idx - 1) // SEM_CAP
        while len(self.esems[eng]) <= k:
            self.esems[eng].append(self.es.enter_context(
                self.nc.semaphore(f"s_{eng}_{len(self.esems[eng])}")))
        return self.esems[eng][k], idx - k * SEM_CAP

    def emit(self):
        ops = self.ops[self.emitted:]
        self.emitted = len(self.ops)
        for op in ops:
            if op.signal and not op.dma:
                self.nsig[op.eng] += 1
                op.sig_idx = self.nsig[op.eng]
        per = {e: [o for o in ops if o.eng == e] for e in ENGS}
        prog = self

        def run(e, h):
            for op in per[e]:
                for d in op.deps:
                    if d.dma:
                        h.wait_ge(d.dsem, d.dval)
                    else:
                        s, v = prog._sem_for(d.eng, d.sig_idx)
                        h.wait_ge(s, v)
                if op.fn is None:
                    if op.signal:
                        s, v = prog._sem_for(e, op.sig_idx)
                        h.sem_inc(s, 1)
                    continue
                inst = op.fn(h)
                if op.dma:
                    inst.then_inc(op.dsem, 16)
                elif op.signal:
                    s, v = prog._sem_for(e, op.sig_idx)
                    inst.then_inc(s, 1)

        for op in ops:
            if op.signal and not op.dma:
                self._sem_for(op.eng, op.sig_idx)
        with self.nc.Block() as block:
            @block.tensor
            def _(h):
                run("pe", h)

            @block.vector
            def _(h):
                run("dve", h)

            @block.scalar
            def _(h):
                run("act", h)

            @block.gpsimd
            def _(h):
                run("pool", h)

            @block.sync
            def _(h):
                run("sp", h)


class K:
    def __init__(self, nc, es):
        self.nc = nc
        self.P = Prog(nc, es)
        self.uid = 0

    def name(self, base):
        self.uid += 1
        return f"{base}_{self.uid}"

    def mm(self, out, lhsT, rhs, start=True, stop=True):
        self.P.add("pe", lambda h: h.matmul(out, lhsT, rhs, start=start, stop=stop),
                   reads=[lhsT, rhs], writes=[out])

    def tr(self, out, in_, ident):
        self.P.add("pe", lambda h: h.transpose(out, in_, ident), reads=[in_, ident], writes=[out])

    def act(self, out, in_, func, bias=None, scale=None, accum=None):
        kw = {}
        rd = [in_]
        wr = [out]
        if bias is not None:
            kw["bias"] = bias
            if not isinstance(bias, (int, float)):
                rd.append(bias)
        if scale is not None:
            kw["scale"] = scale
            if not isinstance(scale, (int, float)):
                rd.append(scale)
        if accum is not None:
            kw["accum_out"] = accum
            wr.append(accum)
        self.P.add("act", lambda h: h.activation(out, in_, func, **kw), reads=rd, writes=wr)

    def ts(self, eng, out, in0, s1, op0, s2=None, op1=None, accum=None):
        rd = [in0]
        if not isinstance(s1, (int, float)):
            rd.append(s1)
        if s2 is not None and not isinstance(s2, (int, float)):
            rd.append(s2)
        wr = [out]
        kw = {}
        if op1 is not None:
            kw["op1"] = op1
        if accum is not None:
            kw["accum_out"] = accum
            wr.append(accum)
        self.P.add(eng, lambda h: h.tensor_scalar(out, in0, s1, s2, op0, **kw), reads=rd, writes=wr)

    def tt(self, eng, out, in0, in1, op):
        self.P.add(eng, lambda h: h.tensor_tensor(out, in0, in1, op), reads=[in0, in1], writes=[out])

    def stt(self, out, in0, scalar, in1, op0, op1):
        rd = [in0, in1]
        if not isinstance(scalar, (int, float)):
            rd.append(scalar)
        self.P.add("dve", lambda h: h.scalar_tensor_tensor(out, in0, scalar, in1, op0, op1),
                   reads=rd, writes=[out])

    def copy(self, eng, out, in_):
        if eng == "act":
            self.P.add("act", lambda h: h.copy(out, in_), reads=[in_], writes=[out])
        else:
            self.P.add(eng, lambda h: h.tensor_copy(out, in_), reads=[in_], writes=[out])

    def memset(self, eng, out, val):
        self.P.add(eng, lambda h: h.memset(out, val), reads=[], writes=[out])

    def recip(self, out, in_):
        self.P.add("dve", lambda h: h.reciprocal(out, in_), reads=[in_], writes=[out])

    def scan(self, out, d0, d1, init, op0, op1):
        self.P.add("dve", lambda h: h.tensor_tensor_scan(out, d0, d1, init, op0, op1),
                   reads=[d0, d1], writes=[out])

    def dma(self, q, out, in_, slow=False):
        if slow:
            self.P.add(q, lambda h: h.dma_start(out=out, in_=in_, allow_slow_non_contiguous=True),
                       reads=[in_], writes=[out], dma=True)
        else:
            self.P.add(q, lambda h: h.dma_start(out=out, in_=in_), reads=[in_], writes=[out], dma=True)


D = 1024
DEPTH = 2
P_IN = 3752
NMEM = 256
DFF = 4096
EPS = 1e-6
WIN_BLOCKS = [(0, 768, 0), (1152, 1664, 768), (2192, 3344, 1280), (2176, 2192, 2432),
              (3728, 3752, 2448), (768, 1152, 2472), (1664, 2176, 2856), (3344, 3728, 3368)]
FM_ROWS = 2432
TM_COLS = 1536
FM_NAQ, FM_NAK, FM_MLQ, FM_MLK, FM_GDQ, FM_GDK, FM_GDV = 0, 384, 768, 1024, 1280, 1664, 2048
TM_NAV, TM_MLV, TM_MLO, TM_GDZ, TM_MLK = 0, 384, 640, 896, 1280


class Ring:
    def __init__(self, k, es, name, n, shape, dtype, psum=False):
        mk = k.nc.psum_tensor if psum else k.nc.sbuf_tensor
        self.t = [es.enter_context(mk(k.name(name), shape, dtype)) for _ in range(n)]
        self.i = 0

    def next(self):
        t = self.t[self.i]
        self.i = (self.i + 1) % len(self.t)
        return t


def rmsnorm_rows(k, xt, hn, junk, st, eng_scale="dve"):
    k.act(junk[:], xt[:], AF.Square, accum=st[:, 0:1])
    k.act(st[:, 1:2], st[:, 0:1], AF.Sqrt, bias=EPS, scale=1.0 / D)
    k.recip(st[:, 2:3], st[:, 1:2])
    k.ts(eng_scale, hn[:], xt[:], st[:, 2:3], ALU.mult)


def phase_inproj(k, S, l, dr, cst):
    nc = k.nc
    NG = S // 512
    with ExitStack() as es:
        w = es.enter_context(nc.sbuf_tensor(k.name("win"), [128, 8, P_IN], BF16))
        nw = es.enter_context(nc.sbuf_tensor(k.name("nw"), [128, 8], F32))
        wfull = es.enter_context(nc.sbuf_tensor(k.name("wfull"), [128, 8, 128], BF16))
        ones = es.enter_context(nc.sbuf_tensor(k.name("ones"), [128, 128], F32))
        xt_r = Ring(k, es, "xt", 2, [128, D], F32)
        hn_r = Ring(k, es, "hn", 2, [128, D], BF16)
        junk = es.enter_context(nc.sbuf_tensor(k.name("junk"), [128, D], BF16))
        st_r = Ring(k, es, "st", 2, [128, 4], F32)
        hT_r = Ring(k, es, "hT", 2, [128, 8, 512], BF16)
        fmst_r = Ring(k, es, "fmst", 4, [128, 512], BF16)
        gst_r = Ring(k, es, "gst", 2, [12, 512], F32)
        tmst_r = Ring(k, es, "tmst", 2, [128, TM_COLS], BF16)
        gtst_r = Ring(k, es, "gtst", 2, [128, 40], F32)
        pt_r = Ring(k, es, "ptr", 2, [128, 8, 128], BF16, psum=True)
        pm_r = Ring(k, es, "pmm", 4, [128, 512], F32, psum=True)

        wsrc = dr["w_in"].ap()[l].rearrange("(c p) n -> p c n", p=128)
        for (a, b, m) in WIN_BLOCKS:
            for c0 in range(0, 8, 4):
                k.dma("pool", w[:, c0:c0 + 4, m:m + (b - a)], wsrc[:, c0:c0 + 4, a:b])
        k.dma("sp", nw[:], dr["norm_mix_w"].ap()[l].rearrange("(c p) -> p c", p=128), slow=True)
        k.memset("dve", ones[:], 1.0)
        for c in range(8):
            k.ts("dve", wfull[:, c, :], ones[:], nw[:, c:c + 1], ALU.mult)

        xsrc = dr["x_cur"].ap().rearrange("(n p) d -> n p d", p=128)
        fm = dr["fm"].ap()
        tm = dr["tm"].ap().rearrange("(n p) c -> n p c", p=128)
        ev = 0
        for g in range(NG):
            hT = hT_r.next()
            for j in range(4):
                t = g * 4 + j
                xt = xt_r.next(); hn = hn_r.next(); st = st_r.next()
                k.dma("sp", xt[:], xsrc[t])
                rmsnorm_rows(k, xt, hn, junk, st)
                pt = pt_r.next()
                for c in range(8):
                    k.tr(pt[:, c, :], hn[:, c * 128:(c + 1) * 128], cst["ident_bf"][:])
                k.tt("dve", hT[:, :, j * 128:(j + 1) * 128], pt[:], wfull[:], ALU.mult)
            for m in range(19):
                ps = pm_r.next()
                for c in range(8):
                    k.mm(ps[:], w[:, c, m * 128:(m + 1) * 128], hT[:, c, :], start=(c == 0), stop=(c == 7))
                stg = fmst_r.next()
                k.copy("act" if ev % 2 == 0 else "dve", stg[:], ps[:])
                ev += 1
                k.dma("pool", fm[m * 128:(m + 1) * 128, g * 512:(g + 1) * 512], stg[:])
            for j in range(4):
                t = g * 4 + j
                stg = tmst_r.next()
                for (c0, n, o) in [(2472, 512, 0), (2984, 512, 512), (3496, 256, 1024), (1024, 256, 1280)]:
                    ps = pm_r.next()
                    for c in range(8):
                        k.mm(ps[:, 0:n], hT[:, c, j * 128:(j + 1) * 128], w[:, c, c0:c0 + n],
                             start=(c == 0), stop=(c == 7))
                    k.copy("act" if ev % 2 == 0 else "dve", stg[:, o:o + n], ps[:, 0:n])
                    ev += 1
                k.dma("pool", tm[t], stg[:])
                ps = pm_r.next()
                for c in range(8):
                    k.mm(ps[:, 0:40], hT[:, c, j * 128:(j + 1) * 128], w[:, c, 2432:2472],
                         start=(c == 0), stop=(c == 7))
                gs = gtst_r.next()
                k.copy("dve", gs[:], ps[:, 0:40])
                k.dma("pool", dr["gt"].ap()[t * 128:(t + 1) * 128, :], gs[:])
        k.P.barrier()
        k.P.emit()


def make_consts_host():
    c = {}
    c["c_ident"] = np.eye(128, dtype=np.float32)
    p = np.arange(128)[:, None]
    f = np.arange(128)[None, :]
    big = np.float32(30000.0)
    z = np.float32(0.0)
    c["c_masks"] = np.stack([
        (p <= f).astype(np.float32), (p >= f).astype(np.float32),
        np.where(p > f, z, big), np.where(f > p, z, big),
        np.where(f > p, z, -big), np.where(p > f, z, -big),
        np.where(f >= p, z, -big), np.where(f <= p, z, -big)], axis=1).astype(np.float32)
    es = np.zeros((128, 2, 64), np.float32)
    es[127, 0, :] = 1.0
    es[0, 1, :] = 1.0
    c["c_esel"] = es
    c["c_blk2"] = np.kron(np.eye(2, dtype=np.float32), np.ones((64, 64), np.float32))
    return c


def setup_consts(k, es, dr):
    nc = k.nc
    cst = {}
    idf = es.enter_context(nc.sbuf_tensor("ident_f", [128, 128], F32))
    idb = es.enter_context(nc.sbuf_tensor("ident_bf", [128, 128], BF16))
    k.dma("sp", idf[:], dr["c_ident"].ap())
    k.copy("dve", idb[:], idf[:])
    cst["ident_f"], cst["ident_bf"] = idf, idb
    for nm, shp in [("c_masks", [128, 8, 128]), ("c_esel", [128, 2, 64]), ("c_blk2", [128, 128])]:
        t = es.enter_context(nc.sbuf_tensor(nm + "_sb", shp, F32))
        k.dma("sp", t[:], dr[nm].ap())
        cst[nm] = t
    blkb = es.enter_context(nc.sbuf_tensor("blk2_bf", [128, 128], BF16))
    k.copy("dve", blkb[:], cst["c_blk2"][:])
    cst["blk2_bf"] = blkb
    k.P.barrier()
    k.P.emit()
    return cst


WEIGHT_SHAPES = {
    "norm_mix_w": (DEPTH, D), "w_in": (DEPTH, D, P_IN), "ml_i_bias": (DEPTH, 2, 4), "ml_f_bias": (DEPTH, 2, 4),
    "ml_norm_w": (DEPTH, 256), "gdn_conv_w": (DEPTH, 5, 1152), "gdn_a_log": (DEPTH, 2, 6),
    "gdn_dt_bias": (DEPTH, 2, 6), "gdn_norm_w": (DEPTH, 384), "w_out": (DEPTH, D, D),
    "norm_xa_w": (DEPTH, D), "norm_mem_w": (DEPTH, D), "w_xq": (DEPTH, D, D), "w_xkv": (DEPTH, D, 2 * D),
    "w_xo": (DEPTH, D, D), "norm_ffn_w": (DEPTH, D), "w_ff1": (DEPTH, D, DFF), "w_ff2": (DEPTH, DFF, D),
    "norm_out_w": (D,),
}


def declare_dram(nc, S, debug=(), ext_in=()):
    dr = {}
    dr["x"] = nc.dram_tensor("x", [S, D], F32, kind="ExternalInput")
    dr["mem"] = nc.dram_tensor("mem", [NMEM, D], F32, kind="ExternalInput")
    for n, shp in WEIGHT_SHAPES.items():
        dr[n] = nc.dram_tensor(n, list(shp), F32, kind="ExternalInput")
    for n, a in make_consts_host().items():
        dr[n] = nc.dram_tensor(n, list(a.shape), F32, kind="ExternalInput")
    dr["nab"] = nc.dram_tensor("nab", [DEPTH * 6, 128, 21 * 128], F32, kind="ExternalInput")

    def scratch(name, shape, dt):
        kind = "ExternalOutput" if name in debug else ("ExternalInput" if name in ext_in else "Internal")
        dr[name] = nc.dram_tensor(name, shape, dt, kind=kind)

    scratch("fm", [FM_ROWS, S], BF16)
    scratch("tm", [S, TM_COLS], BF16)
    scratch("gt", [S, 40], F32)
    scratch("hml", [S, 256], F32)
    scratch("ogd", [S, 384], F32)
    scratch("gqT", [384, S], BF16)
    scratch("gkT", [384, S], BF16)
    scratch("gk_tm", [S, 384], BF16)
    scratch("gv_tm", [S, 384], BF16)
    scratch("y", [S, D], BF16)
    scratch("xs", [S, D], F32)
    dr["out"] = nc.dram_tensor("out", [S, D], F32, kind="ExternalOutput")
    return dr


def load_w(k, dst, src2d, kc, ncols, split=4):
    v = src2d.rearrange("(c p) n -> p c n", p=128)
    for c0 in range(0, kc, split):
        k.dma("pool", dst[:, c0:c0 + split, :], v[:, c0:c0 + split, :])


def norm_to_T(k, xt, nwfull, hT, col0, hn_r, st_r, junk, pt_r, cst):
    hn = hn_r.next(); st = st_r.next()
    rmsnorm_rows(k, xt, hn, junk, st)
    pt = pt_r.next()
    for c in range(8):
        k.tr(pt[:, c, :], hn[:, c * 128:(c + 1) * 128], cst["ident_bf"][:])
    k.tt("dve", hT[:, :, col0:col0 + 128], pt[:], nwfull[:], ALU.mult)


def make_nwfull(k, es, src1d, ones):
    nc = k.nc
    nw = es.enter_context(nc.sbuf_tensor(k.name("nw"), [128, 8], F32))
    wfull = es.enter_context(nc.sbuf_tensor(k.name("wfull"), [128, 8, 128], BF16))
    k.dma("sp", nw[:], src1d.rearrange("(c p) -> p c", p=128), slow=True)
    for c in range(8):
        k.ts("dve", wfull[:, c, :], ones[:], nw[:, c:c + 1], ALU.mult)
    return wfull


def phase_mix_xattn(k, S, l, dr, cst, x_in, x_out):
    nc = k.nc
    NG = S // 512
    with ExitStack() as es:
        wout = es.enter_context(nc.sbuf_tensor(k.name("wout"), [128, 8, D], BF16))
        wxq = es.enter_context(nc.sbuf_tensor(k.name("wxq"), [128, 8, D], BF16))
        wxo = es.enter_context(nc.sbuf_tensor(k.name("wxo"), [128, 8, D], BF16))
        wkv = es.enter_context(nc.sbuf_tensor(k.name("wkv"), [128, 8, 2 * D], BF16))
        kkT = es.enter_context(nc.sbuf_tensor(k.name("kkT"), [128, 8, NMEM], BF16))
        vv = es.enter_context(nc.sbuf_tensor(k.name("vv"), [128, 2, D], BF16))
        memT = es.enter_context(nc.sbuf_tensor(k.name("memT"), [128, 8, NMEM], BF16))
        ones = es.enter_context(nc.sbuf_tensor(k.name("ones"), [128, 128], F32))
        ones_bf = es.enter_context(nc.sbuf_tensor(k.name("onesb"), [128, 128], BF16))
        xg = es.enter_context(nc.sbuf_tensor(k.name("xg"), [128, 4, D], F32))
        yt_r = Ring(k, es, "yt", 2, [128, D], BF16)
        yT = es.enter_context(nc.sbuf_tensor(k.name("yT"), [128, 8, 512], BF16))
        h2T = es.enter_context(nc.sbuf_tensor(k.name("h2T"), [128, 8, 512], BF16))
        qT = es.enter_context(nc.sbuf_tensor(k.name("qT"), [128, 8, 512], BF16))
        oT = es.enter_context(nc.sbuf_tensor(k.name("oT"), [128, 8, 512], BF16))
        PT_r = Ring(k, es, "PT", 4, [128, 512], BF16)
        rden_r = Ring(k, es, "rden", 2, [128, 512], F32)
        hn_r = Ring(k, es, "hn", 2, [128, D], BF16)
        junk = es.enter_context(nc.sbuf_tensor(k.name("junk"), [128, D], BF16))
        st_r = Ring(k, es, "st", 2, [128, 4], F32)
        mt_r = Ring(k, es, "mt", 2, [128, D], F32)
        pt_r = Ring(k, es, "ptr", 2, [128, 8, 128], BF16, psum=True)
        pm_r = Ring(k, es, "pmm", 5, [128, 512], F32, psum=True)

        k.memset("dve", ones[:], 1.0)
        k.memset("dve", ones_bf[:], 1.0)
        load_w(k, wkv, dr["w_xkv"].ap()[l], 8, 2 * D, split=2)
        load_w(k, wout, dr["w_out"].ap()[l], 8, D)
        load_w(k, wxq, dr["w_xq"].ap()[l], 8, D)
        load_w(k, wxo, dr["w_xo"].ap()[l], 8, D)
        nw_mem = make_nwfull(k, es, dr["norm_mem_w"].ap()[l], ones)
        nw_xa = make_nwfull(k, es, dr["norm_xa_w"].ap()[l], ones)
        msrc = dr["mem"].ap().rearrange("(n p) d -> n p d", p=128)
        for mt in range(2):
            m_t = mt_r.next()
            k.dma("sp", m_t[:], msrc[mt])
            norm_to_T(k, m_t, nw_mem, memT, mt * 128, hn_r, st_r, junk, pt_r, cst)
        ev = 0
        for m in range(8):
            ps = pm_r.next()
            for c in range(8):
                k.mm(ps[:, 0:NMEM], wkv[:, c, m * 128:(m + 1) * 128], memT[:, c, :], start=(c == 0), stop=(c == 7))
            k.copy("act", kkT[:, m, :], ps[:, 0:NMEM])
        for mt in range(2):
            for n in range(2):
                ps = pm_r.next()
                for c in range(8):
                    k.mm(ps[:], memT[:, c, mt * 128:(mt + 1) * 128], wkv[:, c, D + n * 512:D + (n + 1) * 512],
                         start=(c == 0), stop=(c == 7))
                k.copy("dve", vv[:, mt, n * 512:(n + 1) * 512], ps[:])

        ysrc = dr["y"].ap().rearrange("(n p) d -> n p d", p=128)
        xsrc = x_in.ap().rearrange("(n p) d -> n p d", p=128)
        xdst = x_out.ap().rearrange("(n p) d -> n p d", p=128)
        for g in range(NG):
            for j in range(4):
                t = g * 4 + j
                yt = yt_r.next()
                k.dma("sp", yt[:], ysrc[t])
                k.dma("sp", xg[:, j, :], xsrc[t])
                pt = pt_r.next()
                for c in range(8):
                    k.tr(pt[:, c, :], yt[:, c * 128:(c + 1) * 128], cst["ident_bf"][:])
                k.copy("act", yT[:, :, j * 128:(j + 1) * 128], pt[:])
            for j in range(4):
                for n in range(2):
                    ps = pm_r.next()
                    for c in range(8):
                        k.mm(ps[:], yT[:, c, j * 128:(j + 1) * 128], wout[:, c, n * 512:(n + 1) * 512],
                             start=(c == 0), stop=(c == 7))
                    k.tt("dve", xg[:, j, n * 512:(n + 1) * 512], ps[:], xg[:, j, n * 512:(n + 1) * 512], ALU.add)
                norm_to_T(k, xg[:, j, :], nw_xa, h2T, j * 128, hn_r, st_r, junk, pt_r, cst)
            for m in range(8):
                ps = pm_r.next()
                for c in range(8):
                    k.mm(ps[:], wxq[:, c, m * 128:(m + 1) * 128], h2T[:, c, :], start=(c == 0), stop=(c == 7))
                k.copy("act" if m % 2 == 0 else "dve", qT[:, m, :], ps[:])
            for hh in range(4):
                PTs = []
                for mt in range(2):
                    ps = pm_r.next()
                    for dc in range(2):
                        k.mm(ps[:], kkT[:, 2 * hh + dc, mt * 128:(mt + 1) * 128], qT[:, 2 * hh + dc, :],
                             start=(dc == 0), stop=(dc == 1))
                    PT = PT_r.next()
                    k.act(PT[:], ps[:], AF.Exp, scale=1.0 / 16.0)
                    PTs.append(PT)
                ps = pm_r.next()
                for mt in range(2):
                    k.mm(ps[:], ones_bf[:], PTs[mt][:], start=(mt == 0), stop=(mt == 1))
                rden = rden_r.next()
                k.recip(rden[:], ps[:])
                for dc in range(2):
                    ps = pm_r.next()
                    for mt in range(2):
                        k.mm(ps[:], vv[:, mt, hh * 256 + dc * 128:hh * 256 + (dc + 1) * 128], PTs[mt][:],
                             start=(mt == 0), stop=(mt == 1))
                    k.tt("dve", oT[:, 2 * hh + dc, :], ps[:], rden[:], ALU.mult)
            for j in range(4):
                t = g * 4 + j
                for n in range(2):
                    ps = pm_r.next()
                    for c in range(8):
                        k.mm(ps[:], oT[:, c, j * 128:(j + 1) * 128], wxo[:, c, n * 512:(n + 1) * 512],
                             start=(c == 0), stop=(c == 7))
                    k.tt("dve", xg[:, j, n * 512:(n + 1) * 512], ps[:], xg[:, j, n * 512:(n + 1) * 512], ALU.add)
                k.dma("pool", xdst[t], xg[:, j, :])
        k.P.barrier()
        k.P.emit()


def phase_ffn(k, S, l, dr, cst, x_io, final_out=None):
    nc = k.nc
    GT = 2
    NG = S // (128 * GT)
    with ExitStack() as es:
        w1 = es.enter_context(nc.sbuf_tensor(k.name("w1"), [128, 8, DFF], BF16))
        w2 = es.enter_context(nc.sbuf_tensor(k.name("w2"), [128, 32, D], BF16))
        ones = es.enter_context(nc.sbuf_tensor(k.name("ones"), [128, 128], F32))
        xg = es.enter_context(nc.sbuf_tensor(k.name("xg"), [128, GT, D], F32))
        h3T = es.enter_context(nc.sbuf_tensor(k.name("h3T"), [128, 8, 128 * GT], BF16))
        uT = es.enter_context(nc.sbuf_tensor(k.name("uT"), [128, 32, 128 * GT], BF16))
        r_r = Ring(k, es, "relu", 3, [128, 128 * GT], BF16)
        hn_r = Ring(k, es, "hn", 2, [128, D], BF16)
        junk = es.enter_context(nc.sbuf_tensor(k.name("junk"), [128, D], BF16))
        st_r = Ring(k, es, "st", 2, [128, 4], F32)
        pt_r = Ring(k, es, "ptr", 2, [128, 8, 128], BF16, psum=True)
        pm_r = Ring(k, es, "pmm", 5, [128, 512], F32, psum=True)
        k.memset("dve", ones[:], 1.0)
        load_w(k, w1, dr["w_ff1"].ap()[l], 8, DFF, split=1)
        load_w(k, w2, dr["w_ff2"].ap()[l], 32, D, split=4)
        nw = make_nwfull(k, es, dr["norm_ffn_w"].ap()[l], ones)
        if final_out is not None:
            nwo = es.enter_context(nc.sbuf_tensor(k.name("nwo"), [128, D], F32))
            src = dr["norm_out_w"]
            k.dma("sp", nwo[:], bass.AP(src, 0, [[0, 128], [1, D]]))
            fo_r = Ring(k, es, "fo", 2, [128, D], F32)
            fdst = final_out.ap().rearrange("(n p) d -> n p d", p=128)
        xv = x_io.ap().rearrange("(n p) d -> n p d", p=128)
        W = 128 * GT
        for g in range(NG):
            for j in range(GT):
                t = g * GT + j
                k.dma("sp", xg[:, j, :], xv[t])
                norm_to_T(k, xg[:, j, :], nw, h3T, j * 128, hn_r, st_r, junk, pt_r, cst)
            for f in range(32):
                ps = pm_r.next()
                for c in range(8):
                    k.mm(ps[:, 0:W], w1[:, c, f * 128:(f + 1) * 128], h3T[:, c, :], start=(c == 0), stop=(c == 7))
                r = r_r.next()
                k.act(r[:], ps[:, 0:W], AF.Relu)
                k.tt("pool" if f % 2 == 0 else "dve", uT[:, f, :], r[:], r[:], ALU.mult)
            for j in range(GT):
                t = g * GT + j
                for n in range(2):
                    ps = pm_r.next()
                    for f in range(32):
                        k.mm(ps[:], uT[:, f, j * 128:(j + 1) * 128], w2[:, f, n * 512:(n + 1) * 512],
                             start=(f == 0), stop=(f == 31))
                    k.tt("dve", xg[:, j, n * 512:(n + 1) * 512], ps[:], xg[:, j, n * 512:(n + 1) * 512], ALU.add)
                if final_out is None:
                    k.dma("pool", xv[t], xg[:, j, :])
                else:
                    st = st_r.next(); fo = fo_r.next()
                    k.act(junk[:], xg[:, j, :], AF.Square, accum=st[:, 0:1])
                    k.act(st[:, 1:2], st[:, 0:1], AF.Sqrt, bias=EPS, scale=1.0 / D)
                    k.recip(st[:, 2:3], st[:, 1:2])
                    k.stt(fo[:], xg[:, j, :], st[:, 2:3], nwo[:], ALU.mult, ALU.mult)
                    k.dma("pool", fdst[t], fo[:])
        k.P.barrier()
        k.P.emit()


NA_CLASSES = {"int": (0, [-2, -1, 0, 1, 2]), "top0": (5, [0, 1, 2, 3]), "top1": (9, [-1, 0, 1, 2]),
              "bot1": (13, [-2, -1, 0, 1]), "bot0": (17, [-3, -2, -1, 0])}
NEG = -30000.0


def na_class(qt, NT):
    if qt == 0:
        return "top0"
    if qt == 1:
        return "top1"
    if qt == NT - 2:
        return "bot1"
    if qt == NT - 1:
        return "bot0"
    return "int"


def na_bias_host(rel_bias, S):
    L = rel_bias.shape[0]
    R, NT = S // 64, S // 128
    out = np.full((L, 6, 21, 128, 128), NEG, np.float32)
    rep = {"int": 2, "top0": 0, "top1": 1, "bot1": NT - 2, "bot0": NT - 1}
    j = np.arange(128)
    for cls, (base, offs) in NA_CLASSES.items():
        qt = rep[cls]
        for n, o in enumerate(offs):
            kt = qt + o
            kr, kc = 2 * kt + j // 64, j % 64
            qr, qc = 2 * qt + j // 64, j % 64
            r0 = np.clip(qr - 4, 0, R - 8)
            c0 = np.clip(qc - 8, 0, 48)
            inw = ((kr[:, None] >= r0[None, :]) & (kr[:, None] <= r0[None, :] + 7) &
                   (kc[:, None] >= c0[None, :]) & (kc[:, None] <= c0[None, :] + 15))
            drr = np.clip(kr[:, None] - qr[None, :] + 7, 0, 14)
            dcc = np.clip(kc[:, None] - qc[None, :] + 15, 0, 30)
            vals = rel_bias[:, :, drr, dcc]
            out[:, :, base + n] = np.where(inw[None, None], vals, np.float32(NEG))
    return np.ascontiguousarray(out.transpose(0, 1, 3, 2, 4).reshape(L * 6, 128, 21 * 128))


def phase_na(k, S, l, dr, cst):
    nc = k.nc
    NT = S // 128
    with ExitStack() as es:
        qT_r = Ring(k, es, "naq", 2, [64, S], BF16)
        kT_r = Ring(k, es, "nak", 2, [64, S], BF16)
        va_r = Ring(k, es, "nav", 2, [128, NT, 65], BF16)
        nb_r = Ring(k, es, "nab", 2, [128, 21 * 128], F32)
        lg_r = Ring(k, es, "nalg", 2, [128, 640], F32)
        PT_r = Ring(k, es, "naPT", 2, [128, 640], BF16)
        yo_r = Ring(k, es, "nayo", 3, [128, 64], BF16)
        rd_r = Ring(k, es, "nard", 3, [128, 1], F32)
        psA_r = Ring(k, es, "psA", 2, [128, 512], F32, psum=True)
        psB_r = Ring(k, es, "psB", 2, [128, 512], F32, psum=True)
        po_r = Ring(k, es, "pso", 2, [128, 512], F32, psum=True)
        fm = dr["fm"].ap()
        tmv = dr["tm"].ap().rearrange("(n p) c -> p n c", p=128)
        yv = dr["y"].ap().rearrange("(n p) c -> n p c", p=128)
        for h in range(6):
            qT = qT_r.next(); kT = kT_r.next(); va = va_r.next(); nb = nb_r.next()
            k.dma("sp", qT[:], fm[FM_NAQ + h * 64:FM_NAQ + (h + 1) * 64, :])
            k.dma("sp", kT[:], fm[FM_NAK + h * 64:FM_NAK + (h + 1) * 64, :])
            for t0 in range(0, NT, 8):
                k.dma("sp", va[:, t0:t0 + 8, 0:64], tmv[:, t0:t0 + 8, TM_NAV + h * 64:TM_NAV + (h + 1) * 64])
            k.memset("pool", va[:, :, 64:65], 1.0)
            k.dma("sp", nb[:], dr["nab"].ap()[l * 6 + h])
            for qt in range(NT):
                base, offs = NA_CLASSES[na_class(qt, NT)]
                n = len(offs)
                psA = psA_r.next(); psB = psB_r.next(); lg = lg_r.next(); PT = PT_r.next()
                for i, o in enumerate(offs):
                    kt = qt + o
                    dst = psA[:, i * 128:(i + 1) * 128] if i < 4 else psB[:, 0:128]
                    k.mm(dst, kT[:, kt * 128:(kt + 1) * 128], qT[:, qt * 128:(qt + 1) * 128])
                na = min(n, 4) * 128
                k.stt(lg[:, 0:na], psA[:, 0:na], 0.125, nb[:, base * 128:base * 128 + na], ALU.mult, ALU.add)
                k.act(PT[:, 0:na], lg[:, 0:na], AF.Exp)
                if n == 5:
                    k.stt(lg[:, 512:640], psB[:, 0:128], 0.125, nb[:, (base + 4) * 128:(base + 5) * 128],
                          ALU.mult, ALU.add)
                    k.act(PT[:, 512:640], lg[:, 512:640], AF.Exp)
                po = po_r.next()
                for i, o in enumerate(offs):
                    kt = qt + o
                    k.mm(po[:, 0:65], PT[:, i * 128:(i + 1) * 128], va[:, kt, :], start=(i == 0), stop=(i == n - 1))
                rd = rd_r.next(); yo = yo_r.next()
                k.recip(rd[:], po[:, 64:65])
                k.ts("dve", yo[:], po[:, 0:64], rd[:], ALU.mult)
                k.dma("pool", yv[qt][:, h * 64:(h + 1) * 64], yo[:])
        k.P.barrier()
        k.P.emit()


def bcast_rows(k, dst, src_handle, off, n):
    k.dma("sp", dst, bass.AP(src_handle, off, [[0, 128], [1, n]]))


class GatePool:
    def __init__(self, k, es, l, dr, cst):
        nc = k.nc
        self.k, self.cst, self.dr = k, cst, dr
        self.bias = es.enter_context(nc.sbuf_tensor(k.name("gbias"), [128, 40], F32))
        self.nA = es.enter_context(nc.sbuf_tensor(k.name("gnA"), [128, 12], F32))
        self.ones = es.enter_context(nc.sbuf_tensor(k.name("gones"), [128, 128], F32))
        k.memset("dve", self.bias[:], 0.0)
        k.memset("dve", self.ones[:], 1.0)
        bcast_rows(k, self.bias[:, 0:8], dr["ml_i_bias"], l * 8, 8)
        bcast_rows(k, self.bias[:, 8:16], dr["ml_f_bias"], l * 8, 8)
        bcast_rows(k, self.bias[:, 28:40], dr["gdn_dt_bias"], l * 12, 12)
        bcast_rows(k, self.nA[:], dr["gdn_a_log"], l * 12, 12)
        k.act(self.nA[:], self.nA[:], AF.Exp)
        k.ts("dve", self.nA[:], self.nA[:], -1.0, ALU.mult)
        self.gt_r = Ring(k, es, "gtt", 4, [128, 40], F32)
        self.a_r = Ring(k, es, "gta", 4, [128, 40], F32)
        self.e_r = Ring(k, es, "gte", 4, [128, 40], F32)
        self.sp_r = Ring(k, es, "gtsp", 4, [128, 40], F32)
        self.w_r = Ring(k, es, "gtw", 4, [128, 32], F32)
        self.x_r = Ring(k, es, "gtx", 4, [128, 32], F32)
        self.bt_r = Ring(k, es, "gtbt", 4, [64, 8], F32)
        self.g_r = Ring(k, es, "gtg", 4, [128, 8], F32)
        self.pg_r = Ring(k, es, "gtpg", 1, [128, 512], F32, psum=True)

    def pre_gen(self, t):
        k = self.k
        gt = self.gt_r.next(); a = self.a_r.next(); e = self.e_r.next(); sp = self.sp_r.next()
        k.dma("sp", gt[:], self.dr["gt"].ap()[t * 128:(t + 1) * 128, :])
        k.tt("dve", a[:], gt[:], self.bias[:], ALU.add)
        yield
        k.act(e[:, 8:28], a[:, 8:28], AF.Exp, scale=-1.0)
        k.act(e[:, 28:40], a[:, 28:40], AF.Exp)
        yield
        k.act(sp[:, 8:40], e[:, 8:40], AF.Ln, bias=1.0)
        self._pre = (a, sp)
        yield

    def mlstm_gen(self, t, d):
        k = self.k
        yield from self.pre_gen(t)
        a, sp = self._pre
        pg = self.pg_r.next()
        lo = 8 + d * 4
        k.mm(pg[:, 0:4], self.cst["c_masks"][:, d, :], sp[:, lo:lo + 4])
        k.mm(pg[0:64, 8:12], self.ones[:, 0:64], sp[:, lo:lo + 4])
        yield
        w = self.w_r.next(); x = self.x_r.next(); bt = self.bt_r.next()
        k.ts("dve", w[:, 0:4], pg[:, 0:4], -1.0, ALU.mult)
        k.tt("dve", w[:, 4:8], pg[:, 0:4], a[:, d * 4:d * 4 + 4], ALU.add)
        yield
        k.act(x[:, 0:8], w[:, 0:8], AF.Exp)
        k.act(bt[:, 0:4], pg[0:64, 8:12], AF.Exp, scale=-1.0)
        self.result = (x, bt)

    def gdn_gen(self, t, d):
        k = self.k
        yield from self.pre_gen(t)
        a, sp = self._pre
        w = self.w_r.next(); x = self.x_r.next(); bt = self.bt_r.next()
        g = self.g_r.next()
        lo = 28 + d * 6
        k.tt("dve", g[:, 0:6], sp[:, lo:lo + 6], self.nA[:, d * 6:d * 6 + 6], ALU.mult)
        pg = self.pg_r.next()
        k.mm(pg[:, 0:6], self.cst["c_masks"][:, d, :], g[:, 0:6])
        k.mm(pg[:, 8:14], self.ones[:], g[:, 0:6])
        yield
        lb = 16 + d * 6
        k.copy("dve", w[:, 0:6], pg[:, 0:6])
        k.tt("dve", w[:, 6:12], pg[:, 0:6], sp[:, lb:lb + 6], ALU.subtract)
        yield
        k.tt("dve", w[:, 12:18], pg[:, 8:14], w[:, 0:6], ALU.subtract)
        k.ts("dve", w[:, 18:24], sp[:, lb:lb + 6], -1.0, ALU.mult)
        yield
        k.act(x[:, 0:24], w[:, 0:24], AF.Exp)
        k.act(bt[:, 0:6], pg[0:64, 8:14], AF.Exp)
        self.result = (w, x, bt)

    def finish(self, gen):
        if gen is not None:
            for _ in gen:
                pass
        return self.result

    @staticmethod
    def step(gen):
        if gen is not None:
            next(gen, None)


def phase_mlstm(k, S, l, dr, cst):
    nc = k.nc
    NT = S // 128
    with ExitStack() as es:
        GP = GatePool(k, es, l, dr, cst)
        qT_r = Ring(k, es, "mlq", 2, [64, 4, 128], BF16)
        kT_r = Ring(k, es, "mlk", 2, [64, 4, 128], BF16)
        ktm_r = Ring(k, es, "mlktm", 2, [128, 256], BF16)
        va_r = Ring(k, es, "mlva", 2, [128, 4, 65], BF16)
        vp_r = Ring(k, es, "mlvp", 3, [128, 65], BF16)
        pm_r = Ring(k, es, "mlpm", 3, [128, 128], BF16)
        C32 = [es.enter_context(nc.sbuf_tensor(k.name("C32"), [64, 65], F32)) for _ in range(4)]
        Cbf = [es.enter_context(nc.sbuf_tensor(k.name("Cbf"), [64, 65], BF16)) for _ in range(4)]
        tmp_r = Ring(k, es, "mltmp", 3, [64, 65], F32)
        sm_r = Ring(k, es, "mlsm", 4, [128, 4], F32)
        hb_r = Ring(k, es, "mlh", 2, [128, 256], F32)
        hf_r = Ring(k, es, "mlhf", 2, [128, 256], F32)
        ot_r = Ring(k, es, "mlo", 2, [128, 256], BF16)
        gw_r = Ring(k, es, "mlgw", 2, [128, 256], F32)
        yo_r = Ring(k, es, "mly", 2, [128, 256], BF16)
        junk = es.enter_context(nc.sbuf_tensor(k.name("mljunk"), [128, 64], F32))
        nwb = es.enter_context(nc.sbuf_tensor(k.name("mlnw"), [128, 256], F32))
        ps_r = Ring(k, es, "mlps", 2, [128, 512], F32, psum=True)
        po_r = Ring(k, es, "mlpo", 2, [128, 512], F32, psum=True)
        pc_r = Ring(k, es, "mlpc", 2, [128, 512], F32, psum=True)
        bcast_rows(k, nwb[:], dr["ml_norm_w"], l * 256, 256)
        for t_ in va_r.t:
            k.memset("dve", t_[:, :, 64:65], 1.0)
        fm = dr["fm"].ap()
        tmv = dr["tm"].ap().rearrange("(n p) c -> n p c", p=128)
        hml = dr["hml"].ap().rearrange("(n p) c -> n p c", p=128)
        yv = dr["y"].ap().rearrange("(n p) c -> n p c", p=128)
        for d in range(2):
            for h in range(4):
                k.memset("dve", C32[h][:], 0.0)
                k.memset("dve", Cbf[h][:], 0.0)
            mask = cst["c_masks"][:, d, :]
            order = range(NT) if d == 0 else range(NT - 1, -1, -1)
            order = list(order)
            cur = GP.finish(GP.mlstm_gen(order[0], d))
            for oi, t in enumerate(order):
                ex, ebt = cur
                gen = GP.mlstm_gen(order[oi + 1], d) if oi + 1 < len(order) else None
                qT = qT_r.next(); kT = kT_r.next(); ktm = ktm_r.next(); va = va_r.next()
                cs = slice(t * 128, (t + 1) * 128)
                k.dma("sp", qT[:], fm[FM_MLQ:FM_MLQ + 256, cs].rearrange("(h d) t -> d h t", d=64))
                k.dma("sp", kT[:], fm[FM_MLK:FM_MLK + 256, cs].rearrange("(h d) t -> d h t", d=64))
                k.dma("sp", ktm[:], tmv[t][:, TM_MLK:TM_MLK + 256])
                k.dma("sp", va[:, :, 0:64], tmv[t][:, TM_MLV:TM_MLV + 256].rearrange("p (h d) -> p h d", d=64))
                hb = hb_r.next()
                for h in range(4):
                    vp = vp_r.next()
                    k.ts("dve", vp[:], va[:, h, :], ex[:, 4 + h:5 + h], ALU.mult, 0.125, ALU.mult)
                    ps = ps_r.next()
                    k.mm(ps[:, 0:128], kT[:, h, :], qT[:, h, :])
                    pm = pm_r.next()
                    k.tt("dve", pm[:], ps[:, 0:128], mask, ALU.mult)
                    po = po_r.next()
                    k.mm(po[:, 0:65], pm[:], vp[:], start=True, stop=False)
                    k.mm(po[:, 0:65], qT[:, h, :], Cbf[h][:], start=False, stop=True)
                    pc = pc_r.next()
                    k.mm(pc[0:64, 0:65], ktm[:, h * 64:(h + 1) * 64], vp[:])
                    tmp = tmp_r.next()
                    k.tt("dve", tmp[:], pc[0:64, 0:65], C32[h][:], ALU.add)
                    k.ts("dve", C32[h][:], tmp[:], ebt[:, h:h + 1], ALU.mult)
                    k.act(Cbf[h][:], tmp[:], AF.Copy, scale=ebt[:, h:h + 1])
                    GP.step(gen)
                    sm = sm_r.next()
                    k.act(sm[:, 3:4], po[:, 64:65], AF.Abs, scale=ex[:, h:h + 1])
                    k.ts("dve", sm[:, 0:1], sm[:, 3:4], 1.0, ALU.max)
                    k.recip(sm[:, 1:2], sm[:, 0:1])
                    k.tt("dve", sm[:, 2:3], sm[:, 1:2], ex[:, h:h + 1], ALU.mult)
                    k.ts("dve", hb[:, h * 64:(h + 1) * 64], po[:, 0:64], sm[:, 2:3], ALU.mult)
                    GP.step(gen)
                cur = GP.finish(gen)
                if d == 0:
                    k.dma("pool", hml[t], hb[:])
                else:
                    hf = hf_r.next(); ot = ot_r.next(); gw = gw_r.next(); yo = yo_r.next()
                    k.dma("sp", hf[:], hml[t])
                    k.dma("sp", ot[:], tmv[t][:, TM_MLO:TM_MLO + 256])
                    k.tt("dve", hb[:], hb[:], hf[:], ALU.add)
                    k.act(gw[:], ot[:], AF.Sigmoid)
                    k.tt("dve", gw[:], gw[:], nwb[:], ALU.mult)
                    sm = sm_r.next()
                    for h in range(4):
                        k.act(junk[:], hb[:, h * 64:(h + 1) * 64], AF.Square, accum=sm[:, h:h + 1])
                    sm2 = sm_r.next()
                    k.act(sm2[:], sm[:], AF.Sqrt, bias=EPS, scale=1.0 / 64.0)
                    sm3 = sm_r.next()
                    k.recip(sm3[:], sm2[:])
                    for h in range(4):
                        k.stt(yo[:, h * 64:(h + 1) * 64], hb[:, h * 64:(h + 1) * 64], sm3[:, h:h + 1],
                              gw[:, h * 64:(h + 1) * 64], ALU.mult, ALU.mult)
                    k.dma("pool", yv[t][:, 384:640], yo[:])
        k.P.barrier()
        k.P.emit()


def phase_gdn_prep(k, S, l, dr, cst):
    nc = k.nc
    NCH = S // 512
    with ExitStack() as es:
        cwr = es.enter_context(nc.sbuf_tensor(k.name("cwr"), [5, 1152], F32))
        cw = es.enter_context(nc.sbuf_tensor(k.name("cw"), [128, 9, 5], F32))
        xin_r = Ring(k, es, "gxin", 2, [128, 516], BF16)
        acc_r = Ring(k, es, "gacc", 2, [128, 512], F32)
        s_r = Ring(k, es, "gs", 2, [128, 512], F32)
        sq_r = Ring(k, es, "gsq", 2, [128, 512], BF16)
        rt_r = Ring(k, es, "grt", 2, [128, 512], F32)
        sn_r = Ring(k, es, "gsn", 2, [128, 512], BF16)
        st_r = Ring(k, es, "gst", 2, [128, 4, 128], BF16)
        pcw = es.enter_context(nc.psum_tensor(k.name("pcw"), [128, 512], F32))
        ps_r = Ring(k, es, "gpps", 2, [128, 512], F32, psum=True)
        pt_r = Ring(k, es, "gppt", 2, [128, 4, 128], BF16, psum=True)
        k.dma("sp", cwr[:], dr["gdn_conv_w"].ap()[l])
        for g in range(9):
            k.tr(pcw[:, g * 8:g * 8 + 5], cwr[0:5, g * 128:(g + 1) * 128], cst["ident_f"][0:5, 0:5])
        for g in range(9):
            k.copy("dve", cw[:, g, :], pcw[:, g * 8:g * 8 + 5])
        fm = dr["fm"].ap()
        dsts = {0: dr["gqT"], 1: dr["gkT"]}
        tms = {1: dr["gk_tm"], 2: dr["gv_tm"]}
        for g in range(9):
            kind, gi = g // 3, g % 3
            for c in range(NCH):
                xin = xin_r.next(); acc = acc_r.next(); s = s_r.next(); sn = sn_r.next()
                lo, hi = max(c * 512 - 2, 0), min(c * 512 + 514, S)
                off = lo - (c * 512 - 2)
                if c == 0:
                    k.memset("pool", xin[:, 0:2], 0.0)
                if c == NCH - 1:
                    k.memset("pool", xin[:, 514:516], 0.0)
                k.dma("sp", xin[:, off:off + hi - lo], fm[FM_GDQ + g * 128:FM_GDQ + (g + 1) * 128, lo:hi])
                k.ts("dve", acc[:], xin[:, 0:512], cw[:, g, 0:1], ALU.mult)
                for kk in range(1, 5):
                    k.stt(acc[:], xin[:, kk:kk + 512], cw[:, g, kk:kk + 1], acc[:], ALU.mult, ALU.add)
                k.act(s[:], acc[:], AF.Silu)
                if kind < 2:
                    sq = sq_r.next(); rt = rt_r.next(); ps = ps_r.next()
                    k.tt("pool", sq[:], s[:], s[:], ALU.mult)
                    k.mm(ps[:], cst["blk2_bf"][:], sq[:])
                    k.act(rt[:], ps[:], AF.Sqrt, bias=EPS)
                    k.recip(rt[:], rt[:])
                    if kind == 0:
                        k.stt(sn[:], s[:], 0.125, rt[:], ALU.mult, ALU.mult)
                    else:
                        k.tt("dve", sn[:], s[:], rt[:], ALU.mult)
                    k.dma("pool", dsts[kind].ap()[gi * 128:(gi + 1) * 128, c * 512:(c + 1) * 512], sn[:])
                else:
                    k.copy("pool", sn[:], s[:])
                if kind >= 1:
                    pt = pt_r.next(); st = st_r.next()
                    for j in range(4):
                        k.tr(pt[:, j, :], sn[:, j * 128:(j + 1) * 128], cst["ident_bf"][:])
                    k.copy("act", st[:], pt[:])
                    dv = tms[kind].ap().rearrange("(n p) c -> p n c", p=128)
                    k.dma("pool", dv[:, c * 4:(c + 1) * 4, gi * 128:(gi + 1) * 128], st[:])
        k.P.barrier()
        k.P.emit()


def phase_gdn(k, S, l, dr, cst, stage=9):
    nc = k.nc
    NT = S // 128
    M = cst["c_masks"]
    with ExitStack() as es:
        GP = GatePool(k, es, l, dr, cst)
        qT_r = Ring(k, es, "gdq", 2, [64, 6, 128], BF16)
        kT_r = Ring(k, es, "gdk", 2, [64, 6, 128], BF16)
        ktm_r = Ring(k, es, "gdktm", 2, [128, 384], BF16)
        vtm_r = Ring(k, es, "gdvtm", 2, [128, 384], BF16)
        dg_r = Ring(k, es, "gddg", 4, [128, 128], F32)
        xp_r = Ring(k, es, "gdxp", 9, [128, 128], F32)
        A_r = Ring(k, es, "gdA", 9, [128, 128], F32)
        B_r = Ring(k, es, "gdB", 9, [128, 128], F32)
        N_r = Ring(k, es, "gdN", 9, [128, 128], F32)
        at_r = Ring(k, es, "gdat", 6, [128, 128], BF16)
        sc_r = Ring(k, es, "gdsc", 2, [128, 2, 64], F32)
        ks_r = Ring(k, es, "gdks", 2, [128, 64], BF16)
        u_r = Ring(k, es, "gdu", 2, [128, 64], F32)
        wk_r = Ring(k, es, "gdwk", 2, [64, 128], BF16)
        vn_r = Ring(k, es, "gdvn", 2, [128, 64], BF16)
        o2_r = Ring(k, es, "gdo2", 2, [128, 64], F32)
        ob_r = Ring(k, es, "gdob", 2, [128, 384], F32)
        of_r = Ring(k, es, "gdof", 2, [128, 384], F32)
        z_r = Ring(k, es, "gdz", 2, [128, 384], BF16)
        gz_r = Ring(k, es, "gdgz", 2, [128, 384], F32)
        yo_r = Ring(k, es, "gdyo", 2, [128, 384], BF16)
        sm_r = Ring(k, es, "gdsm", 4, [128, 8], F32)
        junk = es.enter_context(nc.sbuf_tensor(k.name("gdjunk"), [128, 64], F32))
        nwb = es.enter_context(nc.sbuf_tensor(k.name("gdnw"), [128, 384], F32))
        S32 = [es.enter_context(nc.sbuf_tensor(k.name("S32"), [64, 64], F32)) for _ in range(6)]
        Sbf = [es.enter_context(nc.sbuf_tensor(k.name("Sbf"), [64, 64], BF16)) for _ in range(6)]
        pA_r = Ring(k, es, "gdpA", 1, [128, 512], F32, psum=True)
        pB_r = Ring(k, es, "gdpB", 1, [128, 512], F32, psum=True)
        pc_r = Ring(k, es, "gdpc", 3, [128, 512], F32, psum=True)
        pu_r = Ring(k, es, "gdpu", 1, [128, 512], F32, psum=True)
        pv_r = Ring(k, es, "gdpv", 1, [128, 512], F32, psum=True)
        bcast_rows(k, nwb[:], dr["gdn_norm_w"], l * 384, 384)
        idf, idb, ones = cst["ident_f"], cst["ident_bf"], GP.ones
        tmv = dr["tm"].ap().rearrange("(n p) c -> n p c", p=128)
        ktv = dr["gk_tm"].ap().rearrange("(n p) c -> n p c", p=128)
        vtv = dr["gv_tm"].ap().rearrange("(n p) c -> n p c", p=128)
        ogd = dr["ogd"].ap().rearrange("(n p) c -> n p c", p=128)
        yv = dr["y"].ap().rearrange("(n p) c -> n p c", p=128)
        for d in range(2):
            mA, mB, mC = (M[:, 2, :], M[:, 4, :], M[:, 6, :]) if d == 0 else (M[:, 3, :], M[:, 5, :], M[:, 7, :])
            for h in range(6):
                k.memset("dve", S32[h][:], 0.0)
                k.memset("dve", Sbf[h][:], 0.0)
            order = range(NT) if d == 0 else range(NT - 1, -1, -1)
            order = list(order)
            cur = GP.finish(GP.gdn_gen(order[0], d))
            for oi, t in enumerate(order):
                raw, ex, egl = cur
                gen = GP.gdn_gen(order[oi + 1], d) if oi + 1 < len(order) else None
                qT = qT_r.next(); kT = kT_r.next(); ktm = ktm_r.next(); vtm = vtm_r.next()
                cs = slice(t * 128, (t + 1) * 128)
                k.dma("sp", qT[:], dr["gqT"].ap()[:, cs].rearrange("(h d) t -> d h t", d=64))
                k.dma("sp", kT[:], dr["gkT"].ap()[:, cs].rearrange("(h d) t -> d h t", d=64))
                k.dma("sp", ktm[:], ktv[t])
                k.dma("sp", vtm[:], vtv[t])
                ob = ob_r.next()
                for g0 in range(0, 6, 3):
                    grp = list(range(g0, g0 + 3))
                    st = {}
                    for h in grp:
                        pA = pA_r.next(); pB = pB_r.next()
                        k.mm(pA[:, 0:128], kT[:, h, :], kT[:, h, :])
                        k.mm(pA[:, 128:256], kT[:, h, :], qT[:, h, :])
                        dg1 = dg_r.next(); dg2 = dg_r.next()
                        k.act(dg1[:], idf[:], AF.Copy, scale=raw[:, h:h + 1])
                        k.act(dg2[:], idf[:], AF.Copy, scale=raw[:, 6 + h:7 + h])
                        k.mm(pB[:, 0:128], ones[:], dg1[:])
                        k.mm(pB[:, 128:256], ones[:], dg2[:])
                        xa = xp_r.next(); xb = xp_r.next(); xc = xp_r.next()
                        k.stt(xa[:], pB[:, 0:128], raw[:, 6 + h:7 + h], mA, ALU.subtract, ALU.add)
                        k.stt(xb[:], pB[:, 128:256], raw[:, h:h + 1], mB, ALU.subtract, ALU.add)
                        k.stt(xc[:], pB[:, 0:128], raw[:, h:h + 1], mC, ALU.subtract, ALU.add)
                        k.act(xa[:], xa[:], AF.Exp, scale=-1.0)
                        k.act(xb[:], xb[:], AF.Exp)
                        k.act(xc[:], xc[:], AF.Exp)
                        A = A_r.next(); B = B_r.next(); at = at_r.next(); N = N_r.next()
                        k.tt("dve", A[:], pA[:, 0:128], xa[:], ALU.mult)
                        k.tt("dve", B[:], pA[:, 0:128], xb[:], ALU.mult)
                        k.tt("dve", at[:], pA[:, 128:256], xc[:], ALU.mult)
                        k.tt("dve", N[:], idf[:], B[:], ALU.subtract)
                        st[h] = [A, B, N, at]
                        GP.step(gen)
                    for lv in range(1, 7):
                        for hi, h in enumerate(grp):
                            A, B, N, at = st[h]
                            pc = pc_r.t[hi]
                            A2 = A_r.next()
                            k.mm(pc[:, 0:128], B[:], A[:])
                            if lv < 6:
                                B2 = B_r.next()
                                k.mm(pc[:, 128:256], A[:], B[:])
                            k.copy("act", A2[:], pc[:, 0:128])
                            if lv < 6:
                                k.copy("act" if lv % 2 == 0 else "dve", B2[:], pc[:, 128:256])
                            k.mm(pc[:, 256:384], A2[:], N[:])
                            N2 = N_r.next()
                            k.tt("dve", N2[:], pc[:, 256:384], N[:], ALU.add)
                            st[h] = [A2, B2 if lv < 6 else B, N2, at]
                    for h in grp:
                        A, B, N, at = st[h]
                        hs = slice(h * 64, (h + 1) * 64)
                        sc = sc_r.next()
                        k.act(sc[:, 0, :], vtm[:, hs], AF.Copy, scale=ex[:, 18 + h:19 + h])
                        k.act(sc[:, 1, :], ktm[:, hs], AF.Copy, scale=ex[:, 6 + h:7 + h])
                        ks = ks_r.next()
                        k.ts("dve", ks[:], ktm[:, hs], ex[:, 12 + h:13 + h], ALU.mult)
                        pu = pu_r.next()
                        k.mm(pu[:, 0:64], N[:], sc[:, 0, :])
                        k.mm(pu[0:64, 64:192], sc[:, 1, :], N[:])
                        u = u_r.next(); wk = wk_r.next()
                        k.copy("act", u[:], pu[:, 0:64])
                        k.copy("dve", wk[:], pu[0:64, 64:192])
                        pv = pv_r.next()
                        k.mm(pv[:, 0:64], wk[:], Sbf[h][:])
                        vn = vn_r.next()
                        k.tt("dve", vn[:], u[:], pv[:, 0:64], ALU.subtract)
                        k.mm(pv[:, 64:128], qT[:, h, :], Sbf[h][:])
                        k.mm(pv[:, 128:192], at[:], vn[:])
                        k.mm(pv[0:64, 192:256], ks[:], vn[:])
                        o2 = o2_r.next()
                        k.copy("act", o2[:], pv[:, 128:192])
                        k.stt(ob[:, hs], pv[:, 64:128], ex[:, h:h + 1], o2[:], ALU.mult, ALU.add)
                        k.stt(S32[h][:], S32[h][:], egl[:, h:h + 1], pv[0:64, 192:256], ALU.mult, ALU.add)
                        k.copy("act", Sbf[h][:], S32[h][:])
                        GP.step(gen)
                cur = GP.finish(gen)
                if d == 0:
                    k.dma("pool", ogd[t], ob[:])
                else:
                    of = of_r.next(); z = z_r.next(); gz = gz_r.next(); yo = yo_r.next()
                    k.dma("sp", of[:], ogd[t])
                    k.dma("sp", z[:], tmv[t][:, TM_GDZ:TM_GDZ + 384])
                    k.tt("dve", ob[:], ob[:], of[:], ALU.add)
                    k.act(gz[:], z[:], AF.Silu)
                    k.tt("dve", gz[:], gz[:], nwb[:], ALU.mult)
                    sm = sm_r.next()
                    for h in range(6):
                        k.act(junk[:], ob[:, h * 64:(h + 1) * 64], AF.Square, accum=sm[:, h:h + 1])
                    sm2 = sm_r.next()
                    k.act(sm2[:, 0:6], sm[:, 0:6], AF.Sqrt, bias=EPS, scale=1.0 / 64.0)
                    sm3 = sm_r.next()
                    k.recip(sm3[:, 0:6], sm2[:, 0:6])
                    for h in range(6):
                        hs = slice(h * 64, (h + 1) * 64)
                        k.stt(yo[:, hs], ob[:, hs], sm3[:, h:h + 1], gz[:, hs], ALU.mult, ALU.mult)
                    k.dma("pool", yv[t][:, 640:1024], yo[:])
        k.P.barrier()
        k.P.emit()


S_FULL = 8192


def build_program(S):
    nc = bass.Bass("TRN2", target_bir_lowering=False)
    dr = declare_dram(nc, S)
    with ExitStack() as es:
        k = K(nc, es)
        cst = setup_consts(k, es, dr)
        for l in range(DEPTH):
            dr["x_cur"] = dr["x"] if l == 0 else dr["xs"]
            phase_inproj(k, S, l, dr, cst)
            phase_na(k, S, l, dr, cst)
            phase_mlstm(k, S, l, dr, cst)
            phase_gdn_prep(k, S, l, dr, cst)
            phase_gdn(k, S, l, dr, cst)
            phase_mix_xattn(k, S, l, dr, cst, dr["x_cur"], dr["xs"])
            phase_ffn(k, S, l, dr, cst, dr["xs"], final_out=(dr["out"] if l == DEPTH - 1 else None))
    return nc


def kernel(**inputs):
    x = np.asarray(inputs["x"], dtype=np.float32)
    mem = np.asarray(inputs["mem"], dtype=np.float32)
    B, S, _ = x.shape
    nc = build_program(S)
    shared = {n: np.ascontiguousarray(np.asarray(inputs[n], dtype=np.float32)) for n in WEIGHT_SHAPES}
    shared.update(make_consts_host())
    shared["nab"] = na_bias_host(np.asarray(inputs["na_rel_bias"], dtype=np.float32), S)
    in_maps = []
    for b in range(B):
        m = dict(shared)
        m["x"] = np.ascontiguousarray(x[b])
        m["mem"] = np.ascontiguousarray(mem[b])
        in_maps.append(m)
    res = run_bass_kernel_spmd(nc, in_maps, core_ids=list(range(B)))
    return np.stack([np.asarray(r["out"], dtype=np.float32) for r in res.results], axis=0)
```
